# Optimizing a Trainium2 kernel written in Bass

```python
import math
import jax, jax.numpy as jnp
from jax import lax
import numpy as np

D_MODEL = 1024
BATCH = 1
SEQ = 16384
DEPTH = 2

GRID_W = 64
CTX_LEN = 256
BLOCK_Q = 128
RET_CHUNK = 128
ROPE_THETA = 10000.0
EPS = 1e-6

RET_HEADS = 8
RET_HEAD_DIM = 64
GQA_HEADS = 8
GQA_KV_HEADS = 2
GQA_HEAD_DIM = 64
GQA_GROUP = GQA_HEADS // GQA_KV_HEADS
MLA_HEADS = 8
MLA_Q_RANK = 384
MLA_KV_RANK = 256
MLA_NOPE = 64
MLA_ROPE = 32
MLA_V = 64
FFN_DIM = 2816
N_EXPERTS = 8
TOP_K = 2
EXPERT_DIM = 3584

RET_W = RET_HEADS * RET_HEAD_DIM
GQA_QW = GQA_HEADS * GQA_HEAD_DIM
GQA_KW = GQA_KV_HEADS * GQA_HEAD_DIM
L0_WIDTHS = (RET_W, RET_W, RET_W, RET_W, GQA_QW, GQA_KW, GQA_KW)
L0_IN = sum(L0_WIDTHS)
L0_SPLITS = tuple(int(v) for v in np.cumsum(L0_WIDTHS)[:-1])
L1_WIDTHS = (MLA_Q_RANK, MLA_KV_RANK, MLA_ROPE)
L1_IN = sum(L1_WIDTHS)
L1_SPLITS = tuple(int(v) for v in np.cumsum(L1_WIDTHS)[:-1])

kernel_name = "hybrid_retention_gqa_mla_moe_dit"


def rms_norm(x, g):
    xf = x.astype(jnp.float32)
    y = xf * lax.rsqrt(jnp.mean(xf * xf, axis=-1, keepdims=True) + EPS)
    return (y * g.astype(jnp.float32)).astype(x.dtype)


def modulate(x, g, shift, scale):
    return rms_norm(x, g) * (1 + scale) + shift


def ada_params(cvec, w, b):
    mod = jax.nn.silu(cvec) @ w + b
    return jnp.split(mod[..., None, :], 6, axis=-1)


def axial_rope_tables(n, rot_dim):
    rows = n // GRID_W
    row = jnp.broadcast_to(jnp.arange(rows)[:, None], (rows, GRID_W)).reshape(n).astype(jnp.float32)
    col = jnp.broadcast_to(jnp.arange(GRID_W)[None, :], (rows, GRID_W)).reshape(n).astype(jnp.float32)
    axis_dim = rot_dim // 2
    inv_freq = ROPE_THETA ** (-jnp.arange(0, axis_dim, 2, dtype=jnp.float32) / axis_dim)
    ang = jnp.concatenate([row[:, None] * inv_freq, col[:, None] * inv_freq], axis=-1)
    return jnp.cos(ang), jnp.sin(ang)


def apply_rope(x, cos, sin):
    xf = x.astype(jnp.float32).reshape(x.shape[:-1] + (x.shape[-1] // 2, 2))
    x0, x1 = xf[..., 0], xf[..., 1]
    c = cos[None, :, None, :]
    s = sin[None, :, None, :]
    out = jnp.stack([x0 * c - x1 * s, x0 * s + x1 * c], axis=-1).reshape(x.shape)
    return out.astype(x.dtype)


def block_attention(q, k, v):
    b, n, hk, g, dq = q.shape
    nb = n // BLOCK_Q
    scale = dq ** -0.5
    qb = q.reshape(b, nb, BLOCK_Q, hk, g, dq).transpose(1, 0, 2, 3, 4, 5)

    def one_block(qblk):
        s = jnp.einsum("bqkgd,bskd->bkgqs", qblk, k, preferred_element_type=jnp.float32) * scale
        p = jax.nn.softmax(s, axis=-1).astype(v.dtype)
        return jnp.einsum("bkgqs,bskd->bqkgd", p, v)

    out = lax.map(one_block, qb)
    return out.transpose(1, 0, 2, 3, 4, 5).reshape(b, n, hk, g, v.shape[-1])


def retention_scan(q, k, v, log_gamma, s0):
    b, n, h, d = q.shape
    C = RET_CHUNK
    nc = n // C
    f32 = jnp.float32
    qf = q.astype(f32).reshape(b, nc, C, h, d)
    kf = k.astype(f32).reshape(b, nc, C, h, d)
    vf = v.astype(f32).reshape(b, nc, C, h, d)
    lg = log_gamma.astype(f32)
    idx = jnp.arange(C, dtype=f32)
    rel = idx[:, None] - idx[None, :]
    decay = jnp.where(rel[None] >= 0, jnp.exp(jnp.maximum(rel, 0.0)[None] * lg[:, None, None]), 0.0)
    scores = jnp.einsum("bnchd,bnmhd->bnhcm", qf, kf) * decay[None, None]
    o_inner = jnp.einsum("bnhcm,bnmhe->bnche", scores, vf)
    zeta = jnp.exp((C - 1 - idx)[:, None] * lg[None, :])
    xi = jnp.exp((idx + 1)[:, None] * lg[None, :])
    u = jnp.einsum("bnmhd,bnmhe->nbhde", kf * zeta[None, None, :, :, None], vf)
    g_chunk = jnp.exp(C * lg)[None, :, None, None]

    def step(s, u_n):
        return g_chunk * s + u_n, s

    s_final, s_prev = lax.scan(step, s0, u)
    o_cross = jnp.einsum("bnchd,nbhde->bnche", qf * xi[None, None, :, :, None], s_prev)
    return (o_inner + o_cross).reshape(b, n, h, d), s_final


def bidir_retention(q, k, v, log_decay, s0_fwd, s0_bwd):
    o_f, s_f = retention_scan(q, k, v, log_decay[0], s0_fwd)
    o_b, s_b = retention_scan(q[:, ::-1], k[:, ::-1], v[:, ::-1], log_decay[1], s0_bwd)
    return o_f + o_b[:, ::-1], s_f, s_b


def head_group_norm(o):
    mu = jnp.mean(o, axis=-1, keepdims=True)
    var = jnp.mean(jnp.square(o - mu), axis=-1, keepdims=True)
    return (o - mu) * lax.rsqrt(var + EPS)


def swiglu(h, w1, w3, w2):
    return (jax.nn.silu(h @ w1) * (h @ w3)) @ w2


def even_project(h, w_in, qn_g, kn_g):
    b, n, _ = h.shape
    a_q, a_k, a_v, a_g, b_q, b_k, b_v = jnp.split(h @ w_in, L0_SPLITS, axis=-1)
    rq = a_q.reshape(b, n, RET_HEADS, RET_HEAD_DIM)
    rk = a_k.reshape(b, n, RET_HEADS, RET_HEAD_DIM) * (RET_HEAD_DIM ** -0.5)
    rv = a_v.reshape(b, n, RET_HEADS, RET_HEAD_DIM)
    aq = rms_norm(b_q.reshape(b, n, GQA_HEADS, GQA_HEAD_DIM), qn_g)
    ak = rms_norm(b_k.reshape(b, n, GQA_KV_HEADS, GQA_HEAD_DIM), kn_g)
    av = b_v.reshape(b, n, GQA_KV_HEADS, GQA_HEAD_DIM)
    return rq, rk, rv, a_g, aq, ak, av


def retention_out(o, gate, dtype):
    b, n = o.shape[:2]
    return (head_group_norm(o).reshape(b, n, RET_W) * jax.nn.silu(gate.astype(jnp.float32))).astype(dtype)


def even_layer(x, ctx, c, c_ctx, ada_w, ada_b, n1_g, n2_g, w_in, log_decay, qn_g, kn_g, w_out,
               w1, w3, w2, cos, sin):
    b, n, _ = x.shape
    lc = ctx.shape[1]
    mx = ada_params(c, ada_w, ada_b)
    mc = ada_params(c_ctx, ada_w, ada_b)
    rq_c, rk_c, rv_c, rg_c, aq_c, ak_c, av_c = even_project(modulate(ctx, n1_g, mc[0], mc[1]), w_in, qn_g, kn_g)
    rq_x, rk_x, rv_x, rg_x, aq_x, ak_x, av_x = even_project(modulate(x, n1_g, mx[0], mx[1]), w_in, qn_g, kn_g)
    zero = jnp.zeros((b, RET_HEADS, RET_HEAD_DIM, RET_HEAD_DIM), jnp.float32)
    ro_c, s_f, s_b = bidir_retention(rq_c, rk_c, rv_c, log_decay, zero, zero)
    ro_x, _, _ = bidir_retention(rq_x, rk_x, rv_x, log_decay, s_f, s_b)
    ra_c = retention_out(ro_c, rg_c, ctx.dtype)
    ra_x = retention_out(ro_x, rg_x, x.dtype)
    aq_x = apply_rope(aq_x, cos, sin)
    ak_x = apply_rope(ak_x, cos, sin)
    k_all = jnp.concatenate([ak_x, ak_c], axis=1)
    v_all = jnp.concatenate([av_x, av_c], axis=1)
    ao_x = block_attention(aq_x.reshape(b, n, GQA_KV_HEADS, GQA_GROUP, GQA_HEAD_DIM), k_all, v_all).reshape(b, n, GQA_QW)
    ao_c = block_attention(aq_c.reshape(b, lc, GQA_KV_HEADS, GQA_GROUP, GQA_HEAD_DIM), ak_c, av_c).reshape(b, lc, GQA_QW)
    x = x + mx[2] * (jnp.concatenate([ra_x, ao_x], axis=-1) @ w_out)
    ctx = ctx + mc[2] * (jnp.concatenate([ra_c, ao_c], axis=-1) @ w_out)
    x = x + mx[5] * swiglu(modulate(x, n2_g, mx[3], mx[4]), w1, w3, w2)
    ctx = ctx + mc[5] * swiglu(modulate(ctx, n2_g, mc[3], mc[4]), w1, w3, w2)
    return x, ctx


def mla_keys(ckv, kr, kv_lora_g, w_ukv, kn_g, kr_g, rope):
    b, n, _ = ckv.shape
    kv = (rms_norm(ckv, kv_lora_g) @ w_ukv).reshape(b, n, MLA_HEADS, MLA_NOPE + MLA_V)
    k_nope = rms_norm(kv[..., :MLA_NOPE], kn_g)
    k_rope = rms_norm(kr.reshape(b, n, 1, MLA_ROPE), kr_g)
    if rope is not None:
        k_rope = apply_rope(k_rope, rope[0], rope[1])
    k = jnp.concatenate([k_nope, jnp.broadcast_to(k_rope, (b, n, MLA_HEADS, MLA_ROPE))], axis=-1)
    return k, kv[..., MLA_NOPE:]


def moe_swiglu(h, router, e_w1, e_w3, e_w2):
    b, n, d = h.shape
    t = h.reshape(b * n, d)
    logits = jnp.einsum("td,de->te", t, router, preferred_element_type=jnp.float32)
    top_v, top_i = lax.top_k(logits, TOP_K)
    top_w = jax.nn.softmax(top_v, axis=-1)
    gates = jnp.sum(jax.nn.one_hot(top_i, N_EXPERTS, dtype=jnp.float32) * top_w[..., None], axis=1)
    out = jnp.zeros((b * n, d), jnp.float32)
    for e in range(N_EXPERTS):
        out = out + gates[:, e:e + 1] * swiglu(t, e_w1[e], e_w3[e], e_w2[e]).astype(jnp.float32)
    return out.reshape(b, n, d).astype(h.dtype)


def odd_layer(x, ctx, c, c_ctx, ada_w, ada_b, n1_g, n2_g, w_in, q_lora_g, kv_lora_g, w_uq, w_ukv,
              qn_g, qr_g, kn_g, kr_g, w_out, router, e_w1, e_w3, e_w2, cos, sin):
    b, n, _ = x.shape
    mx = ada_params(c, ada_w, ada_b)
    mc = ada_params(c_ctx, ada_w, ada_b)
    cq_x, ckv_x, kr_x = jnp.split(modulate(x, n1_g, mx[0], mx[1]) @ w_in, L1_SPLITS, axis=-1)
    ckv_c, kr_c = jnp.split(modulate(ctx, n1_g, mc[0], mc[1]) @ w_in[:, MLA_Q_RANK:], (MLA_KV_RANK,), axis=-1)
    q = (rms_norm(cq_x, q_lora_g) @ w_uq).reshape(b, n, MLA_HEADS, MLA_NOPE + MLA_ROPE)
    q = jnp.concatenate([rms_norm(q[..., :MLA_NOPE], qn_g),
                         apply_rope(rms_norm(q[..., MLA_NOPE:], qr_g), cos, sin)], axis=-1)
    k_x, v_x = mla_keys(ckv_x, kr_x, kv_lora_g, w_ukv, kn_g, kr_g, (cos, sin))
    k_c, v_c = mla_keys(ckv_c, kr_c, kv_lora_g, w_ukv, kn_g, kr_g, None)
    o = block_attention(q[:, :, :, None, :], jnp.concatenate([k_x, k_c], axis=1),
                        jnp.concatenate([v_x, v_c], axis=1)).reshape(b, n, MLA_HEADS * MLA_V)
    x = x + mx[2] * (o @ w_out)
    x = x + mx[5] * moe_swiglu(modulate(x, n2_g, mx[3], mx[4]), router, e_w1, e_w3, e_w2)
    return x


def setup_inputs(seed: int = 0) -> dict:
    key = jax.random.key(seed)
    ks = iter(jax.random.split(key, 48))
    D = D_MODEL

    def nrm(shape, scale):
        return jax.random.normal(next(ks), shape, jnp.float32) * scale

    def gain(m):
        return 1.0 + nrm((m,), 0.02)

    base_log_decay = jnp.log1p(-(2.0 ** (-5.0 - jnp.arange(RET_HEADS, dtype=jnp.float32))))
    return {
        "x": nrm((BATCH, SEQ, D), 1.0),
        "c": nrm((BATCH, D), 1.0),
        "ctx": nrm((BATCH, CTX_LEN, D), 1.0),
        "c_ctx": nrm((D,), 1.0),
        "l0_ada_w": nrm((D, 6 * D), 0.5 * D ** -0.5),
        "l0_ada_b": nrm((6 * D,), 0.01),
        "l0_norm1_g": gain(D),
        "l0_norm2_g": gain(D),
        "l0_w_in": nrm((D, L0_IN), D ** -0.5),
        "l0_ret_log_decay": base_log_decay[None, :] * jnp.exp(nrm((2, RET_HEADS), 0.05)),
        "l0_q_norm_g": gain(GQA_HEAD_DIM),
        "l0_k_norm_g": gain(GQA_HEAD_DIM),
        "l0_w_out": nrm((RET_W + GQA_QW, D), (RET_W + GQA_QW) ** -0.5),
        "l0_ffn_w1": nrm((D, FFN_DIM), D ** -0.5),
        "l0_ffn_w3": nrm((D, FFN_DIM), D ** -0.5),
        "l0_ffn_w2": nrm((FFN_DIM, D), FFN_DIM ** -0.5),
        "l1_ada_w": nrm((D, 6 * D), 0.5 * D ** -0.5),
        "l1_ada_b": nrm((6 * D,), 0.01),
        "l1_norm1_g": gain(D),
        "l1_norm2_g": gain(D),
        "l1_w_in": nrm((D, L1_IN), D ** -0.5),
        "l1_q_lora_g": gain(MLA_Q_RANK),
        "l1_kv_lora_g": gain(MLA_KV_RANK),
        "l1_w_uq": nrm((MLA_Q_RANK, MLA_HEADS * (MLA_NOPE + MLA_ROPE)), MLA_Q_RANK ** -0.5),
        "l1_w_ukv": nrm((MLA_KV_RANK, MLA_HEADS * (MLA_NOPE + MLA_V)), MLA_KV_RANK ** -0.5),
        "l1_q_nope_g": gain(MLA_NOPE),
        "l1_q_rope_g": gain(MLA_ROPE),
        "l1_k_nope_g": gain(MLA_NOPE),
        "l1_k_rope_g": gain(MLA_ROPE),
        "l1_w_out": nrm((MLA_HEADS * MLA_V, D), (MLA_HEADS * MLA_V) ** -0.5),
        "l1_router": nrm((D, N_EXPERTS), D ** -0.5),
        "l1_exp_w1": nrm((N_EXPERTS, D, EXPERT_DIM), D ** -0.5),
        "l1_exp_w3": nrm((N_EXPERTS, D, EXPERT_DIM), D ** -0.5),
        "l1_exp_w2": nrm((N_EXPERTS, EXPERT_DIM, D), EXPERT_DIM ** -0.5),
    }


def reference(x, c, ctx, c_ctx, l0_ada_w, l0_ada_b, l0_norm1_g, l0_norm2_g, l0_w_in, l0_ret_log_decay,
              l0_q_norm_g, l0_k_norm_g, l0_w_out, l0_ffn_w1, l0_ffn_w3, l0_ffn_w2,
              l1_ada_w, l1_ada_b, l1_norm1_g, l1_norm2_g, l1_w_in, l1_q_lora_g, l1_kv_lora_g,
              l1_w_uq, l1_w_ukv, l1_q_nope_g, l1_q_rope_g, l1_k_nope_g, l1_k_rope_g, l1_w_out,
              l1_router, l1_exp_w1, l1_exp_w3, l1_exp_w2):
    n = x.shape[1]
    cos_b, sin_b = axial_rope_tables(n, GQA_HEAD_DIM)
    cos_m, sin_m = axial_rope_tables(n, MLA_ROPE)
    for layer in range(DEPTH):
        if layer % 2 == 0:
            x, ctx = even_layer(x, ctx, c, c_ctx, l0_ada_w, l0_ada_b, l0_norm1_g, l0_norm2_g, l0_w_in,
                                l0_ret_log_decay, l0_q_norm_g, l0_k_norm_g, l0_w_out,
                                l0_ffn_w1, l0_ffn_w3, l0_ffn_w2, cos_b, sin_b)
        else:
            x = odd_layer(x, ctx, c, c_ctx, l1_ada_w, l1_ada_b, l1_norm1_g, l1_norm2_g, l1_w_in,
                          l1_q_lora_g, l1_kv_lora_g, l1_w_uq, l1_w_ukv, l1_q_nope_g, l1_q_rope_g,
                          l1_k_nope_g, l1_k_rope_g, l1_w_out, l1_router, l1_exp_w1, l1_exp_w3,
                          l1_exp_w2, cos_m, sin_m)
    return x
```

```python
import numpy as np
import ml_dtypes
import concourse.bass as bass
import concourse.mybir as mybir
from concourse.bass_utils import run_bass_kernel_spmd

F32 = mybir.dt.float32
BF16 = mybir.dt.bfloat16
AF = mybir.ActivationFunctionType
ALU = mybir.AluOpType
AX = mybir.AxisListType

NCORES = 8
D = 1024
KT = 8
GRID_W = 64
LC = 256
EPS = 1e-6
ROPE_THETA = 10000.0
FFN = 2816
NE = 8
EDIM = 3584
L0_IN = 2816
L1_IN = 672


class Op:
    __slots__ = ("eng", "fn", "deps", "dma", "sig", "ms", "dsem", "dval", "ringprev", "tag")


class Sched:
    ENGS = ("pe", "act", "dve", "pool", "sp")
    RING = 8

    def __init__(self):
        self.ops = []
        self.lastw = {}
        self.rds = {}
        self.pend_barrier = {e: [] for e in self.ENGS}
        self.last_op = {e: None for e in self.ENGS}
        self.all_dma = []

    def add(self, eng, fn, reads=(), writes=(), dma=False, tag=None):
        op = Op()
        op.eng, op.fn, op.dma, op.sig, op.ms = eng, fn, dma, False, 0
        op.dsem = op.dval = op.ringprev = None
        op.tag = tag
        deps = set()
        for k in reads:
            w = self.lastw.get(k)
            if w is not None:
                deps.add(w)
            if k.startswith("bank"):
                for r in self.rds.get(k, ()):
                    if r.eng != eng:
                        deps.add(r)
        for k in writes:
            w = self.lastw.get(k)
            if w is not None:
                deps.add(w)
            for r in self.rds.get(k, ()):
                deps.add(r)
        for d in self.pend_barrier[eng]:
            deps.add(d)
        self.pend_barrier[eng] = []
        op.deps = deps
        for k in reads:
            lst = self.rds.setdefault(k, [])
            if not dma:
                lst[:] = [r for r in lst if r.dma or r.eng != eng]
            lst.append(op)
        for k in writes:
            self.lastw[k] = op
            self.rds[k] = []
        self.ops.append(op)
        self.last_op[eng] = op
        if dma:
            self.all_dma.append(op)
        return op

    def barrier(self):
        lasts = [o for o in self.last_op.values() if o is not None]
        dm = list(self.all_dma)
        self.all_dma = []
        for e in self.ENGS:
            self.pend_barrier[e] = self.pend_barrier[e] + lasts + dm

    def finalize(self, nc):
        for op in self.ops:
            for d in op.deps:
                if d.dma:
                    continue
                if d.eng == "pe" and op.eng == "pe" and not op.dma:
                    continue
                d.sig = True
        self.sem = {e: nc.alloc_semaphore("sem_" + e) for e in self.ENGS}
        self.ring = {e: [nc.alloc_semaphore("dma_%s_%d" % (e, i)) for i in range(self.RING)]
                     for e in ("sp", "pool", "act")}
        cnt = {e: 0 for e in self.ENGS}
        dcnt = {e: 0 for e in self.ENGS}
        ringlast = {e: [None] * self.RING for e in self.ENGS}
        for op in self.ops:
            if op.dma:
                i = dcnt[op.eng]
                dcnt[op.eng] += 1
                slot = i % self.RING
                op.dsem = self.ring[op.eng][slot]
                op.dval = 16 * (i // self.RING + 1)
                op.ringprev = ringlast[op.eng][slot]
                ringlast[op.eng][slot] = op
            elif op.sig:
                cnt[op.eng] += 1
                op.ms = cnt[op.eng]
        self.by_eng = {e: [o for o in self.ops if o.eng == e] for e in self.ENGS}
        self.counts = cnt

    def emit(self, ename, eng):
        waited = {}
        for op in self.by_eng[ename]:
            waits = {}
            for d in op.deps:
                if d.dma:
                    key, sem, val = ("d", d.eng, id(d.dsem)), d.dsem, d.dval
                else:
                    if d.eng == "pe" and ename == "pe" and not op.dma:
                        continue
                    key, sem, val = ("c", d.eng, 0), self.sem[d.eng], d.ms
                if key not in waits or waits[key][1] < val:
                    waits[key] = (sem, val)
            if op.dma and op.ringprev is not None:
                d = op.ringprev
                key = ("d", d.eng, id(d.dsem))
                if key not in waits or waits[key][1] < d.dval:
                    waits[key] = (d.dsem, d.dval)
            for key, (sem, val) in waits.items():
                if waited.get(key, 0) < val:
                    eng.wait_ge(sem, val)
                    waited[key] = val
            ins = op.fn(eng)
            if ins is None:
                continue
            if op.dma:
                ins.then_inc(op.dsem, 16)
            elif op.sig:
                ins.then_inc(self.sem[ename], 1)


def _rope_tables(pos_rows, pos_cols, rot_dim):
    axis_dim = rot_dim // 2
    inv_freq = (ROPE_THETA ** (-np.arange(0, axis_dim, 2, dtype=np.float32) / axis_dim)).astype(np.float32)
    ang = np.concatenate([pos_rows[:, None].astype(np.float32) * inv_freq,
                          pos_cols[:, None].astype(np.float32) * inv_freq], axis=-1)
    return np.cos(ang).astype(np.float32), np.sin(ang).astype(np.float32)


def host_consts(core, TL):
    bf = ml_dtypes.bfloat16
    c = {}
    c["ident_bf"] = np.eye(128, dtype=np.float32).astype(bf)
    c["ident_f"] = np.eye(128, dtype=np.float32)
    c["ones_bf"] = np.ones((128, 128), np.float32).astype(bf)
    c["ones_f"] = np.ones((128, 128), np.float32)
    bo = np.zeros((128, 128), np.float32)
    bo[:64, :64] = 1.0 / 64
    bo[64:, 64:] = 1.0 / 64
    c["bo64"] = bo.astype(bf)
    bm = np.zeros((128, 128), np.float32)
    bm[:64, :64] = 1.0 / 64
    bm[64:96, 64:96] = 1.0 / 32
    c["bm96"] = bm.astype(bf)
    bd = np.zeros((128, 128), np.float32)
    bd[:64, :64] = 1.0
    bd[64:, 64:] = 1.0
    c["bdmask"] = bd
    r = np.zeros((128, 128), np.float32)
    for i in range(64):
        r[2 * i + 1, 2 * i] = -1.0
        r[2 * i, 2 * i + 1] = 1.0
    c["rsw"] = r.astype(bf)
    r96 = r.copy()
    r96[:64, :] = 0.0
    r96[:, :64] = 0.0
    r96[96:, :] = 0.0
    r96[:, 96:] = 0.0
    c["rsw96"] = r96.astype(bf)
    t = core * TL + np.arange(TL)
    rows, cols = t // GRID_W, t % GRID_W
    cs, sn = _rope_tables(rows, cols, 64)
    c["cos0"] = np.ascontiguousarray(np.repeat(cs, 2, axis=1).T)
    c["sin0"] = np.ascontiguousarray(np.repeat(sn, 2, axis=1).T)
    c["cos0"] = np.concatenate([c["cos0"], c["cos0"]], axis=0)
    c["sin0"] = np.concatenate([c["sin0"], c["sin0"]], axis=0)
    cs, sn = _rope_tables(rows, cols, 32)
    c1 = np.ones((128, TL), np.float32)
    s1 = np.zeros((128, TL), np.float32)
    c1[64:96] = np.repeat(cs, 2, axis=1).T
    s1[64:96] = np.repeat(sn, 2, axis=1).T
    c["cos1"], c["sin1"] = c1, s1
    m = np.arange(128, dtype=np.float32)
    cc = np.arange(128, dtype=np.float32)
    relf = np.maximum(cc[None, :] - m[:, None], 0.0)
    relb = np.maximum(m[:, None] - cc[None, :], 0.0)
    c["relf"] = relf.astype(np.float32)
    c["relb"] = relb.astype(np.float32)
    c["indf"] = (cc[None, :] >= m[:, None]).astype(np.float32)
    c["indb"] = (m[:, None] >= cc[None, :]).astype(np.float32)
    c["posf"] = np.repeat((127.0 - m)[:, None], 64, axis=1).astype(np.float32)
    c["posb"] = np.repeat(m[:, None], 64, axis=1).astype(np.float32)
    c["rampf"] = np.repeat((cc + 1.0)[None, :], 128, axis=0).astype(np.float32)
    c["rampb"] = np.repeat((128.0 - cc)[None, :], 128, axis=0).astype(np.float32)
    nch = TL // 128
    cp = np.zeros((128, 40), np.float32)
    for r_ in range(NCORES):
        if r_ < core:
            cp[:, r_] = 128.0 * nch * (core - 1 - r_)
            cp[:, 8 + r_] = 1.0
        if r_ > core:
            cp[:, 16 + r_] = 128.0 * nch * (r_ - core - 1)
            cp[:, 24 + r_] = 1.0
    cp[:, 32] = 128.0 * nch * core
    cp[:, 33] = 128.0 * nch * (NCORES - 1 - core)
    c["cpos"] = cp
    return c


class Gen:
    def __init__(self, TL, mode):
        self.TL = TL
        self.T = TL + LC
        self.NCH = TL // 128
        self.S_ALL = NCORES * TL + LC
        self.NKB = self.S_ALL // 128
        self.mode = mode
        self.nc = bass.Bass("TRN2", target_bir_lowering=False)
        self.S = Sched()
        self.din = {}
        self.dout = {}
        self.groups = [(g * 512, 512, 0) for g in range(TL // 512)] + [(TL, LC, 1)]
        self.uid = 0

    def inp(self, name, shape, dt=F32):
        t = self.nc.dram_tensor(name, list(shape), dt, kind="ExternalInput")
        self.din[name] = t
        return t.ap()

    def outp(self, name, shape, dt=F32):
        t = self.nc.dram_tensor(name, list(shape), dt, kind="ExternalOutput")
        self.dout[name] = t
        return t.ap()

    def scratch(self, name, shape, dt):
        return self.nc.dram_tensor(name, list(shape), dt).ap()

    def sb(self, name, shape, dt):
        return self.nc.alloc_sbuf_tensor(name, list(shape), dt)

    def op(self, eng, fn, r=(), w=(), dma=False):
        return self.S.add(eng, fn, r, w, dma)

    def dma(self, q, out, in_, r=(), w=()):
        return self.S.add(q, lambda e, o=out, i=in_: e.dma_start(out=o, in_=i), r, w, dma=True)

    def mm(self, out, lhsT, rhs, start, stop, r=(), w=()):
        return self.S.add("pe", lambda e, o=out, l=lhsT, rr=rhs, s=start, t=stop:
                          e.matmul(o, l, rr, start=s, stop=t), r, w)

    def mmk(self, out, lhs_list, rhs_list, r=(), w=()):
        n = len(lhs_list)

        def fn(e, o=out, ll=lhs_list, rl=rhs_list):
            ins = None
            for k in range(n):
                ins = e.matmul(o, ll[k], rl[k], start=(k == 0), stop=(k == n - 1))
            return ins
        return self.S.add("pe", fn, r, w)

    def act(self, out, in_, func, bias=0.0, scale=1.0, r=(), w=()):
        return self.S.add("act", lambda e, o=out, i=in_, f=func, b=bias, s=scale:
                          e.activation(out=o, in_=i, func=f, bias=b, scale=s), r, w)

    def tt(self, eng, out, in0, in1, op, r=(), w=()):
        return self.S.add(eng, lambda e, o=out, a=in0, b=in1, p=op: e.tensor_tensor(o, a, b, p), r, w)

    def ts(self, eng, out, in0, s1, s2, op0, op1=None, r=(), w=()):
        if op1 is None:
            return self.S.add(eng, lambda e, o=out, a=in0, x=s1, p=op0:
                              e.tensor_scalar(o, a, x, None, p), r, w)
        return self.S.add(eng, lambda e, o=out, a=in0, x=s1, y=s2, p=op0, q=op1:
                          e.tensor_scalar(o, a, x, y, p, q), r, w)

    def stt(self, eng, out, in0, scalar, in1, op0, op1, r=(), w=()):
        return self.S.add(eng, lambda e, o=out, a=in0, s=scalar, b=in1, p=op0, q=op1:
                          e.scalar_tensor_tensor(o, a, s, b, p, q), r, w)

    def cp(self, eng, out, in_, r=(), w=()):
        if eng == "act":
            return self.S.add("act", lambda e, o=out, i=in_: e.copy(o, i), r, w)
        return self.S.add(eng, lambda e, o=out, i=in_: e.tensor_copy(o, i), r, w)

    def recip(self, out, in_, r=(), w=()):
        return self.S.add("dve", lambda e, o=out, i=in_: e.reciprocal(o, i), r, w)

    def memset(self, eng, ap, val, w=()):
        return self.S.add(eng, lambda e, a=ap, v=val: e.memset(a, v), (), w)

    def load_consts(self):
        TL = self.TL
        spec = [("ident_bf", [128, 128], BF16), ("ident_f", [128, 128], F32),
                ("ones_bf", [128, 128], BF16), ("ones_f", [128, 128], F32),
                ("bo64", [128, 128], BF16), ("bm96", [128, 128], BF16),
                ("bdmask", [128, 128], F32), ("rsw", [128, 128], BF16), ("rsw96", [128, 128], BF16),
                ("cpos", [128, 40], F32)]
        self.C = {}
        for name, shape, dt in spec:
            d = self.inp("c_" + name, shape, dt)
            s = self.sb("C_" + name, shape, dt)
            self.dma("sp", s[:], d, w=["C_" + name])
            self.C[name] = s
        self.C_dram = {}
        for name, shape in [("cos0", [128, TL]), ("sin0", [128, TL]), ("cos1", [128, TL]), ("sin1", [128, TL]),
                            ("relf", [128, 128]), ("relb", [128, 128]), ("indf", [128, 128]),
                            ("indb", [128, 128]), ("posf", [128, 64]), ("posb", [128, 64]),
                            ("rampf", [128, 128]), ("rampb", [128, 128])]:
            self.C_dram[name] = self.inp("c_" + name, shape, F32)
        self.bank = [self.nc.alloc_psum_tensor("bank%d" % i, [128, 512], F32) for i in range(8)]

    def ada(self, L, ada_w, ada_b, cvec_sb):
        modT = self.sb("modT%d" % L, [128, 48, 2], F32)
        abT = self.gain_cols("abT%d" % L, ada_b, 48)
        NCHK = 12
        CW = 6 * D // NCHK
        self.ar.reset()
        wbuf = [self.ar.alloc([128, KT, CW], F32) for i in range(2)]
        aw = ada_w.rearrange("(k p) c -> p k c", p=128)
        pb = self.bank[7]
        for ch in range(NCHK):
            wb = wbuf[ch % 2]
            self.dma("sp", wb, aw[:, :, ch * CW:(ch + 1) * CW], w=["adaw%d" % (ch % 2)])
            for jj in range(CW // 128):
                j = ch * (CW // 128) + jj
                self.mmk(pb[:, 2 * j:2 * j + 2],
                         [wb[:, k, jj * 128:(jj + 1) * 128] for k in range(KT)],
                         [cvec_sb[:, k, :] for k in range(KT)],
                         r=["adaw%d" % (ch % 2), "scvec"], w=["bank7"])
        pv = pb[:, 0:96].rearrange("p (j v) -> p j v", v=2)
        for v in range(2):
            self.tt("dve", modT[:, :, v], pv[:, :, v], abT[:], ALU.add, r=["bank7", "abT%d" % L], w=["modT%d" % L])
        self.S.barrier()
        return modT

    def gain_cols(self, name, g_ap, n, pieces=None):
        t = self.sb(name, [128, n], F32)
        rows = self.sb(name + "_rows", [n, 128], F32)
        if pieces is None:
            self.dma("sp", rows[:], g_ap.rearrange("(k p) -> k p", p=128), w=[name + "_rows"])
        else:
            self.memset("pool", rows[:], 0.0, w=[name + "_rows"])
            for (c0, c1, src) in pieces:
                self.dma("sp", rows[0:1, c0:c1], src.rearrange("(o p) -> o p", o=1), r=[name + "_rows"], w=[name + "_rows_%d" % c0])
        pb = self.bank[7]
        rk = [name + "_rows"] + ([name + "_rows_%d" % c0 for (c0, c1, src) in pieces] if pieces else [])
        self.mm(pb[:, 0:n], rows[0:n, :], self.C["ident_f"][0:n, 0:n], True, True, r=rk + ["C_ident_f"], w=["bank7"])
        self.cp("dve", t[:], pb[:, 0:n], r=["bank7"], w=[name])
        return t

    def mod_derived(self, L, modT, n1g, n2g):
        o = {}
        for nm, piece_scale, g in (("gs1", 1, n1g), ("gs2", 4, n2g)):
            t = self.sb("%s_%d" % (nm, L), [128, 8, 2], F32)
            for v in range(2):
                self.stt("dve", t[:, :, v], modT[:, piece_scale * 8:(piece_scale + 1) * 8, v], 1.0, g[:],
                         ALU.add, ALU.mult, r=["modT%d" % L, g.name if hasattr(g, "name") else "g"],
                         w=["%s_%d" % (nm, L)])
            o[nm] = t
        o["sh1"] = modT[:, 0:8, :]
        o["gate1"] = modT[:, 16:24, :]
        o["sh2"] = modT[:, 24:32, :]
        o["gate2"] = modT[:, 40:48, :]
        o["key"] = ["modT%d" % L, "gs1_%d" % L, "gs2_%d" % L]
        return o

    def norm_mod(self, src_fn, src_keys, dst, dst_key, gs, sh, modkeys, tmp):
        sq, rstd, tmpf = tmp
        for gi, (t0, n, v) in enumerate(self.groups):
            pb = self.bank[6]
            for k in range(KT):
                eng = ("act", "pool", "dve")[k % 3]
                src = src_fn(k, t0, n)
                if eng == "act":
                    self.act(sq[:, k, 0:n], src, AF.Square, r=src_keys(gi), w=["nm_sq%d" % k])
                else:
                    self.tt(eng, sq[:, k, 0:n], src, src, ALU.mult, r=src_keys(gi), w=["nm_sq%d" % k])
            self.mmk(pb[:, 0:n], [self.C["ones_bf"][:, :] for k in range(KT)],
                     [sq[:, k, 0:n] for k in range(KT)],
                     r=["nm_sq%d" % k for k in range(KT)] + ["C_ones_bf"], w=["bank6"])
            self.act(rstd[:, 0:n], pb[:, 0:n], AF.Sqrt, bias=self.epsc[:, 0:1], scale=1.0 / D, r=["bank6", "epsc"], w=["nm_rstd"])
            self.recip(rstd[:, 0:n], rstd[:, 0:n], r=["nm_rstd"], w=["nm_rstd"])
            for k in range(KT):
                src = src_fn(k, t0, n)
                tf = tmpf[k % 2]
                self.tt("dve" if k % 2 == 0 else "pool", tf[:, 0:n], src, rstd[:, 0:n], ALU.mult,
                        r=src_keys(gi) + ["nm_rstd"], w=["nm_tmp%d" % (k % 2)])
                self.act(dst[:, k, t0:t0 + n], tf[:, 0:n], AF.Identity, bias=sh[:, k, v:v + 1], scale=gs[:, k, v:v + 1],
                         r=["nm_tmp%d" % (k % 2)] + modkeys, w=["%s.g%d" % (dst_key, gi)])

    def headnorm(self, P, n, src_psum, src_key, blk, blk_key, gain_col, gain_key, out_bf, out_key,
                 rope=None, pbank=5, tmp=None):
        sqb, rs, qn, qnb, t1 = tmp
        pb = self.bank[pbank]
        bk = "bank%d" % pbank
        self.act(sqb[0:P, 0:n], src_psum, AF.Square, r=[src_key], w=["hn_sq"])
        self.mm(pb[0:P, 0:n], blk[0:P, 0:P], sqb[0:P, 0:n], True, True, r=["hn_sq", blk_key], w=[bk])
        self.act(rs[0:P, 0:n], pb[0:P, 0:n], AF.Sqrt, bias=self.epsc[0:P, 0:1], scale=1.0, r=[bk, "epsc"], w=["hn_rs"])
        self.recip(rs[0:P, 0:n], rs[0:P, 0:n], r=["hn_rs"], w=["hn_rs"])
        if rope is None:
            self.stt("dve", out_bf, src_psum, gain_col, rs[0:P, 0:n], ALU.mult, ALU.mult,
                     r=[src_key, gain_key, "hn_rs"], w=[out_key])
            return
        cos_ap, sin_ap, rsw, rkeys = rope
        self.stt("dve", qn[0:P, 0:n], src_psum, gain_col, rs[0:P, 0:n], ALU.mult, ALU.mult,
                 r=[src_key, gain_key, "hn_rs"], w=["hn_qn"])
        self.cp("act", qnb[0:P, 0:n], qn[0:P, 0:n], r=["hn_qn"], w=["hn_qnb"])
        self.mm(pb[0:P, 0:n], rsw[0:P, 0:P], qnb[0:P, 0:n], True, True, r=["hn_qnb"] + rkeys, w=[bk])
        self.tt("pool", t1[0:P, 0:n], qn[0:P, 0:n], cos_ap, ALU.mult, r=["hn_qn"] + rkeys, w=["hn_t1"])
        self.tt("dve", qn[0:P, 0:n], pb[0:P, 0:n], sin_ap, ALU.mult, r=[bk] + rkeys, w=["hn_qn"])
        self.tt("dve", out_bf, qn[0:P, 0:n], t1[0:P, 0:n], ALU.add, r=["hn_qn", "hn_t1"], w=[out_key])

    def attention(self, tagp, dq, scale, nheads, qsrc, ksegs_of_head, nq, out_tile, out_key, out_off, tmp):
        qbuf, kbuf, vbuf, pbuf, osb, rl, rlh, rll, atmp = tmp
        NQG = (nq + 511) // 512
        LA = 2
        its = []
        for h in range(nheads):
            segs = ksegs_of_head(h)
            nseg = len(segs)
            for si, (K_ap, V_ap, kkey, vkey) in enumerate(segs):
                nkb = K_ap.shape[1] // 128
                for qg in range(NQG):
                    for kb in range(nkb):
                        its.append((h, si, qg, kb, si == 0 and kb == 0, si == nseg - 1 and kb == nkb - 1))
        segctr = {}
        chunk_id = {}
        ctr = 0
        for (h, si, qg, kb, first, last) in its:
            if (h, si) not in chunk_id:
                chunk_id[(h, si)] = ctr
                ctr += 1
        loaded = set()
        qloaded = set()

        def load_q(h):
            if h in qloaded or h >= nheads:
                return
            qloaded.add(h)
            self.dma("sp", qbuf[h % 2][0:dq, 0:nq], qsrc(h), r=[tagp + "qsrc"], w=["%s_q%d" % (tagp, h % 2)])

        def load_chunk(h, si):
            if (h, si) in loaded:
                return
            loaded.add((h, si))
            segs = ksegs_of_head(h)
            K_ap, V_ap, kkey, vkey = segs[si]
            c = chunk_id[(h, si)]
            nk = K_ap.shape[1]
            self.dma("sp", kbuf[c % 3][0:dq, 0:nk], K_ap, r=[kkey], w=["%s_k%d" % (tagp, c % 3)])
            self.dma("sp", vbuf[c % 3][:, 0:nk // 128, :], V_ap, r=[vkey], w=["%s_v%d" % (tagp, c % 3)])

        order = sorted(chunk_id.items(), key=lambda kv: kv[1])
        nxt = {}
        for i in range(len(order) - 1):
            nxt[order[i][0]] = order[i + 1][0]
        n = len(its)
        for i in range(n + LA):
            if i < n:
                h, si, qg, kb, first, last = its[i]
                load_q(h)
                load_chunk(h, si)
                if (h, si) in nxt and qg == 0 and kb == 0:
                    nh, nsi = nxt[(h, si)]
                    load_q(nh)
                    load_chunk(nh, nsi)
                c = chunk_id[(h, si)]
                nqq = min(512, nq - qg * 512)
                sb_ = self.bank[i % 4]
                self.mm(sb_[:, 0:nqq], kbuf[c % 3][0:dq, kb * 128:(kb + 1) * 128],
                        qbuf[h % 2][0:dq, qg * 512:qg * 512 + nqq], True, True,
                        r=["%s_k%d" % (tagp, c % 3), "%s_q%d" % (tagp, h % 2)], w=["bank%d" % (i % 4)])
            j = i - LA
            if j >= 0:
                h, si, qg, kb, first, last = its[j]
                c = chunk_id[(h, si)]
                nqq = min(512, nq - qg * 512)
                self.act(pbuf[j % 3][:, 0:nqq], self.bank[j % 4][:, 0:nqq], AF.Exp, scale=scale,
                         r=["bank%d" % (j % 4)], w=["%s_p%d" % (tagp, j % 3)])
                ob = self.bank[4 + qg]
                self.mm(ob[0:65, 0:nqq], vbuf[c % 3][:, kb, :], pbuf[j % 3][:, 0:nqq], first, last,
                        r=["%s_v%d" % (tagp, c % 3), "%s_p%d" % (tagp, j % 3)], w=["bank%d" % (4 + qg)])
                if last:
                    bk = "bank%d" % (4 + qg)
                    self.cp("act", osb[0:65, 0:nqq], ob[0:65, 0:nqq], r=[bk], w=[tagp + "_osb"])
                    self.recip(rl[64:65, 0:nqq], osb[64:65, 0:nqq], r=[tagp + "_osb"], w=[tagp + "_rl"])
                    self.cp("dve", rlh[64:65, 0:nqq], rl[64:65, 0:nqq], r=[tagp + "_rl"], w=[tagp + "_rlh"])
                    self.tt("dve", rl[64:65, 0:nqq], rl[64:65, 0:nqq], rlh[64:65, 0:nqq], ALU.subtract,
                            r=[tagp + "_rl", tagp + "_rlh"], w=[tagp + "_rl"])
                    self.cp("dve", rll[64:65, 0:nqq], rl[64:65, 0:nqq], r=[tagp + "_rl"], w=[tagp + "_rll"])
                    self.mmk(ob[0:64, 0:nqq], [self.C["ones_bf"][64:65, 0:64], self.C["ones_bf"][64:65, 0:64]],
                             [rlh[64:65, 0:nqq], rll[64:65, 0:nqq]],
                             r=[tagp + "_rlh", tagp + "_rll", tagp + "_osb", "C_ones_bf"], w=[bk])
                    q0 = out_off + qg * 512
                    if h % 2 == 0:
                        self.tt("dve", out_tile[0:64, h // 2, q0:q0 + nqq], osb[0:64, 0:nqq], ob[0:64, 0:nqq], ALU.mult,
                                r=[bk, tagp + "_osb"], w=[out_key])
                    else:
                        self.tt("dve", atmp[0:64, 0:nqq], osb[0:64, 0:nqq], ob[0:64, 0:nqq], ALU.mult,
                                r=[bk, tagp + "_osb"], w=[tagp + "_atmp"])
                        self.cp("dve", out_tile[64:128, h // 2, q0:q0 + nqq], atmp[0:64, 0:nqq],
                                r=[tagp + "_atmp"], w=[out_key])


class Arena:
    def __init__(self, gen, nbytes):
        self.t = gen.sb("arena", [128, nbytes // 4], F32)
        self.n = nbytes
        self.off = 0
        self.peak = 0
        self.base = 0

    def reset(self):
        self.off = self.base

    def alloc(self, shape, dt):
        free = 1
        for s in shape[1:]:
            free *= s
        nb = free * (2 if dt == BF16 else 4)
        nb_al = (nb + 63) // 64 * 64
        assert self.off + nb_al <= self.n, ("arena overflow", self.off, nb_al, self.n)
        a = self.off // 4
        ap = self.t[:, a:a + nb_al // 4]
        self.off += nb_al
        self.peak = max(self.peak, self.off)
        if dt == BF16:
            ap = ap.bitcast(BF16)
        ap = ap[0:shape[0], 0:free]
        if len(shape) == 3:
            ap = ap.rearrange("p (a b) -> p a b", b=shape[2])
        elif len(shape) == 4:
            ap = ap.rearrange("p (a b c) -> p a b c", b=shape[2], c=shape[3])
        return ap


def _gen_finish(self):
    S = self.S
    outs = [o for o in S.ops if o.dma]
    S.pend_barrier["sp"] = S.pend_barrier["sp"] + outs + [o for o in S.last_op.values() if o is not None]
    S.add("sp", lambda e: None)
    S.finalize(self.nc)
    nc = self.nc
    with nc.Block() as block:
        @block.tensor
        def _(e):
            S.emit("pe", e)

        @block.scalar
        def _(e):
            S.emit("act", e)

        @block.vector
        def _(e):
            S.emit("dve", e)

        @block.gpsimd
        def _(e):
            S.emit("pool", e)

        @block.sync
        def _(e):
            S.emit("sp", e)


Gen.finish = _gen_finish


def _setup_common(self):
    g = self
    TL, T = g.TL, g.T
    g.load_consts()
    g.epsc = g.sb("epsc", [128, 1], F32)
    g.memset("pool", g.epsc[:], EPS, w=["epsc"])
    g.xT = g.sb("xT", [128, KT, TL], F32)
    g.cT = g.sb("cT", [128, KT, LC], F32)
    g.hT = g.sb("hT", [128, KT, T], BF16)
    g.cvec = g.sb("cvec_sb", [128, KT, 2], F32)
    g.scvec = g.sb("scvec_sb", [128, KT, 2], F32)
    xin = g.inp("xT_in", [128, KT, TL])
    cin = g.inp("cT_in", [128, KT, LC])
    cv = g.inp("cvec", [128, KT, 2])
    for k in range(KT):
        g.dma("sp", g.xT[:, k, :], xin[:, k, :], w=["xT.g%d" % i for i in range(len(g.groups) - 1)])
    g.dma("sp", g.cT[:], cin, w=["xT.g%d" % (len(g.groups) - 1)])
    g.dma("sp", g.cvec[:], cv, w=["cvec"])
    g.act(g.scvec[:], g.cvec[:], AF.Silu, r=["cvec"], w=["scvec"])
    rem = g.nc.sbuf_bytes_remaining
    g.ar = Arena(g, (rem - 11264) // 64 * 64)
    g.S.barrier()


def _res_src(self):
    g = self

    def src_fn(k, t0, n):
        if t0 >= g.TL:
            return g.cT[:, k, t0 - g.TL:t0 - g.TL + n]
        return g.xT[:, k, t0:t0 + n]
    return src_fn, (lambda gi: ["xT.g%d" % gi])


def _res_tile(self, k, t0, n):
    if t0 >= self.TL:
        return self.cT[:, k, t0 - self.TL:t0 - self.TL + n]
    return self.xT[:, k, t0:t0 + n]


def _layer0(self):
    g = self
    S = g.S
    TL, T, NCH = g.TL, g.T, g.NCH
    ar = g.ar
    W = {}
    for nm, shape in [("l0_ada_w", [D, 6 * D]), ("l0_ada_b", [6 * D]), ("l0_norm1_g", [D]), ("l0_norm2_g", [D]),
                      ("l0_w_in", [D, L0_IN]), ("l0_ret_log_decay", [2, 8]), ("l0_q_norm_g", [64]),
                      ("l0_k_norm_g", [64]), ("l0_w_out", [D, D]), ("l0_ffn_w1", [D, FFN]),
                      ("l0_ffn_w3", [D, FFN]), ("l0_ffn_w2", [FFN, D])]:
        W[nm] = g.inp(nm, shape)
    modT = g.ada(0, W["l0_ada_w"], W["l0_ada_b"], g.scvec)
    n1g = g.gain_cols("n1g0", W["l0_norm1_g"], 8)
    n2g = g.gain_cols("n2g0", W["l0_norm2_g"], 8)
    M = g.mod_derived(0, modT, n1g, n2g)
    gq = g.gain_cols("gq0", None, 1, pieces=[(0, 64, W["l0_q_norm_g"]), (64, 128, W["l0_q_norm_g"])])
    gk = g.gain_cols("gk0", None, 1, pieces=[(0, 64, W["l0_k_norm_g"]), (64, 128, W["l0_k_norm_g"])])
    lgb = g.sb("lgb", [128, 16], F32)
    g.dma("sp", lgb[:], W["l0_ret_log_decay"].rearrange("a b -> (a b)").partition_broadcast(128), w=["lgb"])
    lgp = g.sb("lgp", [128, 2, 4], F32)
    for dr in range(2):
        for half in range(2):
            src = lgb[half * 64:(half + 1) * 64, dr * 8 + half:dr * 8 + 8:2]
            g.cp("dve", lgp[half * 64:(half + 1) * 64, dr, :], src, r=["lgb"], w=["lgp"])
    GP = g.sb("GP", [128, 2, 4], F32)
    g.act(GP[:], lgp[:], AF.Exp, scale=128.0, r=["lgp"], w=["GP"])

    ar.reset()
    sq = ar.alloc([128, KT, 512], BF16)
    rstd = ar.alloc([128, 512], F32)
    tmpf = [ar.alloc([128, 512], F32) for _ in range(2)]
    src_fn, src_keys = g.res_src()
    g.norm_mod(src_fn, src_keys, g.hT, "hT", M["gs1"], M["sh1"], M["key"], (sq, rstd, tmpf))
    S.barrier()

    ar.reset()
    hkeys = ["hT.g%d" % i for i in range(len(g.groups))]
    wq = ar.alloc([128, KT, 512], BF16)
    wkv = ar.alloc([128, KT, 256], BF16)
    win = W["l0_w_in"].rearrange("(k p) c -> p k c", p=128)
    g.dma("pool", wq, win[:, :, 2048:2560], w=["wq"])
    g.dma("pool", wkv, win[:, :, 2560:2816], w=["wkv"])
    cosb = ar.alloc([128, 512], F32)
    sinb = ar.alloc([128, 512], F32)
    hn_tmp = (ar.alloc([128, 512], BF16), ar.alloc([128, 512], F32), ar.alloc([128, 512], F32),
              ar.alloc([128, 512], BF16), ar.alloc([128, 512], F32))
    obuf = [ar.alloc([128, 512], BF16) for _ in range(2)]
    vaug = [ar.alloc([128, 2, 65], BF16) for _ in range(2)]
    for i in range(2):
        g.memset("pool", vaug[i], 1.0, w=["vaug%d" % i])
    g.Q0 = g.scratch("Q0", [8, 64, T], BF16)
    if g.mode == "pre0":
        g.K0own = g.outp("K0own", [2, 64, TL], BF16)
        g.V0own = g.outp("V0own", [2, 128, NCH, 65], BF16)
        g.K0ctx = g.outp("K0ctx", [2, 64, LC], BF16)
        g.V0ctx = g.outp("V0ctx", [2, 128, LC // 128, 65], BF16)
    else:
        g.K0own = g.scratch("K0own", [2, 64, TL], BF16)
        g.V0own = g.scratch("V0own", [2, 128, NCH, 65], BF16)
        g.K0ctx = g.scratch("K0ctx", [2, 64, LC], BF16)
        g.V0ctx = g.scratch("V0ctx", [2, 128, LC // 128, 65], BF16)
    cnt = 0
    for gi, (t0, n, v) in enumerate(g.groups):
        rope = None
        if v == 0:
            g.dma("sp", cosb[:, 0:n], g.C_dram["cos0"][:, t0:t0 + n], w=["cosb"])
            g.dma("sp", sinb[:, 0:n], g.C_dram["sin0"][:, t0:t0 + n], w=["sinb"])
            rope = (cosb[0:64, 0:n], sinb[0:64, 0:n], g.C["rsw"], ["cosb", "sinb", "C_rsw"])
        for hh in range(10):
            pb = g.bank[hh % 2]
            bk = "bank%d" % (hh % 2)
            if hh < 2:
                wsl = [wkv[:, k, hh * 64:(hh + 1) * 64] for k in range(KT)]
                wk_ = "wkv"
                gain, gkey = gk, "gk0"
            else:
                wsl = [wq[:, k, (hh - 2) * 64:(hh - 1) * 64] for k in range(KT)]
                wk_ = "wq"
                gain, gkey = gq, "gq0"
            g.mmk(pb[0:64, 0:n], wsl, [g.hT[:, k, t0:t0 + n] for k in range(KT)], r=[wk_, "hT.g%d" % gi], w=[bk])
            ob = obuf[cnt % 2]
            okey = "obuf%d" % (cnt % 2)
            cnt += 1
            g.headnorm(64, n, pb[0:64, 0:n], bk, g.C["bo64"], "C_bo64", gain[0:64, 0:1], gkey, ob[0:64, 0:n], okey,
                       rope=rope, pbank=2, tmp=hn_tmp)
            if hh < 2:
                dst = g.K0own[hh, :, t0:t0 + n] if v == 0 else g.K0ctx[hh, :, :]
                g.dma("sp", dst, ob[0:64, 0:n], r=[okey], w=["K0own" if v == 0 else "K0ctx"])
            else:
                g.dma("sp", g.Q0[hh - 2, :, t0:t0 + n], ob[0:64, 0:n], r=[okey], w=["Q0"])
        for tt_ in range(n // 128):
            c0 = t0 + tt_ * 128
            pb = g.bank[3]
            g.mmk(pb[:, 0:128], [g.hT[:, k, c0:c0 + 128] for k in range(KT)], [wkv[:, k, 128:256] for k in range(KT)],
                  r=["wkv", "hT.g%d" % gi], w=["bank3"])
            va = vaug[tt_ % 2]
            g.cp("act", va[:, :, 0:64], pb[:, 0:128].rearrange("p (a b) -> p a b", b=64), r=["bank3"], w=["vaug%d" % (tt_ % 2)])
            for kvh in range(2):
                if v == 0:
                    dst = g.V0own[kvh, :, c0 // 128, :]
                else:
                    dst = g.V0ctx[kvh, :, (c0 - TL) // 128, :]
                g.dma("sp", dst, va[:, kvh, :], r=["vaug%d" % (tt_ % 2)], w=["V0own" if v == 0 else "V0ctx"])
    S.barrier()

    ar.base = 0
    ar.reset()
    g.raT = ar.alloc([128, 4, T], BF16)
    ar.base = ar.off
    ub_store = ar.alloc([128, 4, NCH + 2, 128], BF16)
    wrp = [ar.alloc([128, KT, 512], BF16) for _ in range(2)]
    ZF = ar.alloc([128, 2, 128], F32)
    kz = [ar.alloc([128, 128], BF16) for _ in range(2)]
    vb = ar.alloc([128, 128], BF16)
    Uacc = ar.alloc([128, 2, 4, 128], F32)
    sctx = ar.alloc([128, 2, 4, 128], F32)
    ut = ar.alloc([128, 2, 128], F32)
    posf = ar.alloc([128, 64], F32)
    posb = ar.alloc([128, 64], F32)
    g.dma("sp", posf, g.C_dram["posf"], w=["posf"])
    g.dma("sp", posb, g.C_dram["posb"], w=["posb"])
    g.memset("pool", Uacc, 0.0, w=["Uacc"])
    g.memset("pool", sctx, 0.0, w=["sctx"])
    if g.mode == "pre0":
        g.Uown = g.outp("Uown", [128, 2, 4, 128], F32)
    else:
        g.Uown = g.scratch("Uown", [128, 2, 4, 128], F32)

    def load_wrp(p):
        wb = wrp[p % 2]
        for qi in range(4):
            g.dma("pool", wb[:, :, qi * 128:(qi + 1) * 128], win[:, :, qi * 512 + p * 128:qi * 512 + (p + 1) * 128],
                  w=["wrp%d_%d" % (p % 2, qi)])
    g.load_wrp = load_wrp
    chunks = [(j * 128, j, 0) for j in range(NCH)] + [(TL + j * 128, NCH + j, 1) for j in range(2)]
    g.chunks = chunks
    load_wrp(0)
    for p in range(4):
        if p + 1 < 4:
            load_wrp(p + 1)
        wb = wrp[p % 2]
        wk_ = "wrp%d" % (p % 2)
        for dr, pos, pkey in ((0, posf, "posf"), (1, posb, "posb")):
            for half in range(2):
                g.act(ZF[:, dr, half * 64:(half + 1) * 64], pos[:, :], AF.Exp,
                      scale=lgb[:, dr * 8 + 2 * p + half:dr * 8 + 2 * p + half + 1], r=[pkey, "lgb"], w=["ZF"])
        g.ts("dve", ZF[:, :, :], ZF[:, :, :], 0.125, None, ALU.mult, r=["ZF"], w=["ZF"])
        for (c0, j, isctx) in chunks:
            gi = min(c0 // 512, len(g.groups) - 1) if not isctx else len(g.groups) - 1
            hk = "hT.g%d" % gi
            kp = g.bank[0]
            vp = g.bank[1]
            g.mmk(kp[:, 0:128], [g.hT[:, k, c0:c0 + 128] for k in range(KT)], [wb[:, k, 128:256] for k in range(KT)],
                  r=[wk_ + "_1", hk], w=["bank0"])
            g.mmk(vp[:, 0:128], [g.hT[:, k, c0:c0 + 128] for k in range(KT)], [wb[:, k, 256:384] for k in range(KT)],
                  r=[wk_ + "_2", hk], w=["bank1"])
            g.tt("dve", kz[0][:, :], kp[:, 0:128], ZF[:, 0, :], ALU.mult, r=["bank0", "ZF"], w=["kz0"])
            g.tt("dve", kz[1][:, :], kp[:, 0:128], ZF[:, 1, :], ALU.mult, r=["bank0", "ZF"], w=["kz1"])
            g.cp("act", vb[:, :], vp[:, 0:128], r=["bank1"], w=["vb"])
            up = g.bank[2]
            for dr in range(2):
                g.mm(up[:, dr * 128:(dr + 1) * 128], kz[dr][:, :], vb[:, :], True, True, r=["kz%d" % dr, "vb"], w=["bank2"])
            for dr in range(2):
                g.tt("dve", ut[:, dr, :], up[:, dr * 128:(dr + 1) * 128], g.C["bdmask"][:, :], ALU.mult,
                     r=["bank2", "C_bdmask"], w=["ut"])
            g.cp("pool", ub_store[:, p, j, :], ut[:, 1, :], r=["ut"], w=["ub_store"])
            acc = sctx if isctx else Uacc
            akey = "sctx" if isctx else "Uacc"
            g.stt("dve", acc[:, 0, p, :], acc[:, 0, p, :], GP[:, 0, p:p + 1], ut[:, 0, :], ALU.mult, ALU.add,
                  r=[akey, "GP", "ut"], w=[akey])
        for (lo, hi, acc, akey) in ((0, NCH, Uacc, "Uacc"), (NCH, NCH + 2, sctx, "sctx")):
            for j in range(hi - 1, lo - 1, -1):
                g.stt("dve", acc[:, 1, p, :], acc[:, 1, p, :], GP[:, 1, p:p + 1], ub_store[:, p, j, :], ALU.mult, ALU.add,
                      r=[akey, "GP", "ub_store"], w=[akey])
    g.dma("sp", g.Uown, Uacc, r=["Uacc"], w=["Uown"])
    g.ub_store, g.sctx, g.lgb, g.lgp, g.GP, g.M0, g.W0 = ub_store, sctx, lgb, lgp, GP, M, W
    g.wrp = wrp
    g.ar_mark = ar.off


Gen.setup_common = _setup_common
Gen.res_src = _res_src
Gen.res_tile = _res_tile
Gen.layer0_pre = _layer0


def _layer0_main(self, stop=None):
    g = self
    S = g.S
    TL, T, NCH = g.TL, g.T, g.NCH
    ar = g.ar
    M, W = g.M0, g.W0
    lgb, lgp, GP = g.lgb, g.lgp, g.GP
    ub_store, sctx = g.ub_store, g.sctx
    wrp = g.wrp
    if g.mode == "fused":
        raise NotImplementedError
    else:
        g.K0all = g.inp("K0all", [2, NCORES, 64, TL], BF16)
        g.V0all = g.inp("V0all", [2, NCORES, 128, NCH, 65], BF16)
        g.Uall = g.inp("Uall", [NCORES, 128, 2, 4, 128], F32)
    S.barrier()
    ar.off = g.ar_mark
    Sin = ar.alloc([128, 2, 4, 128], F32)
    coef = ar.alloc([128, 2, 4, NCORES], F32)
    coefc = ar.alloc([128, 2, 4], F32)
    ex = ar.alloc([128, 4], F32)
    utmp = [ar.alloc([128, 2, 4, 128], F32) for _ in range(1)]
    cpos = g.C["cpos"]
    for dr in range(2):
        for r_ in range(NCORES):
            g.ts("dve", ex[:, :], lgp[:, dr, :], cpos[:, dr * 16 + r_:dr * 16 + r_ + 1], None, ALU.mult,
                 r=["lgp", "C_cpos"], w=["ex"])
            g.act(ex[:, :], ex[:, :], AF.Exp, r=["ex"], w=["ex"])
            g.ts("dve", coef[:, dr, :, r_], ex[:, :], cpos[:, dr * 16 + 8 + r_:dr * 16 + 9 + r_], None, ALU.mult,
                 r=["ex", "C_cpos"], w=["coef"])
        g.ts("dve", ex[:, :], lgp[:, dr, :], cpos[:, 32 + dr:33 + dr], None, ALU.mult, r=["lgp", "C_cpos"], w=["ex"])
        g.act(coefc[:, dr, :], ex[:, :], AF.Exp, r=["ex"], w=["coefc"])
        for p in range(4):
            g.ts("dve", Sin[:, dr, p, :], sctx[:, dr, p, :], coefc[:, dr, p:p + 1], None, ALU.mult,
                 r=["sctx", "coefc"], w=["Sin"])
    for r_ in range(NCORES):
        ub = utmp[0]
        g.dma("sp", ub, g.Uall[r_], w=["utmp0"])
        for dr in range(2):
            for p in range(4):
                g.stt("dve", Sin[:, dr, p, :], ub[:, dr, p, :], coef[:, dr, p, r_:r_ + 1], Sin[:, dr, p, :],
                      ALU.mult, ALU.add, r=["utmp0", "coef", "Sin"], w=["Sin"])
    Rb = ar.alloc([128, 128], F32)
    tf = ar.alloc([128, 128], F32)
    for p in range(4):
        for (lo, hi, init) in ((0, NCH, True), (NCH, NCH + 2, False)):
            if init:
                g.cp("dve", Rb[:, :], Sin[:, 1, p, :], r=["Sin"], w=["Rb"])
            else:
                g.memset("dve", Rb[:, :], 0.0, w=["Rb"])
            for j in range(hi - 1, lo - 1, -1):
                g.cp("dve", tf[:, :], ub_store[:, p, j, :], r=["ub_store"], w=["tf"])
                g.cp("dve", ub_store[:, p, j, :], Rb[:, :], r=["Rb", "tf"], w=["ub_store"])
                g.stt("dve", Rb[:, :], Rb[:, :], GP[:, 1, p:p + 1], tf[:, :], ALU.mult, ALU.add,
                      r=["Rb", "tf", "GP", "ub_store"], w=["Rb"])
    if stop == "4a":
        return
    ramp = [ar.alloc([128, 128], F32) for _ in range(2)]
    rel = [ar.alloc([128, 128], F32) for _ in range(2)]
    ind = [ar.alloc([128, 128], F32) for _ in range(2)]
    for i, (a, b, c) in enumerate((("rampf", "relf", "indf"), ("rampb", "relb", "indb"))):
        g.dma("sp", ramp[i], g.C_dram[a], w=["ramp%d" % i])
        g.dma("sp", rel[i], g.C_dram[b], w=["rel%d" % i])
        g.dma("sp", ind[i], g.C_dram[c], w=["ind%d" % i])
    posf = ar.alloc([128, 64], F32)
    g.dma("sp", posf, g.C_dram["posf"], w=["posf2"])
    XI = ar.alloc([128, 2, 128], F32)
    MASK = ar.alloc([128, 2, 128], F32)
    mt = ar.alloc([128, 128], F32)
    ZFm = ar.alloc([128, 128], F32)
    qTb = ar.alloc([128, 128], BF16)
    qx = [ar.alloc([128, 128], BF16) for _ in range(2)]
    kTb = ar.alloc([128, 128], BF16)
    sg = ar.alloc([128, 128], F32)
    kzf = ar.alloc([128, 128], BF16)
    vbb = ar.alloc([128, 128], BF16)
    vpad = ar.alloc([128, 2, 128], BF16)
    ATb = ar.alloc([128, 256], BF16)
    Sf = ar.alloc([128, 128], F32)
    Sfb = [ar.alloc([128, 128], BF16) for _ in range(2)]
    of = ar.alloc([128, 128], F32)
    obf = ar.alloc([128, 128], BF16)
    cen = ar.alloc([128, 128], F32)
    sqc = ar.alloc([128, 128], BF16)
    rs = ar.alloc([128, 128], F32)
    yy = ar.alloc([128, 128], F32)
    g.memset("pool", vpad, 0.0, w=["vpad"])
    bo = g.C["bo64"]
    it = 0
    for p in range(4):
        g.load_wrp(p)
        wb = wrp[p % 2]
        wk = ["wrp%d_%d" % (p % 2, qi) for qi in range(4)]
        for dr in range(2):
            g.act(XI[:, dr, :], ramp[dr][:, :], AF.Exp, scale=lgp[:, dr, p:p + 1], r=["ramp%d" % dr, "lgp"], w=["XI"])
        for half in range(2):
            hcol = 2 * p + half
            g.act(MASK[:, half, :], rel[0][:, :], AF.Exp, scale=lgb[:, hcol:hcol + 1], r=["rel0", "lgb"], w=["MASK"])
            g.tt("dve", MASK[:, half, :], MASK[:, half, :], ind[0][:, :], ALU.mult, r=["MASK", "ind0"], w=["MASK"])
            g.act(mt[:, :], rel[1][:, :], AF.Exp, scale=lgb[:, 8 + hcol:9 + hcol], r=["rel1", "lgb"], w=["mt"])
            g.tt("dve", mt[:, :], mt[:, :], ind[1][:, :], ALU.mult, r=["mt", "ind1"], w=["mt"])
            g.tt("dve", MASK[:, half, :], MASK[:, half, :], mt[:, :], ALU.add, r=["MASK", "mt"], w=["MASK"])
            g.act(ZFm[:, half * 64:(half + 1) * 64], posf[:, :], AF.Exp, scale=lgb[:, hcol:hcol + 1],
                  r=["posf2", "lgb"], w=["ZFm"])
        g.ts("dve", ZFm[:, :], ZFm[:, :], 0.125, None, ALU.mult, r=["ZFm"], w=["ZFm"])
        for (c0, j, isctx) in g.chunks:
            gi = min(c0 // 512, len(g.groups) - 1) if not isctx else len(g.groups) - 1
            hk = "hT.g%d" % gi
            if j == 0:
                g.cp("dve", Sf[:, :], Sin[:, 0, p, :], r=["Sin"], w=["Sf"])
            if j == NCH:
                g.memset("dve", Sf[:, :], 0.0, w=["Sf"])
            sfb = Sfb[it % 2]
            sfk = "Sfb%d" % (it % 2)
            it += 1
            g.cp("act", sfb[:, :], Sf[:, :], r=["Sf"], w=[sfk])
            hsl = [g.hT[:, k, c0:c0 + 128] for k in range(KT)]
            b0, b1, b2, b3, b4 = g.bank[0], g.bank[1], g.bank[2], g.bank[3], g.bank[4]
            b5, b6, b7 = g.bank[5], g.bank[6], g.bank[7]
            g.mmk(b0[:, 0:128], [wb[:, k, 0:128] for k in range(KT)], hsl, r=[wk[0], hk], w=["bank0"])
            g.mmk(b5[:, 0:128], [wb[:, k, 128:256] for k in range(KT)], hsl, r=[wk[1], hk], w=["bank5"])
            g.mmk(b5[:, 128:256], [wb[:, k, 384:512] for k in range(KT)], hsl, r=[wk[3], hk], w=["bank5"])
            g.mmk(b1[:, 0:128], hsl, [wb[:, k, 128:256] for k in range(KT)], r=[wk[1], hk], w=["bank1"])
            g.mmk(b6[:, 0:128], hsl, [wb[:, k, 256:384] for k in range(KT)], r=[wk[2], hk], w=["bank6"])
            g.cp("dve", qTb[:, :], b0[:, 0:128], r=["bank0"], w=["qTb"])
            for dr in range(2):
                g.tt("dve", qx[dr][:, :], b0[:, 0:128], XI[:, dr, :], ALU.mult, r=["bank0", "XI"], w=["qx%d" % dr])
            g.act(kTb[:, :], b5[:, 0:128], AF.Identity, scale=0.125, r=["bank5"], w=["kTb"])
            g.act(sg[:, :], b5[:, 128:256], AF.Silu, r=["bank5"], w=["sg"])
            g.tt("dve", kzf[:, :], b1[:, 0:128], ZFm[:, :], ALU.mult, r=["bank1", "ZFm"], w=["kzf"])
            g.cp("act", vbb[:, :], b6[:, 0:128], r=["bank6"], w=["vbb"])
            g.cp("pool", vpad[:, 0, 0:64], vbb[:, 0:64], r=["vbb"], w=["vpad"])
            g.cp("pool", vpad[:, 1, 64:128], vbb[:, 64:128], r=["vbb"], w=["vpad"])
            g.mm(b2[:, 0:128], kTb[0:64, :], qTb[0:64, :], True, True, r=["kTb", "qTb"], w=["bank2"])
            g.mm(b7[:, 0:128], kTb[64:128, :], qTb[64:128, :], True, True, r=["kTb", "qTb"], w=["bank7"])
            g.tt("dve", ATb[:, 0:128], b2[:, 0:128], MASK[:, 0, :], ALU.mult, r=["bank2", "MASK"], w=["ATb"])
            g.tt("dve", ATb[:, 128:256], b7[:, 0:128], MASK[:, 1, :], ALU.mult, r=["bank7", "MASK"], w=["ATb"])
            g.mm(b3[:, 0:128], vpad[:, 0, :], ATb[:, 0:128], True, False, r=["vpad", "ATb"], w=["bank3"])
            g.mm(b3[:, 0:128], vpad[:, 1, :], ATb[:, 128:256], False, False, r=["vpad", "ATb"], w=["bank3"])
            g.mm(b3[:, 0:128], sfb[:, :], qx[0][:, :], False, False, r=[sfk, "qx0"], w=["bank3"])
            g.mm(b3[:, 0:128], ub_store[:, p, j, :], qx[1][:, :], False, True, r=["ub_store", "qx1"], w=["bank3"])
            g.mm(b2[:, 256:384], kzf[:, :], vbb[:, :], True, True, r=["kzf", "vbb", "ATb"], w=["bank2"])
            g.tt("dve", mt[:, :], b2[:, 256:384], g.C["bdmask"][:, :], ALU.mult, r=["bank2", "C_bdmask"], w=["mt"])
            g.stt("dve", Sf[:, :], Sf[:, :], GP[:, 0, p:p + 1], mt[:, :], ALU.mult, ALU.add, r=["Sf", "mt", "GP", sfk], w=["Sf"])
            g.cp("act", of[:, :], b3[:, 0:128], r=["bank3"], w=["of"])
            g.cp("pool", obf[:, :], of[:, :], r=["of"], w=["obf"])
            g.mm(b4[:, 0:128], bo[:, :], obf[:, :], True, True, r=["obf", "C_bo64"], w=["bank4"])
            g.tt("dve", cen[:, :], of[:, :], b4[:, 0:128], ALU.subtract, r=["of", "bank4"], w=["cen"])
            g.tt("pool", sqc[:, :], cen[:, :], cen[:, :], ALU.mult, r=["cen"], w=["sqc"])
            g.mm(b4[:, 128:256], bo[:, :], sqc[:, :], True, True, r=["sqc", "C_bo64", "cen"], w=["bank4"])
            g.act(rs[:, :], b4[:, 128:256], AF.Sqrt, bias=g.epsc[:, 0:1], r=["bank4", "epsc"], w=["rs"])
            g.recip(rs[:, :], rs[:, :], r=["rs"], w=["rs"])
            g.tt("dve", yy[:, :], cen[:, :], rs[:, :], ALU.mult, r=["cen", "rs"], w=["yy"])
            g.tt("pool", g.raT[:, p, c0:c0 + 128], yy[:, :], sg[:, :], ALU.mult, r=["yy", "sg"], w=["raT"])
    S.barrier()
    if stop == "4b":
        return

    ar.reset()
    g.atT = ar.alloc([128, 4, T], BF16)
    ar.base = ar.off
    CK = min(1024, TL)
    tmp = ([ar.alloc([128, T], BF16) for _ in range(2)], [ar.alloc([128, CK], BF16) for _ in range(3)],
           [ar.alloc([128, CK // 128, 65], BF16) for _ in range(3)], [ar.alloc([128, 512], BF16) for _ in range(3)],
           ar.alloc([128, 512], F32), ar.alloc([128, 512], F32), ar.alloc([128, 512], BF16),
           ar.alloc([128, 512], BF16), ar.alloc([128, 512], BF16))

    def ksegs_lat(h):
        kvh = h // 4
        segs = []
        for r_ in range(NCORES):
            for c in range(TL // CK):
                segs.append((g.K0all[kvh, r_, :, c * CK:(c + 1) * CK],
                             g.V0all[kvh, r_, :, c * CK // 128:(c + 1) * CK // 128, :], "K0all", "V0all"))
        segs.append((g.K0ctx[kvh], g.V0ctx[kvh], "K0ctx", "V0ctx"))
        return segs

    g.attention("a0", 64, 0.125, 8, lambda h: g.Q0[h, :, 0:TL], ksegs_lat, TL, g.atT, "atT", 0, tmp)
    g.attention("a0", 64, 0.125, 8, lambda h: g.Q0[h, :, TL:T],
                lambda h: [(g.K0ctx[h // 4], g.V0ctx[h // 4], "K0ctx", "V0ctx")], LC, g.atT, "atT", TL, tmp)
    S.barrier()
    if stop == "5":
        return

    ar.reset()
    wo = ar.alloc([128, KT, D], BF16)
    g.dma("pool", wo, W["l0_w_out"].rearrange("(k p) c -> p k c", p=128), w=["wo"])
    for gi, (t0, n, v) in enumerate(g.groups):
        cat = [g.raT[:, kt, t0:t0 + n] for kt in range(4)] + [g.atT[:, kt, t0:t0 + n] for kt in range(4)]
        for dc in range(8):
            pb = g.bank[dc % 4]
            bk = "bank%d" % (dc % 4)
            g.mmk(pb[:, 0:n], [wo[:, kt, dc * 128:(dc + 1) * 128] for kt in range(8)], cat, r=["wo", "raT", "atT"], w=[bk])
            res = g.res_tile(dc, t0, n)
            g.stt("dve", res, pb[:, 0:n], M["gate1"][:, dc, v:v + 1], res, ALU.mult, ALU.add,
                  r=[bk, "xT.g%d" % gi] + M["key"], w=["xT.g%d" % gi])
    S.barrier()
    if stop == "6":
        return

    ar.reset()
    sq = ar.alloc([128, KT, 512], BF16)
    rstd = ar.alloc([128, 512], F32)
    tmpf = [ar.alloc([128, 512], F32) for _ in range(2)]
    src_fn, src_keys = g.res_src()
    g.norm_mod(src_fn, src_keys, g.hT, "hT", M["gs2"], M["sh2"], M["key"], (sq, rstd, tmpf))
    FC = 256
    NFC = FFN // FC
    w1v = W["l0_ffn_w1"].rearrange("(k p) f -> p k f", p=128)
    w3v = W["l0_ffn_w3"].rearrange("(k p) f -> p k f", p=128)
    w2v = W["l0_ffn_w2"].rearrange("(ft p) d -> p ft d", p=128)
    w1c = [ar.alloc([128, KT, FC], BF16) for _ in range(2)]
    w3c = [ar.alloc([128, KT, FC], BF16) for _ in range(2)]
    w2c = [ar.alloc([128, FC // 128, D], BF16) for _ in range(2)]
    gT = [ar.alloc([128, FC // 128, 512], BF16) for _ in range(2)]
    sil = [ar.alloc([128, 512], F32) for _ in range(2)]

    def load_fc(fc):
        i = fc % 2
        g.dma("pool", w1c[i], w1v[:, :, fc * FC:(fc + 1) * FC], w=["w1c%d" % i])
        g.dma("pool", w3c[i], w3v[:, :, fc * FC:(fc + 1) * FC], w=["w3c%d" % i])
        g.dma("pool", w2c[i], w2v[:, fc * (FC // 128):(fc + 1) * (FC // 128), :], w=["w2c%d" % i])
    load_fc(0)
    cnt = 0
    for fc in range(NFC):
        if fc + 1 < NFC:
            load_fc(fc + 1)
        i = fc % 2
        for gi, (t0, n, v) in enumerate(g.groups):
            hk = "hT.g%d" % gi
            gt = gT[cnt % 2]
            gk_ = "gT%d" % (cnt % 2)
            cnt += 1
            hsl = [g.hT[:, k, t0:t0 + n] for k in range(KT)]
            for ft in range(FC // 128):
                h1, h3 = g.bank[2 * ft], g.bank[2 * ft + 1]
                g.mmk(h1[:, 0:n], [w1c[i][:, k, ft * 128:(ft + 1) * 128] for k in range(KT)], hsl,
                      r=["w1c%d" % i, hk], w=["bank%d" % (2 * ft)])
                g.mmk(h3[:, 0:n], [w3c[i][:, k, ft * 128:(ft + 1) * 128] for k in range(KT)], hsl,
                      r=["w3c%d" % i, hk], w=["bank%d" % (2 * ft + 1)])
                g.act(sil[ft][:, 0:n], h1[:, 0:n], AF.Silu, r=["bank%d" % (2 * ft)], w=["sil%d" % ft])
                g.tt("dve", gt[:, ft, 0:n], sil[ft][:, 0:n], h3[:, 0:n], ALU.mult,
                     r=["sil%d" % ft, "bank%d" % (2 * ft + 1)], w=[gk_])
            for dc in range(8):
                pb = g.bank[4 + dc % 4]
                bk = "bank%d" % (4 + dc % 4)
                g.mmk(pb[:, 0:n], [w2c[i][:, ft, dc * 128:(dc + 1) * 128] for ft in range(FC // 128)],
                      [gt[:, ft, 0:n] for ft in range(FC // 128)], r=["w2c%d" % i, gk_], w=[bk])
                res = g.res_tile(dc, t0, n)
                g.stt("dve", res, pb[:, 0:n], M["gate2"][:, dc, v:v + 1], res, ALU.mult, ALU.add,
                      r=[bk, "xT.g%d" % gi] + M["key"], w=["xT.g%d" % gi])
    S.barrier()
    ar.base = 0
    ar.reset()


def _out_layer0(self):
    g = self
    xo = g.outp("xT_out", [128, KT, g.TL])
    co = g.outp("cT_out", [128, KT, LC])
    for k in range(KT):
        g.dma("sp", xo[:, k, :], g.xT[:, k, :], r=["xT.g%d" % i for i in range(len(g.groups))])
    g.dma("sp", co, g.cT[:], r=["xT.g%d" % (len(g.groups) - 1)])


Gen.layer0_main = _layer0_main
Gen.out_layer0 = _out_layer0


def _to_fm(a, n):
    return np.ascontiguousarray(a.reshape(n, KT, 128).transpose(2, 1, 0))


def _from_fm(a):
    n = a.shape[2]
    return np.ascontiguousarray(a.transpose(2, 1, 0).reshape(n, D))


def core_inputs(inputs, TL, core, xT=None, cT=None):
    m = {}
    for k, v in host_consts(core, TL).items():
        m["c_" + k] = v
    x = inputs["x"][0]
    if xT is None:
        m["xT_in"] = _to_fm(x[core * TL:(core + 1) * TL], TL)
        m["cT_in"] = _to_fm(inputs["ctx"][0], LC)
    else:
        m["xT_in"], m["cT_in"] = xT, cT
    cv = np.stack([inputs["c"][0], inputs["c_ctx"]], axis=-1)
    m["cvec"] = np.ascontiguousarray(cv.reshape(KT, 128, 2).transpose(1, 0, 2))
    return m


def run_prog(g, in_maps):
    names = set(g.din.keys())
    maps = [{k: v for k, v in m.items() if k in names} for m in in_maps]
    for m in maps:
        missing = names - set(m.keys())
        assert not missing, missing
    res = run_bass_kernel_spmd(g.nc, maps, core_ids=list(range(NCORES)))
    return res.results


def build_l0(TL, mode, stop=None):
    g = Gen(TL, mode)
    g.setup_common()
    g.layer0_pre()
    if mode == "main0":
        g.layer0_main(stop)
        g.out_layer0()
    g.finish()
    return g


def run_layer0(inputs, TL, stop=None):
    L0 = [k for k in inputs if k.startswith("l0_")]
    base = []
    for c in range(NCORES):
        m = core_inputs(inputs, TL, c)
        for k in L0:
            m[k] = inputs[k]
        base.append(m)
    g1 = build_l0(TL, "pre0")
    r1 = run_prog(g1, base)
    K0all = np.ascontiguousarray(np.stack([r["K0own"] for r in r1], axis=1))
    V0all = np.ascontiguousarray(np.stack([r["V0own"] for r in r1], axis=1))
    Uall = np.ascontiguousarray(np.stack([r["Uown"] for r in r1], axis=0))
    for m in base:
        m["K0all"], m["V0all"], m["Uall"] = K0all, V0all, Uall
    g2 = build_l0(TL, "main0", stop)
    r2 = run_prog(g2, base)
    return [r["xT_out"] for r in r2], [r["cT_out"] for r in r2]


def _layer1_pre(self):
    g = self
    S = g.S
    TL, T, NCH = g.TL, g.T, g.NCH
    ar = g.ar
    W = {}
    for nm, shape in [("l1_ada_w", [D, 6 * D]), ("l1_ada_b", [6 * D]), ("l1_norm1_g", [D]), ("l1_norm2_g", [D]),
                      ("l1_w_in", [D, L1_IN]), ("l1_q_lora_g", [384]), ("l1_kv_lora_g", [256]),
                      ("l1_w_uq", [384, 768]), ("l1_w_ukv", [256, 1024]), ("l1_q_nope_g", [64]),
                      ("l1_q_rope_g", [32]), ("l1_k_nope_g", [64]), ("l1_k_rope_g", [32]),
                      ("l1_w_out", [512, D]), ("l1_router", [D, NE]), ("l1_exp_w1", [NE, D, EDIM]),
                      ("l1_exp_w3", [NE, D, EDIM]), ("l1_exp_w2", [NE, EDIM, D])]:
        if g.mode == "pre1" and nm in ("l1_w_out", "l1_router", "l1_exp_w1", "l1_exp_w3", "l1_exp_w2"):
            continue
        W[nm] = g.inp(nm, shape)
    g.W1 = W
    modT = g.ada(1, W["l1_ada_w"], W["l1_ada_b"], g.scvec)
    n1g = g.gain_cols("n1g1", W["l1_norm1_g"], 8)
    n2g = g.gain_cols("n2g1", W["l1_norm2_g"], 8)
    M = g.mod_derived(1, modT, n1g, n2g)
    g.M1 = M
    qlg = g.gain_cols("qlg", W["l1_q_lora_g"], 3)
    kvlg = g.gain_cols("kvlg", W["l1_kv_lora_g"], 2)
    gq1 = g.gain_cols("gq1", None, 1, pieces=[(0, 64, W["l1_q_nope_g"]), (64, 96, W["l1_q_rope_g"])])
    gk1 = g.gain_cols("gk1", None, 1, pieces=[(0, 64, W["l1_k_nope_g"]), (64, 96, W["l1_k_rope_g"])])

    ar.reset()
    sq = ar.alloc([128, KT, 512], BF16)
    rstd = ar.alloc([128, 512], F32)
    tmpf = [ar.alloc([128, 512], F32) for _ in range(2)]
    src_fn, src_keys = g.res_src()
    g.norm_mod(src_fn, src_keys, g.hT, "hT", M["gs1"], M["sh1"], M["key"], (sq, rstd, tmpf))
    S.barrier()

    ar.reset()
    wi1 = ar.alloc([128, KT, L1_IN], BF16)
    g.dma("pool", wi1, W["l1_w_in"].rearrange("(k p) c -> p k c", p=128), w=["wi1"])
    wuq = ar.alloc([128, 3, 768], BF16)
    g.dma("pool", wuq, W["l1_w_uq"].rearrange("(k p) c -> p k c", p=128), w=["wuq"])
    wukp = ar.alloc([128, 2, 8, 96], BF16)
    wuv = ar.alloc([128, 2, 8, 64], BF16)
    wkrp = ar.alloc([128, KT, 96], BF16)
    g.memset("pool", wukp, 0.0, w=["wukp"])
    g.memset("pool", wkrp, 0.0, w=["wkrp"])
    ukv = W["l1_w_ukv"].rearrange("(ct p) (h c) -> p ct h c", p=128, c=128)
    for ct in range(2):
        g.dma("pool", wukp[:, ct, :, 0:64], ukv[:, ct, :, 0:64], r=["wukp"], w=["wukp_%d" % ct])
        g.dma("pool", wuv[:, ct, :, :], ukv[:, ct, :, 64:128], w=["wuv_%d" % ct])
    g.dma("pool", wkrp[:, :, 64:96], W["l1_w_in"].rearrange("(k p) c -> p k c", p=128)[:, :, 640:672], r=["wkrp"], w=["wkrp_d"])
    wkeys = ["wukp", "wukp_0", "wukp_1", "wkrp", "wkrp_d"]
    cosb = ar.alloc([128, 512], F32)
    sinb = ar.alloc([128, 512], F32)
    hn_tmp = (ar.alloc([128, 512], BF16), ar.alloc([128, 512], F32), ar.alloc([128, 512], F32),
              ar.alloc([128, 512], BF16), ar.alloc([128, 512], F32))
    obuf = [ar.alloc([128, 512], BF16) for _ in range(2)]
    vaug = [ar.alloc([128, 8, 65], BF16) for _ in range(2)]
    for i in range(2):
        g.memset("pool", vaug[i], 1.0, w=["vaug%d" % i])
    cf = ar.alloc([128, 3, 512], F32)
    csq = ar.alloc([128, 3, 512], BF16)
    crs = ar.alloc([128, 512], F32)
    cqn = ar.alloc([128, 3, 512], BF16)
    ckvn = ar.alloc([128, 2, 512], BF16)
    g.Q1 = g.scratch("Q1", [8, 96, TL], BF16)
    mk = g.outp if g.mode == "pre1" else g.scratch
    g.K1own = mk("K1own", [8, 96, TL], BF16)
    g.V1own = mk("V1own", [8, 128, NCH, 65], BF16)
    g.K1ctx = mk("K1ctx", [8, 96, LC], BF16)
    g.V1ctx = mk("V1ctx", [8, 128, LC // 128, 65], BF16)

    def lora_norm(gi, t0, n, col0, ntile, gains, gkey, out, okey, divisor):
        for ct in range(ntile):
            pb = g.bank[ct]
            g.mmk(pb[:, 0:n], [wi1[:, k, col0 + ct * 128:col0 + (ct + 1) * 128] for k in range(KT)],
                  [g.hT[:, k, t0:t0 + n] for k in range(KT)], r=["wi1", "hT.g%d" % gi], w=["bank%d" % ct])
            g.cp("act", cf[:, ct, 0:n], pb[:, 0:n], r=["bank%d" % ct], w=["cf%d" % ct])
            g.tt("pool", csq[:, ct, 0:n], cf[:, ct, 0:n], cf[:, ct, 0:n], ALU.mult, r=["cf%d" % ct], w=["csq%d" % ct])
        pb = g.bank[3]
        g.mmk(pb[:, 0:n], [g.C["ones_bf"][:, :]] * ntile, [csq[:, ct, 0:n] for ct in range(ntile)],
              r=["csq%d" % ct for ct in range(ntile)] + ["C_ones_bf"], w=["bank3"])
        g.act(crs[:, 0:n], pb[:, 0:n], AF.Sqrt, bias=g.epsc[:, 0:1], scale=1.0 / divisor, r=["bank3", "epsc"], w=["crs"])
        g.recip(crs[:, 0:n], crs[:, 0:n], r=["crs"], w=["crs"])
        for ct in range(ntile):
            g.stt("dve", out[:, ct, 0:n], cf[:, ct, 0:n], gains[:, ct:ct + 1], crs[:, 0:n], ALU.mult, ALU.mult,
                  r=["cf%d" % ct, "crs", gkey], w=[okey])

    cnt = 0
    for gi, (t0, n, v) in enumerate(g.groups):
        rope = None
        if v == 0:
            g.dma("sp", cosb[:, 0:n], g.C_dram["cos1"][:, t0:t0 + n], w=["cosb"])
            g.dma("sp", sinb[:, 0:n], g.C_dram["sin1"][:, t0:t0 + n], w=["sinb"])
            rope = (cosb[0:96, 0:n], sinb[0:96, 0:n], g.C["rsw96"], ["cosb", "sinb", "C_rsw96"])
            lora_norm(gi, t0, n, 0, 3, qlg, "qlg", cqn, "cqn", 384.0)
        lora_norm(gi, t0, n, 384, 2, kvlg, "kvlg", ckvn, "ckvn", 256.0)
        for hh in range(16 if v == 0 else 8):
            h = hh % 8
            pb = g.bank[4 + hh % 2]
            bk = "bank%d" % (4 + hh % 2)
            if hh < 8:
                g.mmk(pb[0:96, 0:n], [wukp[:, ct, h, :] for ct in range(2)] + [wkrp[:, k, :] for k in range(KT)],
                      [ckvn[:, ct, 0:n] for ct in range(2)] + [g.hT[:, k, t0:t0 + n] for k in range(KT)],
                      r=wkeys + ["ckvn", "hT.g%d" % gi], w=[bk])
                gain, gkey = gk1, "gk1"
            else:
                g.mmk(pb[0:96, 0:n], [wuq[:, ct, h * 96:(h + 1) * 96] for ct in range(3)],
                      [cqn[:, ct, 0:n] for ct in range(3)], r=["wuq", "cqn"], w=[bk])
                gain, gkey = gq1, "gq1"
            ob = obuf[cnt % 2]
            okey = "obuf%d" % (cnt % 2)
            cnt += 1
            g.headnorm(96, n, pb[0:96, 0:n], bk, g.C["bm96"], "C_bm96", gain[0:96, 0:1], gkey, ob[0:96, 0:n], okey,
                       rope=rope, pbank=6, tmp=hn_tmp)
            if hh < 8:
                dst = g.K1own[h, :, t0:t0 + n] if v == 0 else g.K1ctx[h, :, :]
                g.dma("sp", dst, ob[0:96, 0:n], r=[okey], w=["K1own" if v == 0 else "K1ctx"])
            else:
                g.dma("sp", g.Q1[h, :, t0:t0 + n], ob[0:96, 0:n], r=[okey], w=["Q1"])
        for tt_ in range(n // 128):
            c0 = tt_ * 128
            pb = g.bank[7]
            g.mmk(pb[:, 0:512], [ckvn[:, ct, c0:c0 + 128] for ct in range(2)],
                  [wuv[:, ct, :, :].rearrange("p h c -> p (h c)") for ct in range(2)],
                  r=["wuv_0", "wuv_1", "ckvn"], w=["bank7"])
            va = vaug[tt_ % 2]
            g.cp("act", va[:, :, 0:64], pb[:, 0:512].rearrange("p (a b) -> p a b", b=64), r=["bank7"], w=["vaug%d" % (tt_ % 2)])
            blk = (t0 + c0) // 128 if v == 0 else (t0 + c0 - TL) // 128
            dst = (g.V1own if v == 0 else g.V1ctx)[:, :, blk, :].rearrange("h p c -> p h c")
            g.dma("sp", dst, va[:, :, :], r=["vaug%d" % (tt_ % 2)], w=["V1own" if v == 0 else "V1ctx"])
    S.barrier()


def _layer1_main(self, stop=None):
    g = self
    S = g.S
    TL, T, NCH = g.TL, g.T, g.NCH
    ar = g.ar
    M, W = g.M1, g.W1
    if g.mode == "fused":
        raise NotImplementedError
    else:
        g.K1all = g.inp("K1all", [8, NCORES, 96, TL], BF16)
        g.V1all = g.inp("V1all", [8, NCORES, 128, NCH, 65], BF16)
    S.barrier()
    ar.base = 0
    ar.reset()
    g.atT = ar.alloc([128, 4, T], BF16)
    ar.base = ar.off
    CK = min(1024, TL)
    tmp = ([ar.alloc([128, T], BF16) for _ in range(2)], [ar.alloc([128, CK], BF16) for _ in range(3)],
           [ar.alloc([128, CK // 128, 65], BF16) for _ in range(3)], [ar.alloc([128, 512], BF16) for _ in range(3)],
           ar.alloc([128, 512], F32), ar.alloc([128, 512], F32), ar.alloc([128, 512], BF16),
           ar.alloc([128, 512], BF16), ar.alloc([128, 512], BF16))

    def ksegs(h):
        segs = []
        for r_ in range(NCORES):
            for c in range(TL // CK):
                segs.append((g.K1all[h, r_, :, c * CK:(c + 1) * CK],
                             g.V1all[h, r_, :, c * CK // 128:(c + 1) * CK // 128, :], "K1all", "V1all"))
        segs.append((g.K1ctx[h], g.V1ctx[h], "K1ctx", "V1ctx"))
        return segs

    g.attention("a1", 96, 96.0 ** -0.5, 8, lambda h: g.Q1[h, :, 0:TL], ksegs, TL, g.atT, "atT", 0, tmp)
    S.barrier()
    if stop == "attn":
        return
    ar.reset()
    lat_groups = [gr for gr in g.groups if gr[2] == 0]
    wo = ar.alloc([128, 4, D], BF16)
    g.dma("pool", wo, W["l1_w_out"].rearrange("(k p) c -> p k c", p=128), w=["wo1"])
    for gi, (t0, n, v) in enumerate(lat_groups):
        cat = [g.atT[:, kt, t0:t0 + n] for kt in range(4)]
        for dc in range(8):
            pb = g.bank[dc % 4]
            bk = "bank%d" % (dc % 4)
            g.mmk(pb[:, 0:n], [wo[:, kt, dc * 128:(dc + 1) * 128] for kt in range(4)], cat, r=["wo1", "atT"], w=[bk])
            res = g.res_tile(dc, t0, n)
            g.stt("dve", res, pb[:, 0:n], M["gate1"][:, dc, v:v + 1], res, ALU.mult, ALU.add,
                  r=[bk, "xT.g%d" % gi] + M["key"], w=["xT.g%d" % gi])
    S.barrier()
    if stop == "wout":
        return
    ar.base = 0
    ar.reset()
    NG = len(lat_groups)
    gT_sb = ar.alloc([8, TL], F32)
    esel_d = g.inp("c_esel", [8, NE * 128], F32)
    esel = ar.alloc([8, NE * 128], F32)
    g.dma("sp", esel, esel_d, w=["esel"])
    gbc = ar.alloc([128, NG, 512], F32)
    moe_mark = ar.off
    sq = ar.alloc([128, KT, 512], BF16)
    rstd = ar.alloc([128, 512], F32)
    tmpf = [ar.alloc([128, 512], F32) for _ in range(2)]
    h2f = ar.alloc([128, KT, 512], F32)
    rt = ar.alloc([128, KT, NE], F32)
    g.dma("sp", rt, W["l1_router"].rearrange("(k p) e -> p k e", p=128), w=["rt"])
    lg = ar.alloc([128, NCH, NE], F32)
    gates = ar.alloc([128, NCH, NE], F32)
    sm = [ar.alloc([128, 8], F32) for _ in range(4)]
    col = [ar.alloc([128, 1], F32) for _ in range(5)]
    for gi, (t0, n, v) in enumerate(lat_groups):
        pb = g.bank[6]
        for k in range(KT):
            eng = ("act", "pool", "dve")[k % 3]
            src = g.xT[:, k, t0:t0 + n]
            if eng == "act":
                g.act(sq[:, k, 0:n], src, AF.Square, r=["xT.g%d" % gi], w=["nm_sq%d" % k])
            else:
                g.tt(eng, sq[:, k, 0:n], src, src, ALU.mult, r=["xT.g%d" % gi], w=["nm_sq%d" % k])
        g.mmk(pb[:, 0:n], [g.C["ones_bf"][:, :]] * KT, [sq[:, k, 0:n] for k in range(KT)],
              r=["nm_sq%d" % k for k in range(KT)] + ["C_ones_bf"], w=["bank6"])
        g.act(rstd[:, 0:n], pb[:, 0:n], AF.Sqrt, bias=g.epsc[:, 0:1], scale=1.0 / D, r=["bank6", "epsc"], w=["nm_rstd"])
        g.recip(rstd[:, 0:n], rstd[:, 0:n], r=["nm_rstd"], w=["nm_rstd"])
        for k in range(KT):
            tf = tmpf[k % 2]
            g.tt("dve" if k % 2 == 0 else "pool", tf[:, 0:n], g.xT[:, k, t0:t0 + n], rstd[:, 0:n], ALU.mult,
                 r=["xT.g%d" % gi, "nm_rstd"], w=["nm_tmp%d" % (k % 2)])
            g.act(h2f[:, k, 0:n], tf[:, 0:n], AF.Identity, bias=M["sh2"][:, k, 0:1], scale=M["gs2"][:, k, 0:1],
                  r=["nm_tmp%d" % (k % 2)] + M["key"], w=["h2f%d" % k])
            g.cp("pool", g.hT[:, k, t0:t0 + n], h2f[:, k, 0:n], r=["h2f%d" % k], w=["hT.g%d" % gi])
        for tt_ in range(n // 128):
            tile_i = (t0 + tt_ * 128) // 128
            pb2 = g.bank[7]
            g.mmk(pb2[:, 0:NE], [h2f[:, k, tt_ * 128:(tt_ + 1) * 128] for k in range(KT)], [rt[:, k, :] for k in range(KT)],
                  r=["h2f%d" % k for k in range(KT)] + ["rt"], w=["bank7"])
            g.cp("dve", lg[:, tile_i, :], pb2[:, 0:NE], r=["bank7"], w=["lg"])
    for ti in range(NCH):
        lt = lg[:, ti, :]
        m1, m2, nm1, den, rden = [c[:, 0:1] for c in col]
        eq, lg2, sel, ex = [s_[:, :] for s_ in sm]
        g.op("dve", lambda e, o=m1, i=lt: e.tensor_reduce(o, i, AX.X, ALU.max), r=["lg"], w=["c_m1"])
        g.ts("dve", eq, lt, m1, None, ALU.is_equal, r=["lg", "c_m1"], w=["s_eq"])
        g.stt("dve", lg2, eq, -1e30, lt, ALU.mult, ALU.add, r=["s_eq", "lg"], w=["s_lg2"])
        g.op("dve", lambda e, o=m2, i=lg2: e.tensor_reduce(o, i, AX.X, ALU.max), r=["s_lg2"], w=["c_m2"])
        g.ts("dve", sel, lt, m2, None, ALU.is_ge, r=["lg", "c_m2"], w=["s_sel"])
        g.ts("dve", nm1, m1, -1.0, None, ALU.mult, r=["c_m1"], w=["c_nm1"])
        g.act(ex, lt, AF.Exp, bias=nm1, scale=1.0, r=["lg", "c_nm1"], w=["s_ex"])
        g.tt("dve", ex, ex, sel, ALU.mult, r=["s_ex", "s_sel"], w=["s_ex"])
        g.op("dve", lambda e, o=den, i=ex: e.tensor_reduce(o, i, AX.X, ALU.add), r=["s_ex"], w=["c_den"])
        g.recip(rden, den, r=["c_den"], w=["c_rden"])
        g.ts("dve", gates[:, ti, :], ex, rden, None, ALU.mult, r=["s_ex", "c_rden"], w=["gates"])
        pb = g.bank[6]
        g.mm(pb[0:8, 0:128], gates[:, ti, :], g.C["ident_f"][:, :], True, True, r=["gates", "C_ident_f"], w=["bank6"])
        g.cp("act", gT_sb[0:8, ti * 128:(ti + 1) * 128], pb[0:8, 0:128], r=["bank6"], w=["gT_sb"])
    if stop == "gates":
        go = g.outp("gates_out", [8, TL])
        g.dma("sp", go, gT_sb, r=["gT_sb"])
        return
    S.barrier()
    ar.off = moe_mark
    FC = 256
    NFC = EDIM // FC
    w1c = [ar.alloc([128, KT, FC], BF16) for _ in range(2)]
    w3c = [ar.alloc([128, KT, FC], BF16) for _ in range(2)]
    w2c = [ar.alloc([128, FC // 128, D], BF16) for _ in range(2)]
    gTt = [ar.alloc([128, FC // 128, 512], BF16) for _ in range(2)]
    sil = [ar.alloc([128, 512], F32) for _ in range(2)]
    sig = [ar.alloc([128, 512], F32) for _ in range(2)]

    def load_w(idx):
        e, fc = idx // NFC, idx % NFC
        i = idx % 2
        g.dma("pool", w1c[i], W["l1_exp_w1"][e].rearrange("(k p) f -> p k f", p=128)[:, :, fc * FC:(fc + 1) * FC], w=["w1c%d" % i])
        g.dma("pool", w3c[i], W["l1_exp_w3"][e].rearrange("(k p) f -> p k f", p=128)[:, :, fc * FC:(fc + 1) * FC], w=["w3c%d" % i])
        g.dma("pool", w2c[i], W["l1_exp_w2"][e].rearrange("(ft p) d -> p ft d", p=128)[:, fc * (FC // 128):(fc + 1) * (FC // 128), :], w=["w2c%d" % i])
    load_w(0)
    cnt = 0
    c2 = 0
    for e in range(NE):
        for gi, (t0, n, v) in enumerate(lat_groups):
            pb = g.bank[gi % 4]
            g.mm(pb[:, 0:n], esel[0:8, e * 128:(e + 1) * 128], gT_sb[0:8, t0:t0 + n], True, True,
                 r=["esel", "gT_sb"], w=["bank%d" % (gi % 4)])
            g.cp("act", gbc[:, gi, 0:n], pb[:, 0:n], r=["bank%d" % (gi % 4)], w=["gbc"])
        for fc in range(NFC):
            idx = e * NFC + fc
            if idx + 1 < NE * NFC:
                load_w(idx + 1)
            i = idx % 2
            for gi, (t0, n, v) in enumerate(lat_groups):
                hk = "hT.g%d" % gi
                gt = gTt[cnt % 2]
                gk_ = "gTt%d" % (cnt % 2)
                cnt += 1
                hsl = [g.hT[:, k, t0:t0 + n] for k in range(KT)]
                for ft in range(FC // 128):
                    j = c2 % 2
                    c2 += 1
                    h1, h3 = g.bank[2 * j], g.bank[2 * j + 1]
                    g.mmk(h1[:, 0:n], [w1c[i][:, k, ft * 128:(ft + 1) * 128] for k in range(KT)], hsl,
                          r=["w1c%d" % i, hk], w=["bank%d" % (2 * j)])
                    g.mmk(h3[:, 0:n], [w3c[i][:, k, ft * 128:(ft + 1) * 128] for k in range(KT)], hsl,
                          r=["w3c%d" % i, hk], w=["bank%d" % (2 * j + 1)])
                    g.act(sil[j][:, 0:n], h1[:, 0:n], AF.Silu, r=["bank%d" % (2 * j)], w=["sil%d" % j])
                    g.tt("pool", sig[j][:, 0:n], sil[j][:, 0:n], gbc[:, gi, 0:n], ALU.mult, r=["sil%d" % j, "gbc"], w=["sig%d" % j])
                    g.tt("dve", gt[:, ft, 0:n], sig[j][:, 0:n], h3[:, 0:n], ALU.mult,
                         r=["sig%d" % j, "bank%d" % (2 * j + 1)], w=[gk_])
                for dc in range(8):
                    pb = g.bank[4 + dc % 4]
                    bk = "bank%d" % (4 + dc % 4)
                    g.mmk(pb[:, 0:n], [w2c[i][:, ft, dc * 128:(dc + 1) * 128] for ft in range(FC // 128)],
                          [gt[:, ft, 0:n] for ft in range(FC // 128)], r=["w2c%d" % i, gk_], w=[bk])
                    res = g.xT[:, dc, t0:t0 + n]
                    g.stt("dve", res, pb[:, 0:n], M["gate2"][:, dc, 0:1], res, ALU.mult, ALU.add,
                          r=[bk, "xT.g%d" % gi] + M["key"], w=["xT.g%d" % gi])
    S.barrier()


def _out_final(self):
    g = self
    xo = g.outp("xT_out", [128, KT, g.TL])
    for k in range(KT):
        g.dma("sp", xo[:, k, :], g.xT[:, k, :], r=["xT.g%d" % i for i in range(len(g.groups))])


Gen.layer1_pre = _layer1_pre
Gen.layer1_main = _layer1_main
Gen.out_final = _out_final


def build_l1(TL, mode, stop=None):
    g = Gen(TL, mode)
    g.setup_common()
    g.layer1_pre()
    if mode == "main1":
        g.layer1_main(stop)
        g.out_final()
    g.finish()
    return g


def _esel():
    e = np.zeros((8, NE * 128), np.float32)
    for i in range(NE):
        e[i, i * 128:(i + 1) * 128] = 1.0
    return e


def run_layer1(inputs, TL, xTs, cTs, stop=None):
    L1 = [k for k in inputs if k.startswith("l1_")]
    base = []
    for c in range(NCORES):
        m = core_inputs(inputs, TL, c, xTs[c], cTs[c])
        for k in L1:
            m[k] = inputs[k]
        m["c_esel"] = _esel()
        base.append(m)
    g1 = build_l1(TL, "pre1")
    r1 = run_prog(g1, base)
    K1all = np.ascontiguousarray(np.stack([r["K1own"] for r in r1], axis=1))
    V1all = np.ascontiguousarray(np.stack([r["V1own"] for r in r1], axis=1))
    for m in base:
        m["K1all"], m["V1all"] = K1all, V1all
    g2 = build_l1(TL, "main1", stop)
    r2 = run_prog(g2, base)
    return r2


def kernel(**inputs):
    inputs = {k: np.asarray(v) for k, v in inputs.items()}
    SEQ = inputs["x"].shape[1]
    TL = SEQ // NCORES
    xTs, cTs = run_layer0(inputs, TL)
    r2 = run_layer1(inputs, TL, xTs, cTs)
    out = np.concatenate([_from_fm(r["xT_out"]) for r in r2], axis=0)
    return out[None].astype(np.float32)
```

```python
import numpy as np
import ml_dtypes
import concourse.bass as bass
import concourse.mybir as mybir
from concourse.bass_utils import run_bass_kernel_spmd

F32 = mybir.dt.float32
BF16 = mybir.dt.bfloat16
AF = mybir.ActivationFunctionType
ALU = mybir.AluOpType
AX = mybir.AxisListType

NCORES = 8
D = 1024
KT = 8
GRID_W = 64
LC = 256
EPS = 1e-6
ROPE_THETA = 10000.0
FFN = 2816
NE = 8
EDIM = 3584
L0_IN = 2816
L1_IN = 672


class Op:
    __slots__ = ("eng", "fn", "deps", "dma", "sig", "ms", "dsem", "dval", "ringprev", "tag")


class Sched:
    ENGS = ("pe", "act", "dve", "pool", "sp")
    RING = 8

    def __init__(self):
        self.ops = []
        self.lastw = {}
        self.rds = {}
        self.pend_barrier = {e: [] for e in self.ENGS}
        self.last_op = {e: None for e in self.ENGS}
        self.all_dma = []

    def add(self, eng, fn, reads=(), writes=(), dma=False, tag=None):
        op = Op()
        op.eng, op.fn, op.dma, op.sig, op.ms = eng, fn, dma, False, 0
        op.dsem = op.dval = op.ringprev = None
        op.tag = tag
        deps = set()
        for k in reads:
            w = self.lastw.get(k)
            if w is not None:
                deps.add(w)
            if k.startswith("bank"):
                for r in self.rds.get(k, ()):
                    if r.eng != eng:
                        deps.add(r)
        for k in writes:
            w = self.lastw.get(k)
            if w is not None:
                deps.add(w)
            for r in self.rds.get(k, ()):
                deps.add(r)
        for d in self.pend_barrier[eng]:
            deps.add(d)
        self.pend_barrier[eng] = []
        op.deps = deps
        for k in reads:
            lst = self.rds.setdefault(k, [])
            if not dma:
                lst[:] = [r for r in lst if r.dma or r.eng != eng]
            lst.append(op)
        for k in writes:
            self.lastw[k] = op
            self.rds[k] = []
        self.ops.append(op)
        self.last_op[eng] = op
        if dma:
            self.all_dma.append(op)
        return op

    def barrier(self):
        lasts = [o for o in self.last_op.values() if o is not None]
        dm = list(self.all_dma)
        self.all_dma = []
        for e in self.ENGS:
            self.pend_barrier[e] = self.pend_barrier[e] + lasts + dm

    def finalize(self, nc):
        for op in self.ops:
            for d in op.deps:
                if d.dma:
                    continue
                if d.eng == "pe" and op.eng == "pe" and not op.dma:
                    continue
                d.sig = True
        self.sem = {e: nc.alloc_semaphore("sem_" + e) for e in self.ENGS}
        self.ring = {e: [nc.alloc_semaphore("dma_%s_%d" % (e, i)) for i in range(self.RING)]
                     for e in ("sp", "pool", "act")}
        cnt = {e: 0 for e in self.ENGS}
        dcnt = {e: 0 for e in self.ENGS}
        ringlast = {e: [None] * self.RING for e in self.ENGS}
        for op in self.ops:
            if op.dma and op.tag == "cc":
                op.dsem = nc.alloc_semaphore("cc_%d" % len(self.ops) + "_%d" % id(op))
                op.dval = 1
            elif op.dma:
                i = dcnt[op.eng]
                dcnt[op.eng] += 1
                slot = i % self.RING
                op.dsem = self.ring[op.eng][slot]
                op.dval = 16 * (i // self.RING + 1)
                op.ringprev = ringlast[op.eng][slot]
                ringlast[op.eng][slot] = op
            elif op.sig:
                cnt[op.eng] += 1
                op.ms = cnt[op.eng]
        self.by_eng = {e: [o for o in self.ops if o.eng == e] for e in self.ENGS}
        self.counts = cnt

    def emit(self, ename, eng):
        waited = {}
        for op in self.by_eng[ename]:
            waits = {}
            for d in op.deps:
                if d.dma:
                    key, sem, val = ("d", d.eng, id(d.dsem)), d.dsem, d.dval
                else:
                    if d.eng == "pe" and ename == "pe" and not op.dma:
                        continue
                    key, sem, val = ("c", d.eng, 0), self.sem[d.eng], d.ms
                if key not in waits or waits[key][1] < val:
                    waits[key] = (sem, val)
            if op.dma and op.ringprev is not None:
                d = op.ringprev
                key = ("d", d.eng, id(d.dsem))
                if key not in waits or waits[key][1] < d.dval:
                    waits[key] = (d.dsem, d.dval)
            for key, (sem, val) in waits.items():
                if waited.get(key, 0) < val:
                    eng.wait_ge(sem, val)
                    waited[key] = val
            ins = op.fn(eng)
            if ins is None:
                continue
            if op.dma and op.tag == "cc":
                ins.then_inc(op.dsem, 1)
            elif op.dma:
                ins.then_inc(op.dsem, 16)
            elif op.sig:
                ins.then_inc(self.sem[ename], 1)


def _rope_tables(pos_rows, pos_cols, rot_dim):
    axis_dim = rot_dim // 2
    inv_freq = (ROPE_THETA ** (-np.arange(0, axis_dim, 2, dtype=np.float32) / axis_dim)).astype(np.float32)
    ang = np.concatenate([pos_rows[:, None].astype(np.float32) * inv_freq,
                          pos_cols[:, None].astype(np.float32) * inv_freq], axis=-1)
    return np.cos(ang).astype(np.float32), np.sin(ang).astype(np.float32)


def host_consts(core, TL):
    bf = ml_dtypes.bfloat16
    c = {}
    c["ident_bf"] = np.eye(128, dtype=np.float32).astype(bf)
    c["ident_f"] = np.eye(128, dtype=np.float32)
    c["ones_bf"] = np.ones((128, 128), np.float32).astype(bf)
    c["ones_f"] = np.ones((128, 128), np.float32)
    bo = np.zeros((128, 128), np.float32)
    bo[:64, :64] = 1.0 / 64
    bo[64:, 64:] = 1.0 / 64
    c["bo64"] = bo.astype(bf)
    bm = np.zeros((128, 128), np.float32)
    bm[:64, :64] = 1.0 / 64
    bm[64:96, 64:96] = 1.0 / 32
    c["bm96"] = bm.astype(bf)
    bd = np.zeros((128, 128), np.float32)
    bd[:64, :64] = 1.0
    bd[64:, 64:] = 1.0
    c["bdmask"] = bd
    r = np.zeros((128, 128), np.float32)
    for i in range(64):
        r[2 * i + 1, 2 * i] = -1.0
        r[2 * i, 2 * i + 1] = 1.0
    c["rsw"] = r.astype(bf)
    r96 = r.copy()
    r96[:64, :] = 0.0
    r96[:, :64] = 0.0
    r96[96:, :] = 0.0
    r96[:, 96:] = 0.0
    c["rsw96"] = r96.astype(bf)
    t = core * TL + np.arange(TL)
    rows, cols = t // GRID_W, t % GRID_W
    cs, sn = _rope_tables(rows, cols, 64)
    c["cos0"] = np.ascontiguousarray(np.repeat(cs, 2, axis=1).T)
    c["sin0"] = np.ascontiguousarray(np.repeat(sn, 2, axis=1).T)
    c["cos0"] = np.concatenate([c["cos0"], c["cos0"]], axis=0)
    c["sin0"] = np.concatenate([c["sin0"], c["sin0"]], axis=0)
    cs, sn = _rope_tables(rows, cols, 32)
    c1 = np.ones((128, TL), np.float32)
    s1 = np.zeros((128, TL), np.float32)
    c1[64:96] = np.repeat(cs, 2, axis=1).T
    s1[64:96] = np.repeat(sn, 2, axis=1).T
    c["cos1"], c["sin1"] = c1, s1
    m = np.arange(128, dtype=np.float32)
    cc = np.arange(128, dtype=np.float32)
    relf = np.maximum(cc[None, :] - m[:, None], 0.0)
    relb = np.maximum(m[:, None] - cc[None, :], 0.0)
    c["relf"] = relf.astype(np.float32)
    c["relb"] = relb.astype(np.float32)
    c["indf"] = (cc[None, :] >= m[:, None]).astype(np.float32)
    c["indb"] = (m[:, None] >= cc[None, :]).astype(np.float32)
    c["posf"] = np.repeat((127.0 - m)[:, None], 64, axis=1).astype(np.float32)
    c["posb"] = np.repeat(m[:, None], 64, axis=1).astype(np.float32)
    c["rampf"] = np.repeat((cc + 1.0)[None, :], 128, axis=0).astype(np.float32)
    c["rampb"] = np.repeat((128.0 - cc)[None, :], 128, axis=0).astype(np.float32)
    nch = TL // 128
    cp = np.zeros((128, 40), np.float32)
    for r_ in range(NCORES):
        if r_ < core:
            cp[:, r_] = 128.0 * nch * (core - 1 - r_)
            cp[:, 8 + r_] = 1.0
        if r_ > core:
            cp[:, 16 + r_] = 128.0 * nch * (r_ - core - 1)
            cp[:, 24 + r_] = 1.0
    cp[:, 32] = 128.0 * nch * core
    cp[:, 33] = 128.0 * nch * (NCORES - 1 - core)
    c["cpos"] = cp
    return c


class Gen:
    def __init__(self, TL, mode):
        self.TL = TL
        self.T = TL + LC
        self.NCH = TL // 128
        self.S_ALL = NCORES * TL + LC
        self.NKB = self.S_ALL // 128
        self.mode = mode
        self.nc = bass.Bass("TRN2", target_bir_lowering=False)
        self.S = Sched()
        self.din = {}
        self.dout = {}
        self.groups = [(g * 512, 512, 0) for g in range(TL // 512)] + [(TL, LC, 1)]
        self.uid = 0

    def inp(self, name, shape, dt=F32):
        t = self.nc.dram_tensor(name, list(shape), dt, kind="ExternalInput")
        self.din[name] = t
        return t.ap()

    def outp(self, name, shape, dt=F32):
        t = self.nc.dram_tensor(name, list(shape), dt, kind="ExternalOutput")
        self.dout[name] = t
        return t.ap()

    def scratch(self, name, shape, dt):
        return self.nc.dram_tensor(name, list(shape), dt).ap()

    def sb(self, name, shape, dt):
        return self.nc.alloc_sbuf_tensor(name, list(shape), dt)

    def op(self, eng, fn, r=(), w=(), dma=False):
        return self.S.add(eng, fn, r, w, dma)

    def dma(self, q, out, in_, r=(), w=()):
        return self.S.add(q, lambda e, o=out, i=in_: e.dma_start(out=o, in_=i), r, w, dma=True)

    def mm(self, out, lhsT, rhs, start, stop, r=(), w=()):
        return self.S.add("pe", lambda e, o=out, l=lhsT, rr=rhs, s=start, t=stop:
                          e.matmul(o, l, rr, start=s, stop=t), r, w)

    def mmk(self, out, lhs_list, rhs_list, r=(), w=()):
        n = len(lhs_list)

        def fn(e, o=out, ll=lhs_list, rl=rhs_list):
            ins = None
            for k in range(n):
                ins = e.matmul(o, ll[k], rl[k], start=(k == 0), stop=(k == n - 1))
            return ins
        return self.S.add("pe", fn, r, w)

    def act(self, out, in_, func, bias=0.0, scale=1.0, r=(), w=()):
        return self.S.add("act", lambda e, o=out, i=in_, f=func, b=bias, s=scale:
                          e.activation(out=o, in_=i, func=f, bias=b, scale=s), r, w)

    def tt(self, eng, out, in0, in1, op, r=(), w=()):
        return self.S.add(eng, lambda e, o=out, a=in0, b=in1, p=op: e.tensor_tensor(o, a, b, p), r, w)

    def ts(self, eng, out, in0, s1, s2, op0, op1=None, r=(), w=()):
        if op1 is None:
            return self.S.add(eng, lambda e, o=out, a=in0, x=s1, p=op0:
                              e.tensor_scalar(o, a, x, None, p), r, w)
        return self.S.add(eng, lambda e, o=out, a=in0, x=s1, y=s2, p=op0, q=op1:
                          e.tensor_scalar(o, a, x, y, p, q), r, w)

    def stt(self, eng, out, in0, scalar, in1, op0, op1, r=(), w=()):
        return self.S.add(eng, lambda e, o=out, a=in0, s=scalar, b=in1, p=op0, q=op1:
                          e.scalar_tensor_tensor(o, a, s, b, p, q), r, w)

    def cp(self, eng, out, in_, r=(), w=()):
        if eng == "act":
            return self.S.add("act", lambda e, o=out, i=in_: e.copy(o, i), r, w)
        return self.S.add(eng, lambda e, o=out, i=in_: e.tensor_copy(o, i), r, w)

    def recip(self, out, in_, r=(), w=()):
        return self.S.add("dve", lambda e, o=out, i=in_: e.reciprocal(o, i), r, w)

    def memset(self, eng, ap, val, w=()):
        return self.S.add(eng, lambda e, a=ap, v=val: e.memset(a, v), (), w)

    def load_consts(self):
        TL = self.TL
        spec = [("ident_bf", [128, 128], BF16), ("ident_f", [128, 128], F32),
                ("ones_bf", [128, 128], BF16), ("ones_f", [128, 128], F32),
                ("bo64", [128, 128], BF16), ("bm96", [128, 128], BF16),
                ("bdmask", [128, 128], F32), ("rsw", [128, 128], BF16), ("rsw96", [128, 128], BF16),
                ("cpos", [128, 40], F32)]
        self.C = {}
        for name, shape, dt in spec:
            d = self.inp("c_" + name, shape, dt)
            s = self.sb("C_" + name, shape, dt)
            self.dma("sp", s[:], d, w=["C_" + name])
            self.C[name] = s
        self.C_dram = {}
        for name, shape in [("cos0", [128, TL]), ("sin0", [128, TL]), ("cos1", [128, TL]), ("sin1", [128, TL]),
                            ("relf", [128, 128]), ("relb", [128, 128]), ("indf", [128, 128]),
                            ("indb", [128, 128]), ("posf", [128, 64]), ("posb", [128, 64]),
                            ("rampf", [128, 128]), ("rampb", [128, 128])]:
            self.C_dram[name] = self.inp("c_" + name, shape, F32)
        self.bank = [self.nc.alloc_psum_tensor("bank%d" % i, [128, 512], F32) for i in range(8)]

    def ada(self, L, ada_w, ada_b, cvec_sb):
        modT = self.sb("modT%d" % L, [128, 48, 2], F32)
        abT = self.gain_cols("abT%d" % L, ada_b, 48)
        NCHK = 12
        CW = 6 * D // NCHK
        self.ar.reset()
        wbuf = [self.ar.alloc([128, KT, CW], F32) for i in range(2)]
        aw = ada_w.rearrange("(k p) c -> p k c", p=128)
        pb = self.bank[7]
        for ch in range(NCHK):
            wb = wbuf[ch % 2]
            self.dma("sp", wb, aw[:, :, ch * CW:(ch + 1) * CW], w=["adaw%d" % (ch % 2)])
            for jj in range(CW // 128):
                j = ch * (CW // 128) + jj
                self.mmk(pb[:, 2 * j:2 * j + 2],
                         [wb[:, k, jj * 128:(jj + 1) * 128] for k in range(KT)],
                         [cvec_sb[:, k, :] for k in range(KT)],
                         r=["adaw%d" % (ch % 2), "scvec"], w=["bank7"])
        pv = pb[:, 0:96].rearrange("p (j v) -> p j v", v=2)
        for v in range(2):
            self.tt("dve", modT[:, :, v], pv[:, :, v], abT[:], ALU.add, r=["bank7", "abT%d" % L], w=["modT%d" % L])
        self.S.barrier()
        return modT

    def gain_cols(self, name, g_ap, n, pieces=None):
        t = self.sb(name, [128, n], F32)
        rows = self.sb(name + "_rows", [n, 128], F32)
        if pieces is None:
            self.dma("sp", rows[:], g_ap.rearrange("(k p) -> k p", p=128), w=[name + "_rows"])
        else:
            self.memset("pool", rows[:], 0.0, w=[name + "_rows"])
            for (c0, c1, src) in pieces:
                self.dma("sp", rows[0:1, c0:c1], src.rearrange("(o p) -> o p", o=1), r=[name + "_rows"], w=[name + "_rows_%d" % c0])
        pb = self.bank[7]
        rk = [name + "_rows"] + ([name + "_rows_%d" % c0 for (c0, c1, src) in pieces] if pieces else [])
        self.mm(pb[:, 0:n], rows[0:n, :], self.C["ident_f"][0:n, 0:n], True, True, r=rk + ["C_ident_f"], w=["bank7"])
        self.cp("dve", t[:], pb[:, 0:n], r=["bank7"], w=[name])
        return t

    def mod_derived(self, L, modT, n1g, n2g):
        o = {}
        for nm, piece_scale, g in (("gs1", 1, n1g), ("gs2", 4, n2g)):
            t = self.sb("%s_%d" % (nm, L), [128, 8, 2], F32)
            for v in range(2):
                self.stt("dve", t[:, :, v], modT[:, piece_scale * 8:(piece_scale + 1) * 8, v], 1.0, g[:],
                         ALU.add, ALU.mult, r=["modT%d" % L, g.name if hasattr(g, "name") else "g"],
                         w=["%s_%d" % (nm, L)])
            o[nm] = t
        o["sh1"] = modT[:, 0:8, :]
        o["gate1"] = modT[:, 16:24, :]
        o["sh2"] = modT[:, 24:32, :]
        o["gate2"] = modT[:, 40:48, :]
        o["key"] = ["modT%d" % L, "gs1_%d" % L, "gs2_%d" % L]
        return o

    def norm_mod(self, src_fn, src_keys, dst, dst_key, gs, sh, modkeys, tmp):
        sq, rstd, tmpf = tmp
        for gi, (t0, n, v) in enumerate(self.groups):
            pb = self.bank[6]
            for k in range(KT):
                eng = ("act", "pool", "dve")[k % 3]
                src = src_fn(k, t0, n)
                if eng == "act":
                    self.act(sq[:, k, 0:n], src, AF.Square, r=src_keys(gi), w=["nm_sq%d" % k])
                else:
                    self.tt(eng, sq[:, k, 0:n], src, src, ALU.mult, r=src_keys(gi), w=["nm_sq%d" % k])
            self.mmk(pb[:, 0:n], [self.C["ones_bf"][:, :] for k in range(KT)],
                     [sq[:, k, 0:n] for k in range(KT)],
                     r=["nm_sq%d" % k for k in range(KT)] + ["C_ones_bf"], w=["bank6"])
            self.act(rstd[:, 0:n], pb[:, 0:n], AF.Sqrt, bias=self.epsc[:, 0:1], scale=1.0 / D, r=["bank6", "epsc"], w=["nm_rstd"])
            self.recip(rstd[:, 0:n], rstd[:, 0:n], r=["nm_rstd"], w=["nm_rstd"])
            for k in range(KT):
                src = src_fn(k, t0, n)
                tf = tmpf[k % 2]
                self.tt("dve" if k % 2 == 0 else "pool", tf[:, 0:n], src, rstd[:, 0:n], ALU.mult,
                        r=src_keys(gi) + ["nm_rstd"], w=["nm_tmp%d" % (k % 2)])
                self.act(dst[:, k, t0:t0 + n], tf[:, 0:n], AF.Identity, bias=sh[:, k, v:v + 1], scale=gs[:, k, v:v + 1],
                         r=["nm_tmp%d" % (k % 2)] + modkeys, w=["%s.g%d" % (dst_key, gi)])

    def headnorm(self, P, n, src_psum, src_key, blk, blk_key, gain_col, gain_key, out_bf, out_key,
                 rope=None, pbank=5, tmp=None):
        sqb, rs, qn, qnb, t1 = tmp
        pb = self.bank[pbank]
        bk = "bank%d" % pbank
        self.act(sqb[0:P, 0:n], src_psum, AF.Square, r=[src_key], w=["hn_sq"])
        self.mm(pb[0:P, 0:n], blk[0:P, 0:P], sqb[0:P, 0:n], True, True, r=["hn_sq", blk_key], w=[bk])
        self.act(rs[0:P, 0:n], pb[0:P, 0:n], AF.Sqrt, bias=self.epsc[0:P, 0:1], scale=1.0, r=[bk, "epsc"], w=["hn_rs"])
        self.recip(rs[0:P, 0:n], rs[0:P, 0:n], r=["hn_rs"], w=["hn_rs"])
        if rope is None:
            self.stt("dve", out_bf, src_psum, gain_col, rs[0:P, 0:n], ALU.mult, ALU.mult,
                     r=[src_key, gain_key, "hn_rs"], w=[out_key])
            return
        cos_ap, sin_ap, rsw, rkeys = rope
        self.stt("dve", qn[0:P, 0:n], src_psum, gain_col, rs[0:P, 0:n], ALU.mult, ALU.mult,
                 r=[src_key, gain_key, "hn_rs"], w=["hn_qn"])
        self.cp("act", qnb[0:P, 0:n], qn[0:P, 0:n], r=["hn_qn"], w=["hn_qnb"])
        self.mm(pb[0:P, 0:n], rsw[0:P, 0:P], qnb[0:P, 0:n], True, True, r=["hn_qnb"] + rkeys, w=[bk])
        self.tt("pool", t1[0:P, 0:n], qn[0:P, 0:n], cos_ap, ALU.mult, r=["hn_qn"] + rkeys, w=["hn_t1"])
        self.tt("dve", qn[0:P, 0:n], pb[0:P, 0:n], sin_ap, ALU.mult, r=[bk] + rkeys, w=["hn_qn"])
        self.tt("dve", out_bf, qn[0:P, 0:n], t1[0:P, 0:n], ALU.add, r=["hn_qn", "hn_t1"], w=[out_key])

    def attention(self, tagp, dq, scale, nheads, qsrc, ksegs_of_head, nq, out_tile, out_key, out_off, tmp):
        qbuf, kbuf, vbuf, pbuf, osb, rl, rlh, rll, atmp = tmp
        NQG = (nq + 511) // 512
        LA = 2
        its = []
        for h in range(nheads):
            segs = ksegs_of_head(h)
            nseg = len(segs)
            for si, (K_ap, V_ap, kkey, vkey) in enumerate(segs):
                nkb = K_ap.shape[1] // 128
                for qg in range(NQG):
                    for kb in range(nkb):
                        its.append((h, si, qg, kb, si == 0 and kb == 0, si == nseg - 1 and kb == nkb - 1))
        segctr = {}
        chunk_id = {}
        ctr = 0
        for (h, si, qg, kb, first, last) in its:
            if (h, si) not in chunk_id:
                chunk_id[(h, si)] = ctr
                ctr += 1
        loaded = set()
        qloaded = set()

        def load_q(h):
            if h in qloaded or h >= nheads:
                return
            qloaded.add(h)
            self.dma("sp", qbuf[h % 2][0:dq, 0:nq], qsrc(h), r=[tagp + "qsrc"], w=["%s_q%d" % (tagp, h % 2)])

        def load_chunk(h, si):
            if (h, si) in loaded:
                return
            loaded.add((h, si))
            segs = ksegs_of_head(h)
            K_ap, V_ap, kkey, vkey = segs[si]
            c = chunk_id[(h, si)]
            nk = K_ap.shape[1]
            self.dma("sp", kbuf[c % 3][0:dq, 0:nk], K_ap, r=[kkey], w=["%s_k%d" % (tagp, c % 3)])
            self.dma("sp", vbuf[c % 3][:, 0:nk // 128, :], V_ap, r=[vkey], w=["%s_v%d" % (tagp, c % 3)])

        order = sorted(chunk_id.items(), key=lambda kv: kv[1])
        nxt = {}
        for i in range(len(order) - 1):
            nxt[order[i][0]] = order[i + 1][0]
        n = len(its)
        for i in range(n + LA):
            if i < n:
                h, si, qg, kb, first, last = its[i]
                load_q(h)
                load_chunk(h, si)
                if (h, si) in nxt and qg == 0 and kb == 0:
                    nh, nsi = nxt[(h, si)]
                    load_q(nh)
                    load_chunk(nh, nsi)
                c = chunk_id[(h, si)]
                nqq = min(512, nq - qg * 512)
                sb_ = self.bank[i % 4]
                self.mm(sb_[:, 0:nqq], kbuf[c % 3][0:dq, kb * 128:(kb + 1) * 128],
                        qbuf[h % 2][0:dq, qg * 512:qg * 512 + nqq], True, True,
                        r=["%s_k%d" % (tagp, c % 3), "%s_q%d" % (tagp, h % 2)], w=["bank%d" % (i % 4)])
            j = i - LA
            if j >= 0:
                h, si, qg, kb, first, last = its[j]
                c = chunk_id[(h, si)]
                nqq = min(512, nq - qg * 512)
                self.act(pbuf[j % 3][:, 0:nqq], self.bank[j % 4][:, 0:nqq], AF.Exp, scale=scale,
                         r=["bank%d" % (j % 4)], w=["%s_p%d" % (tagp, j % 3)])
                ob = self.bank[4 + qg]
                self.mm(ob[0:65, 0:nqq], vbuf[c % 3][:, kb, :], pbuf[j % 3][:, 0:nqq], first, last,
                        r=["%s_v%d" % (tagp, c % 3), "%s_p%d" % (tagp, j % 3)], w=["bank%d" % (4 + qg)])
                if last:
                    bk = "bank%d" % (4 + qg)
                    self.cp("act", osb[0:65, 0:nqq], ob[0:65, 0:nqq], r=[bk], w=[tagp + "_osb"])
                    self.recip(rl[64:65, 0:nqq], osb[64:65, 0:nqq], r=[tagp + "_osb"], w=[tagp + "_rl"])
                    self.cp("dve", rlh[64:65, 0:nqq], rl[64:65, 0:nqq], r=[tagp + "_rl"], w=[tagp + "_rlh"])
                    self.tt("dve", rl[64:65, 0:nqq], rl[64:65, 0:nqq], rlh[64:65, 0:nqq], ALU.subtract,
                            r=[tagp + "_rl", tagp + "_rlh"], w=[tagp + "_rl"])
                    self.cp("dve", rll[64:65, 0:nqq], rl[64:65, 0:nqq], r=[tagp + "_rl"], w=[tagp + "_rll"])
                    self.mmk(ob[0:64, 0:nqq], [self.C["ones_bf"][64:65, 0:64], self.C["ones_bf"][64:65, 0:64]],
                             [rlh[64:65, 0:nqq], rll[64:65, 0:nqq]],
                             r=[tagp + "_rlh", tagp + "_rll", tagp + "_osb", "C_ones_bf"], w=[bk])
                    q0 = out_off + qg * 512
                    if h % 2 == 0:
                        self.tt("dve", out_tile[0:64, h // 2, q0:q0 + nqq], osb[0:64, 0:nqq], ob[0:64, 0:nqq], ALU.mult,
                                r=[bk, tagp + "_osb"], w=[out_key])
                    else:
                        self.tt("dve", atmp[0:64, 0:nqq], osb[0:64, 0:nqq], ob[0:64, 0:nqq], ALU.mult,
                                r=[bk, tagp + "_osb"], w=[tagp + "_atmp"])
                        self.cp("dve", out_tile[64:128, h // 2, q0:q0 + nqq], atmp[0:64, 0:nqq],
                                r=[tagp + "_atmp"], w=[out_key])


class Arena:
    def __init__(self, gen, nbytes):
        self.t = gen.sb("arena", [128, nbytes // 4], F32)
        self.n = nbytes
        self.off = 0
        self.peak = 0
        self.base = 0

    def reset(self):
        self.off = self.base

    def alloc(self, shape, dt):
        free = 1
        for s in shape[1:]:
            free *= s
        nb = free * (2 if dt == BF16 else 4)
        nb_al = (nb + 63) // 64 * 64
        assert self.off + nb_al <= self.n, ("arena overflow", self.off, nb_al, self.n)
        a = self.off // 4
        ap = self.t[:, a:a + nb_al // 4]
        self.off += nb_al
        self.peak = max(self.peak, self.off)
        if dt == BF16:
            ap = ap.bitcast(BF16)
        ap = ap[0:shape[0], 0:free]
        if len(shape) == 3:
            ap = ap.rearrange("p (a b) -> p a b", b=shape[2])
        elif len(shape) == 4:
            ap = ap.rearrange("p (a b c) -> p a b c", b=shape[2], c=shape[3])
        return ap


def _gen_finish(self):
    S = self.S
    outs = [o for o in S.ops if o.dma]
    S.pend_barrier["sp"] = S.pend_barrier["sp"] + outs + [o for o in S.last_op.values() if o is not None]
    S.add("sp", lambda e: None)
    S.finalize(self.nc)
    nc = self.nc
    with nc.Block() as block:
        @block.tensor
        def _(e):
            S.emit("pe", e)

        @block.scalar
        def _(e):
            S.emit("act", e)

        @block.vector
        def _(e):
            S.emit("dve", e)

        @block.gpsimd
        def _(e):
            S.emit("pool", e)

        @block.sync
        def _(e):
            S.emit("sp", e)


Gen.finish = _gen_finish


def _setup_common(self):
    g = self
    TL, T = g.TL, g.T
    g.load_consts()
    g.epsc = g.sb("epsc", [128, 1], F32)
    g.memset("pool", g.epsc[:], EPS, w=["epsc"])
    g.xT = g.sb("xT", [128, KT, TL], F32)
    g.cT = g.sb("cT", [128, KT, LC], F32)
    g.hT = g.sb("hT", [128, KT, T], BF16)
    g.cvec = g.sb("cvec_sb", [128, KT, 2], F32)
    g.scvec = g.sb("scvec_sb", [128, KT, 2], F32)
    xin = g.inp("xT_in", [128, KT, TL])
    cin = g.inp("cT_in", [128, KT, LC])
    cv = g.inp("cvec", [128, KT, 2])
    for k in range(KT):
        g.dma("sp", g.xT[:, k, :], xin[:, k, :], w=["xT.g%d" % i for i in range(len(g.groups) - 1)])
    g.dma("sp", g.cT[:], cin, w=["xT.g%d" % (len(g.groups) - 1)])
    g.dma("sp", g.cvec[:], cv, w=["cvec"])
    g.act(g.scvec[:], g.cvec[:], AF.Silu, r=["cvec"], w=["scvec"])
    rem = g.nc.sbuf_bytes_remaining
    g.ar = Arena(g, (rem - 14336) // 64 * 64)
    g.S.barrier()


def _res_src(self):
    g = self

    def src_fn(k, t0, n):
        if t0 >= g.TL:
            return g.cT[:, k, t0 - g.TL:t0 - g.TL + n]
        return g.xT[:, k, t0:t0 + n]
    return src_fn, (lambda gi: ["xT.g%d" % gi])


def _res_tile(self, k, t0, n):
    if t0 >= self.TL:
        return self.cT[:, k, t0 - self.TL:t0 - self.TL + n]
    return self.xT[:, k, t0:t0 + n]


def _layer0(self):
    g = self
    S = g.S
    TL, T, NCH = g.TL, g.T, g.NCH
    ar = g.ar
    W = {}
    for nm, shape in [("l0_ada_w", [D, 6 * D]), ("l0_ada_b", [6 * D]), ("l0_norm1_g", [D]), ("l0_norm2_g", [D]),
                      ("l0_w_in", [D, L0_IN]), ("l0_ret_log_decay", [2, 8]), ("l0_q_norm_g", [64]),
                      ("l0_k_norm_g", [64]), ("l0_w_out", [D, D]), ("l0_ffn_w1", [D, FFN]),
                      ("l0_ffn_w3", [D, FFN]), ("l0_ffn_w2", [FFN, D])]:
        W[nm] = g.inp(nm, shape)
    modT = g.ada(0, W["l0_ada_w"], W["l0_ada_b"], g.scvec)
    n1g = g.gain_cols("n1g0", W["l0_norm1_g"], 8)
    n2g = g.gain_cols("n2g0", W["l0_norm2_g"], 8)
    M = g.mod_derived(0, modT, n1g, n2g)
    gq = g.gain_cols("gq0", None, 1, pieces=[(0, 64, W["l0_q_norm_g"]), (64, 128, W["l0_q_norm_g"])])
    gk = g.gain_cols("gk0", None, 1, pieces=[(0, 64, W["l0_k_norm_g"]), (64, 128, W["l0_k_norm_g"])])
    lgb = g.sb("lgb", [128, 16], F32)
    g.dma("sp", lgb[:], W["l0_ret_log_decay"].rearrange("a b -> (a b)").partition_broadcast(128), w=["lgb"])
    lgp = g.sb("lgp", [128, 2, 4], F32)
    for dr in range(2):
        for half in range(2):
            src = lgb[half * 64:(half + 1) * 64, dr * 8 + half:dr * 8 + 8:2]
            g.cp("dve", lgp[half * 64:(half + 1) * 64, dr, :], src, r=["lgb"], w=["lgp"])
    GP = g.sb("GP", [128, 2, 4], F32)
    g.act(GP[:], lgp[:], AF.Exp, scale=128.0, r=["lgp"], w=["GP"])

    ar.reset()
    sq = ar.alloc([128, KT, 512], BF16)
    rstd = ar.alloc([128, 512], F32)
    tmpf = [ar.alloc([128, 512], F32) for _ in range(2)]
    src_fn, src_keys = g.res_src()
    g.norm_mod(src_fn, src_keys, g.hT, "hT", M["gs1"], M["sh1"], M["key"], (sq, rstd, tmpf))
    S.barrier()

    ar.reset()
    hkeys = ["hT.g%d" % i for i in range(len(g.groups))]
    wq = ar.alloc([128, KT, 512], BF16)
    wkv = ar.alloc([128, KT, 256], BF16)
    win = W["l0_w_in"].rearrange("(k p) c -> p k c", p=128)
    g.dma("pool", wq, win[:, :, 2048:2560], w=["wq"])
    g.dma("pool", wkv, win[:, :, 2560:2816], w=["wkv"])
    cosb = ar.alloc([128, 512], F32)
    sinb = ar.alloc([128, 512], F32)
    hn_tmp = (ar.alloc([128, 512], BF16), ar.alloc([128, 512], F32), ar.alloc([128, 512], F32),
              ar.alloc([128, 512], BF16), ar.alloc([128, 512], F32))
    obuf = [ar.alloc([128, 512], BF16) for _ in range(2)]
    vaug = [ar.alloc([128, 2, 65], BF16) for _ in range(2)]
    for i in range(2):
        g.memset("pool", vaug[i], 1.0, w=["vaug%d" % i])
    g.Q0 = g.scratch("Q0", [8, 64, T], BF16)
    if g.mode == "pre0":
        g.K0own = g.outp("K0own", [2, 64, TL], BF16)
        g.V0own = g.outp("V0own", [2, 128, NCH, 65], BF16)
        g.K0ctx = g.outp("K0ctx", [2, 64, LC], BF16)
        g.V0ctx = g.outp("V0ctx", [2, 128, LC // 128, 65], BF16)
    else:
        g.K0own = g.scratch("K0own", [2, 64, TL], BF16)
        g.V0own = g.scratch("V0own", [2, 128, NCH, 65], BF16)
        g.K0ctx = g.scratch("K0ctx", [2, 64, LC], BF16)
        g.V0ctx = g.scratch("V0ctx", [2, 128, LC // 128, 65], BF16)
    cnt = 0
    for gi, (t0, n, v) in enumerate(g.groups):
        rope = None
        if v == 0:
            g.dma("sp", cosb[:, 0:n], g.C_dram["cos0"][:, t0:t0 + n], w=["cosb"])
            g.dma("sp", sinb[:, 0:n], g.C_dram["sin0"][:, t0:t0 + n], w=["sinb"])
            rope = (cosb[0:64, 0:n], sinb[0:64, 0:n], g.C["rsw"], ["cosb", "sinb", "C_rsw"])
        for hh in range(10):
            pb = g.bank[hh % 2]
            bk = "bank%d" % (hh % 2)
            if hh < 2:
                wsl = [wkv[:, k, hh * 64:(hh + 1) * 64] for k in range(KT)]
                wk_ = "wkv"
                gain, gkey = gk, "gk0"
            else:
                wsl = [wq[:, k, (hh - 2) * 64:(hh - 1) * 64] for k in range(KT)]
                wk_ = "wq"
                gain, gkey = gq, "gq0"
            g.mmk(pb[0:64, 0:n], wsl, [g.hT[:, k, t0:t0 + n] for k in range(KT)], r=[wk_, "hT.g%d" % gi], w=[bk])
            ob = obuf[cnt % 2]
            okey = "obuf%d" % (cnt % 2)
            cnt += 1
            g.headnorm(64, n, pb[0:64, 0:n], bk, g.C["bo64"], "C_bo64", gain[0:64, 0:1], gkey, ob[0:64, 0:n], okey,
                       rope=rope, pbank=2, tmp=hn_tmp)
            if hh < 2:
                dst = g.K0own[hh, :, t0:t0 + n] if v == 0 else g.K0ctx[hh, :, :]
                g.dma("sp", dst, ob[0:64, 0:n], r=[okey], w=["K0own" if v == 0 else "K0ctx"])
            else:
                g.dma("sp", g.Q0[hh - 2, :, t0:t0 + n], ob[0:64, 0:n], r=[okey], w=["Q0"])
        for tt_ in range(n // 128):
            c0 = t0 + tt_ * 128
            pb = g.bank[3]
            g.mmk(pb[:, 0:128], [g.hT[:, k, c0:c0 + 128] for k in range(KT)], [wkv[:, k, 128:256] for k in range(KT)],
                  r=["wkv", "hT.g%d" % gi], w=["bank3"])
            va = vaug[tt_ % 2]
            g.cp("act", va[:, :, 0:64], pb[:, 0:128].rearrange("p (a b) -> p a b", b=64), r=["bank3"], w=["vaug%d" % (tt_ % 2)])
            for kvh in range(2):
                if v == 0:
                    dst = g.V0own[kvh, :, c0 // 128, :]
                else:
                    dst = g.V0ctx[kvh, :, (c0 - TL) // 128, :]
                g.dma("sp", dst, va[:, kvh, :], r=["vaug%d" % (tt_ % 2)], w=["V0own" if v == 0 else "V0ctx"])
    S.barrier()

    ar.base = 0
    ar.reset()
    g.raT = ar.alloc([128, 4, T], BF16)
    ar.base = ar.off
    ub_store = ar.alloc([128, 4, NCH + 2, 128], BF16)
    wrp = [ar.alloc([128, KT, 512], BF16) for _ in range(2)]
    ZF = ar.alloc([128, 2, 128], F32)
    kz = [ar.alloc([128, 128], BF16) for _ in range(2)]
    vb = ar.alloc([128, 128], BF16)
    Uacc = ar.alloc([128, 2, 4, 128], F32)
    sctx = ar.alloc([128, 2, 4, 128], F32)
    ut = ar.alloc([128, 2, 128], F32)
    posf = ar.alloc([128, 64], F32)
    posb = ar.alloc([128, 64], F32)
    g.dma("sp", posf, g.C_dram["posf"], w=["posf"])
    g.dma("sp", posb, g.C_dram["posb"], w=["posb"])
    g.memset("pool", Uacc, 0.0, w=["Uacc"])
    g.memset("pool", sctx, 0.0, w=["sctx"])
    if g.mode == "pre0":
        g.Uown = g.outp("Uown", [128, 2, 4, 128], F32)
    else:
        g.Uown = g.scratch("Uown", [128, 2, 4, 128], F32)

    def load_wrp(p):
        wb = wrp[p % 2]
        for qi in range(4):
            g.dma("pool", wb[:, :, qi * 128:(qi + 1) * 128], win[:, :, qi * 512 + p * 128:qi * 512 + (p + 1) * 128],
                  w=["wrp%d_%d" % (p % 2, qi)])
    g.load_wrp = load_wrp
    chunks = [(j * 128, j, 0) for j in range(NCH)] + [(TL + j * 128, NCH + j, 1) for j in range(2)]
    g.chunks = chunks
    load_wrp(0)
    for p in range(4):
        if p + 1 < 4:
            load_wrp(p + 1)
        wb = wrp[p % 2]
        wk_ = "wrp%d" % (p % 2)
        for dr, pos, pkey in ((0, posf, "posf"), (1, posb, "posb")):
            for half in range(2):
                g.act(ZF[:, dr, half * 64:(half + 1) * 64], pos[:, :], AF.Exp,
                      scale=lgb[:, dr * 8 + 2 * p + half:dr * 8 + 2 * p + half + 1], r=[pkey, "lgb"], w=["ZF"])
        g.ts("dve", ZF[:, :, :], ZF[:, :, :], 0.125, None, ALU.mult, r=["ZF"], w=["ZF"])
        for (c0, j, isctx) in chunks:
            gi = min(c0 // 512, len(g.groups) - 1) if not isctx else len(g.groups) - 1
            hk = "hT.g%d" % gi
            kp = g.bank[0]
            vp = g.bank[1]
            g.mmk(kp[:, 0:128], [g.hT[:, k, c0:c0 + 128] for k in range(KT)], [wb[:, k, 128:256] for k in range(KT)],
                  r=[wk_ + "_1", hk], w=["bank0"])
            g.mmk(vp[:, 0:128], [g.hT[:, k, c0:c0 + 128] for k in range(KT)], [wb[:, k, 256:384] for k in range(KT)],
                  r=[wk_ + "_2", hk], w=["bank1"])
            g.tt("dve", kz[0][:, :], kp[:, 0:128], ZF[:, 0, :], ALU.mult, r=["bank0", "ZF"], w=["kz0"])
            g.tt("dve", kz[1][:, :], kp[:, 0:128], ZF[:, 1, :], ALU.mult, r=["bank0", "ZF"], w=["kz1"])
            g.cp("act", vb[:, :], vp[:, 0:128], r=["bank1"], w=["vb"])
            up = g.bank[2]
            for dr in range(2):
                g.mm(up[:, dr * 128:(dr + 1) * 128], kz[dr][:, :], vb[:, :], True, True, r=["kz%d" % dr, "vb"], w=["bank2"])
            for dr in range(2):
                g.tt("dve", ut[:, dr, :], up[:, dr * 128:(dr + 1) * 128], g.C["bdmask"][:, :], ALU.mult,
                     r=["bank2", "C_bdmask"], w=["ut"])
            g.cp("pool", ub_store[:, p, j, :], ut[:, 1, :], r=["ut"], w=["ub_store"])
            acc = sctx if isctx else Uacc
            akey = "sctx" if isctx else "Uacc"
            g.stt("dve", acc[:, 0, p, :], acc[:, 0, p, :], GP[:, 0, p:p + 1], ut[:, 0, :], ALU.mult, ALU.add,
                  r=[akey, "GP", "ut"], w=[akey])
        for (lo, hi, acc, akey) in ((0, NCH, Uacc, "Uacc"), (NCH, NCH + 2, sctx, "sctx")):
            for j in range(hi - 1, lo - 1, -1):
                g.stt("dve", acc[:, 1, p, :], acc[:, 1, p, :], GP[:, 1, p:p + 1], ub_store[:, p, j, :], ALU.mult, ALU.add,
                      r=[akey, "GP", "ub_store"], w=[akey])
    g.dma("sp", g.Uown, Uacc, r=["Uacc"], w=["Uown"])
    g.ub_store, g.sctx, g.lgb, g.lgp, g.GP, g.M0, g.W0 = ub_store, sctx, lgb, lgp, GP, M, W
    g.Uacc = Uacc
    g.wrp = wrp
    g.ar_mark = ar.off


Gen.setup_common = _setup_common
Gen.res_src = _res_src
Gen.res_tile = _res_tile
Gen.layer0_pre = _layer0


def _layer0_main(self, stop=None):
    g = self
    S = g.S
    TL, T, NCH = g.TL, g.T, g.NCH
    ar = g.ar
    M, W = g.M0, g.W0
    lgb, lgp, GP = g.lgb, g.lgp, g.GP
    ub_store, sctx = g.ub_store, g.sctx
    wrp = g.wrp
    if g.mode == "fused":
        if stop == "4ax":
            g.stage_tst = g.scratch("stage_tst", [D, 128], F32)
            g.dma("sp", g.stage_tst, W["l0_w_in"][:, 0:128], w=["stage_tst"])
        S.barrier()
        rg = [list(range(NCORES))]
        nk, nv, nu = 2 * 64 * TL, 2 * 128 * NCH * 65, 128 * 1024
        k2 = g.scratch("K0all2d", [NCORES * nk // 512, 512], BF16)
        v2 = g.scratch("V0all2d", [NCORES * nv // 512, 512], BF16)
        u2 = g.scratch("Uall2d", [NCORES * nu // 256, 256], F32)
        for (src, dst, key) in ((g.K0own.rearrange("k d (a b) -> (k d a) b", b=512), k2, "K0all"),
                                (g.V0own.rearrange("k p j c -> (k p j c)").rearrange("(a b) -> a b", b=512), v2, "V0all"),
                                (g.Uown.rearrange("p a b c -> (p a b c)").rearrange("(x d) -> x d", d=256), u2, "Uall")):
            S.add("pool", lambda e, s_=src, d_=dst: e.collective_compute(
                "AllGather", ALU.bypass, replica_groups=rg, ins=[s_.opt()], outs=[d_.opt()]),
                ["K0own", "V0own", "Uown"], [key], dma=True, tag="cc")
        g.K0all = k2.rearrange("(r k d a) b -> k r d (a b)", r=NCORES, k=2, d=64)
        g.V0all = v2.rearrange("a b -> (a b)").rearrange("(r k p j c) -> k r p j c", r=NCORES, k=2, p=128, c=65)
        g.Uall = u2.rearrange("x d -> (x d)").rearrange("(r p a b c) -> r p a b c", r=NCORES, p=128, a=2, b=4)
    else:
        g.K0all = g.inp("K0all", [2, NCORES, 64, TL], BF16)
        g.V0all = g.inp("V0all", [2, NCORES, 128, NCH, 65], BF16)
        g.Uall = g.inp("Uall", [NCORES, 128, 2, 4, 128], F32)
    S.barrier()
    ar.off = g.ar_mark
    Sin = g.Uacc
    coef = ar.alloc([128, 2, 4, NCORES], F32)
    coefc = ar.alloc([128, 2, 4], F32)
    ex = ar.alloc([128, 4], F32)
    utmp = [ar.alloc([128, 2, 4, 128], F32) for _ in range(1)]
    cpos = g.C["cpos"]
    for dr in range(2):
        for r_ in range(NCORES):
            g.ts("dve", ex[:, :], lgp[:, dr, :], cpos[:, dr * 16 + r_:dr * 16 + r_ + 1], None, ALU.mult,
                 r=["lgp", "C_cpos"], w=["ex"])
            g.act(ex[:, :], ex[:, :], AF.Exp, r=["ex"], w=["ex"])
            g.ts("dve", coef[:, dr, :, r_], ex[:, :], cpos[:, dr * 16 + 8 + r_:dr * 16 + 9 + r_], None, ALU.mult,
                 r=["ex", "C_cpos"], w=["coef"])
        g.ts("dve", ex[:, :], lgp[:, dr, :], cpos[:, 32 + dr:33 + dr], None, ALU.mult, r=["lgp", "C_cpos"], w=["ex"])
        g.act(coefc[:, dr, :], ex[:, :], AF.Exp, r=["ex"], w=["coefc"])
        for p in range(4):
            g.ts("dve", Sin[:, dr, p, :], sctx[:, dr, p, :], coefc[:, dr, p:p + 1], None, ALU.mult,
                 r=["sctx", "coefc"], w=["Uacc"])
    for r_ in range(NCORES):
        ub = utmp[0]
        g.dma("sp", ub, g.Uall[r_], w=["utmp0"])
        for dr in range(2):
            for p in range(4):
                g.stt("dve", Sin[:, dr, p, :], ub[:, dr, p, :], coef[:, dr, p, r_:r_ + 1], Sin[:, dr, p, :],
                      ALU.mult, ALU.add, r=["utmp0", "coef", "Uacc"], w=["Uacc"])
    Rb = ar.alloc([128, 128], F32)
    tf = ar.alloc([128, 128], F32)
    for p in range(4):
        for (lo, hi, init) in ((0, NCH, True), (NCH, NCH + 2, False)):
            if init:
                g.cp("dve", Rb[:, :], Sin[:, 1, p, :], r=["Uacc"], w=["Rb"])
            else:
                g.memset("dve", Rb[:, :], 0.0, w=["Rb"])
            for j in range(hi - 1, lo - 1, -1):
                g.cp("dve", tf[:, :], ub_store[:, p, j, :], r=["ub_store"], w=["tf"])
                g.cp("dve", ub_store[:, p, j, :], Rb[:, :], r=["Rb", "tf"], w=["ub_store"])
                g.stt("dve", Rb[:, :], Rb[:, :], GP[:, 1, p:p + 1], tf[:, :], ALU.mult, ALU.add,
                      r=["Rb", "tf", "GP", "ub_store"], w=["Rb"])
    if stop == "4a":
        return
    if stop == "4ap":
        g.load_wrp(0)
        return
    if stop == "4ax":
        tst = ar.alloc([128, KT, 128], F32)
        g.dma("sp", tst, g.stage_tst.rearrange("(k p) c -> p k c", p=128), w=["tst"])
        return
    if stop == "4as":
        tst = ar.alloc([128, KT, 128], F32)
        g.dma("sp", tst, W["l0_w_in"].rearrange("(k p) c -> p k c", p=128)[:, :, 0:128], w=["tst"])
        return
    ramp = [ar.alloc([128, 128], F32) for _ in range(2)]
    rel = [ar.alloc([128, 128], F32) for _ in range(2)]
    ind = [ar.alloc([128, 128], F32) for _ in range(2)]
    for i, (a, b, c) in enumerate((("rampf", "relf", "indf"), ("rampb", "relb", "indb"))):
        g.dma("sp", ramp[i], g.C_dram[a], w=["ramp%d" % i])
        g.dma("sp", rel[i], g.C_dram[b], w=["rel%d" % i])
        g.dma("sp", ind[i], g.C_dram[c], w=["ind%d" % i])
    posf = ar.alloc([128, 64], F32)
    g.dma("sp", posf, g.C_dram["posf"], w=["posf2"])
    XI = ar.alloc([128, 2, 128], F32)
    MASK = ar.alloc([128, 2, 128], F32)
    mt = ar.alloc([128, 128], F32)
    ZFm = ar.alloc([128, 128], F32)
    qTb = ar.alloc([128, 128], BF16)
    qx = [ar.alloc([128, 128], BF16) for _ in range(2)]
    kTb = ar.alloc([128, 128], BF16)
    sg = ar.alloc([128, 128], F32)
    kzf = ar.alloc([128, 128], BF16)
    vbb = ar.alloc([128, 128], BF16)
    vpad = ar.alloc([128, 2, 128], BF16)
    ATb = ar.alloc([128, 256], BF16)
    Sf = ar.alloc([128, 128], F32)
    Sfb = [ar.alloc([128, 128], BF16) for _ in range(2)]
    of = ar.alloc([128, 128], F32)
    obf = ar.alloc([128, 128], BF16)
    cen = ar.alloc([128, 128], F32)
    sqc = ar.alloc([128, 128], BF16)
    rs = ar.alloc([128, 128], F32)
    yy = ar.alloc([128, 128], F32)
    g.memset("pool", vpad, 0.0, w=["vpad"])
    bo = g.C["bo64"]
    it = 0
    for p in range(4):
        g.load_wrp(p)
        wb = wrp[p % 2]
        wk = ["wrp%d_%d" % (p % 2, qi) for qi in range(4)]
        for dr in range(2):
            g.act(XI[:, dr, :], ramp[dr][:, :], AF.Exp, scale=lgp[:, dr, p:p + 1], r=["ramp%d" % dr, "lgp"], w=["XI"])
        for half in range(2):
            hcol = 2 * p + half
            g.act(MASK[:, half, :], rel[0][:, :], AF.Exp, scale=lgb[:, hcol:hcol + 1], r=["rel0", "lgb"], w=["MASK"])
            g.tt("dve", MASK[:, half, :], MASK[:, half, :], ind[0][:, :], ALU.mult, r=["MASK", "ind0"], w=["MASK"])
            g.act(mt[:, :], rel[1][:, :], AF.Exp, scale=lgb[:, 8 + hcol:9 + hcol], r=["rel1", "lgb"], w=["mt"])
            g.tt("dve", mt[:, :], mt[:, :], ind[1][:, :], ALU.mult, r=["mt", "ind1"], w=["mt"])
            g.tt("dve", MASK[:, half, :], MASK[:, half, :], mt[:, :], ALU.add, r=["MASK", "mt"], w=["MASK"])
            g.act(ZFm[:, half * 64:(half + 1) * 64], posf[:, :], AF.Exp, scale=lgb[:, hcol:hcol + 1],
                  r=["posf2", "lgb"], w=["ZFm"])
        g.ts("dve", ZFm[:, :], ZFm[:, :], 0.125, None, ALU.mult, r=["ZFm"], w=["ZFm"])
        for (c0, j, isctx) in g.chunks:
            gi = min(c0 // 512, len(g.groups) - 1) if not isctx else len(g.groups) - 1
            hk = "hT.g%d" % gi
            if j == 0:
                g.cp("dve", Sf[:, :], Sin[:, 0, p, :], r=["Uacc"], w=["Sf"])
            if j == NCH:
                g.memset("dve", Sf[:, :], 0.0, w=["Sf"])
            sfb = Sfb[it % 2]
            sfk = "Sfb%d" % (it % 2)
            it += 1
            g.cp("act", sfb[:, :], Sf[:, :], r=["Sf"], w=[sfk])
            hsl = [g.hT[:, k, c0:c0 + 128] for k in range(KT)]
            b0, b1, b2, b3, b4 = g.bank[0], g.bank[1], g.bank[2], g.bank[3], g.bank[4]
            b5, b6, b7 = g.bank[5], g.bank[6], g.bank[7]
            g.mmk(b0[:, 0:128], [wb[:, k, 0:128] for k in range(KT)], hsl, r=[wk[0], hk], w=["bank0"])
            g.mmk(b5[:, 0:128], [wb[:, k, 128:256] for k in range(KT)], hsl, r=[wk[1], hk], w=["bank5"])
            g.mmk(b5[:, 128:256], [wb[:, k, 384:512] for k in range(KT)], hsl, r=[wk[3], hk], w=["bank5"])
            g.mmk(b1[:, 0:128], hsl, [wb[:, k, 128:256] for k in range(KT)], r=[wk[1], hk], w=["bank1"])
            g.mmk(b6[:, 0:128], hsl, [wb[:, k, 256:384] for k in range(KT)], r=[wk[2], hk], w=["bank6"])
            g.cp("dve", qTb[:, :], b0[:, 0:128], r=["bank0"], w=["qTb"])
            for dr in range(2):
                g.tt("dve", qx[dr][:, :], b0[:, 0:128], XI[:, dr, :], ALU.mult, r=["bank0", "XI"], w=["qx%d" % dr])
            g.act(kTb[:, :], b5[:, 0:128], AF.Identity, scale=0.125, r=["bank5"], w=["kTb"])
            g.act(sg[:, :], b5[:, 128:256], AF.Silu, r=["bank5"], w=["sg"])
            g.tt("dve", kzf[:, :], b1[:, 0:128], ZFm[:, :], ALU.mult, r=["bank1", "ZFm"], w=["kzf"])
            g.cp("act", vbb[:, :], b6[:, 0:128], r=["bank6"], w=["vbb"])
            g.cp("pool", vpad[:, 0, 0:64], vbb[:, 0:64], r=["vbb"], w=["vpad"])
            g.cp("pool", vpad[:, 1, 64:128], vbb[:, 64:128], r=["vbb"], w=["vpad"])
            g.mm(b2[:, 0:128], kTb[0:64, :], qTb[0:64, :], True, True, r=["kTb", "qTb"], w=["bank2"])
            g.mm(b7[:, 0:128], kTb[64:128, :], qTb[64:128, :], True, True, r=["kTb", "qTb"], w=["bank7"])
            g.tt("dve", ATb[:, 0:128], b2[:, 0:128], MASK[:, 0, :], ALU.mult, r=["bank2", "MASK"], w=["ATb"])
            g.tt("dve", ATb[:, 128:256], b7[:, 0:128], MASK[:, 1, :], ALU.mult, r=["bank7", "MASK"], w=["ATb"])
            g.mm(b3[:, 0:128], vpad[:, 0, :], ATb[:, 0:128], True, False, r=["vpad", "ATb"], w=["bank3"])
            g.mm(b3[:, 0:128], vpad[:, 1, :], ATb[:, 128:256], False, False, r=["vpad", "ATb"], w=["bank3"])
            g.mm(b3[:, 0:128], sfb[:, :], qx[0][:, :], False, False, r=[sfk, "qx0"], w=["bank3"])
            g.mm(b3[:, 0:128], ub_store[:, p, j, :], qx[1][:, :], False, True, r=["ub_store", "qx1"], w=["bank3"])
            g.mm(b2[:, 256:384], kzf[:, :], vbb[:, :], True, True, r=["kzf", "vbb", "ATb"], w=["bank2"])
            g.tt("dve", mt[:, :], b2[:, 256:384], g.C["bdmask"][:, :], ALU.mult, r=["bank2", "C_bdmask"], w=["mt"])
            g.stt("dve", Sf[:, :], Sf[:, :], GP[:, 0, p:p + 1], mt[:, :], ALU.mult, ALU.add, r=["Sf", "mt", "GP", sfk], w=["Sf"])
            g.cp("act", of[:, :], b3[:, 0:128], r=["bank3"], w=["of"])
            g.cp("pool", obf[:, :], of[:, :], r=["of"], w=["obf"])
            g.mm(b4[:, 0:128], bo[:, :], obf[:, :], True, True, r=["obf", "C_bo64"], w=["bank4"])
            g.tt("dve", cen[:, :], of[:, :], b4[:, 0:128], ALU.subtract, r=["of", "bank4"], w=["cen"])
            g.tt("pool", sqc[:, :], cen[:, :], cen[:, :], ALU.mult, r=["cen"], w=["sqc"])
            g.mm(b4[:, 128:256], bo[:, :], sqc[:, :], True, True, r=["sqc", "C_bo64", "cen"], w=["bank4"])
            g.act(rs[:, :], b4[:, 128:256], AF.Sqrt, bias=g.epsc[:, 0:1], r=["bank4", "epsc"], w=["rs"])
            g.recip(rs[:, :], rs[:, :], r=["rs"], w=["rs"])
            g.tt("dve", yy[:, :], cen[:, :], rs[:, :], ALU.mult, r=["cen", "rs"], w=["yy"])
            g.tt("pool", g.raT[:, p, c0:c0 + 128], yy[:, :], sg[:, :], ALU.mult, r=["yy", "sg"], w=["raT"])
    S.barrier()
    if stop == "4b":
        return

    ar.reset()
    g.atT = ar.alloc([128, 4, T], BF16)
    ar.base = ar.off
    CK = min(1024, TL)
    tmp = ([ar.alloc([128, T], BF16) for _ in range(2)], [ar.alloc([128, CK], BF16) for _ in range(3)],
           [ar.alloc([128, CK // 128, 65], BF16) for _ in range(3)], [ar.alloc([128, 512], BF16) for _ in range(3)],
           ar.alloc([128, 512], F32), ar.alloc([128, 512], F32), ar.alloc([128, 512], BF16),
           ar.alloc([128, 512], BF16), ar.alloc([128, 512], BF16))

    def ksegs_lat(h):
        kvh = h // 4
        segs = []
        for r_ in range(NCORES):
            for c in range(TL // CK):
                segs.append((g.K0all[kvh, r_, :, c * CK:(c + 1) * CK],
                             g.V0all[kvh, r_, :, c * CK // 128:(c + 1) * CK // 128, :], "K0all", "V0all"))
        segs.append((g.K0ctx[kvh], g.V0ctx[kvh], "K0ctx", "V0ctx"))
        return segs

    g.attention("a0", 64, 0.125, 8, lambda h: g.Q0[h, :, 0:TL], ksegs_lat, TL, g.atT, "atT", 0, tmp)
    g.attention("a0", 64, 0.125, 8, lambda h: g.Q0[h, :, TL:T],
                lambda h: [(g.K0ctx[h // 4], g.V0ctx[h // 4], "K0ctx", "V0ctx")], LC, g.atT, "atT", TL, tmp)
    S.barrier()
    if stop == "5":
        return

    ar.reset()
    wo = ar.alloc([128, KT, D], BF16)
    g.dma("pool", wo, W["l0_w_out"].rearrange("(k p) c -> p k c", p=128), w=["wo"])
    for gi, (t0, n, v) in enumerate(g.groups):
        cat = [g.raT[:, kt, t0:t0 + n] for kt in range(4)] + [g.atT[:, kt, t0:t0 + n] for kt in range(4)]
        for dc in range(8):
            pb = g.bank[dc % 4]
            bk = "bank%d" % (dc % 4)
            g.mmk(pb[:, 0:n], [wo[:, kt, dc * 128:(dc + 1) * 128] for kt in range(8)], cat, r=["wo", "raT", "atT"], w=[bk])
            res = g.res_tile(dc, t0, n)
            g.stt("dve", res, pb[:, 0:n], M["gate1"][:, dc, v:v + 1], res, ALU.mult, ALU.add,
                  r=[bk, "xT.g%d" % gi] + M["key"], w=["xT.g%d" % gi])
    S.barrier()
    if stop == "6":
        return

    ar.reset()
    sq = ar.alloc([128, KT, 512], BF16)
    rstd = ar.alloc([128, 512], F32)
    tmpf = [ar.alloc([128, 512], F32) for _ in range(2)]
    src_fn, src_keys = g.res_src()
    g.norm_mod(src_fn, src_keys, g.hT, "hT", M["gs2"], M["sh2"], M["key"], (sq, rstd, tmpf))
    FC = 256
    NFC = FFN // FC
    w1v = W["l0_ffn_w1"].rearrange("(k p) f -> p k f", p=128)
    w3v = W["l0_ffn_w3"].rearrange("(k p) f -> p k f", p=128)
    w2v = W["l0_ffn_w2"].rearrange("(ft p) d -> p ft d", p=128)
    w1c = [ar.alloc([128, KT, FC], BF16) for _ in range(2)]
    w3c = [ar.alloc([128, KT, FC], BF16) for _ in range(2)]
    w2c = [ar.alloc([128, FC // 128, D], BF16) for _ in range(2)]
    gT = [ar.alloc([128, FC // 128, 512], BF16) for _ in range(2)]
    sil = [ar.alloc([128, 512], F32) for _ in range(2)]

    def load_fc(fc):
        i = fc % 2
        g.dma("pool", w1c[i], w1v[:, :, fc * FC:(fc + 1) * FC], w=["w1c%d" % i])
        g.dma("pool", w3c[i], w3v[:, :, fc * FC:(fc + 1) * FC], w=["w3c%d" % i])
        g.dma("pool", w2c[i], w2v[:, fc * (FC // 128):(fc + 1) * (FC // 128), :], w=["w2c%d" % i])
    load_fc(0)
    cnt = 0
    for fc in range(NFC):
        if fc + 1 < NFC:
            load_fc(fc + 1)
        i = fc % 2
        for gi, (t0, n, v) in enumerate(g.groups):
            hk = "hT.g%d" % gi
            gt = gT[cnt % 2]
            gk_ = "gT%d" % (cnt % 2)
            cnt += 1
            hsl = [g.hT[:, k, t0:t0 + n] for k in range(KT)]
            for ft in range(FC // 128):
                h1, h3 = g.bank[2 * ft], g.bank[2 * ft + 1]
                g.mmk(h1[:, 0:n], [w1c[i][:, k, ft * 128:(ft + 1) * 128] for k in range(KT)], hsl,
                      r=["w1c%d" % i, hk], w=["bank%d" % (2 * ft)])
                g.mmk(h3[:, 0:n], [w3c[i][:, k, ft * 128:(ft + 1) * 128] for k in range(KT)], hsl,
                      r=["w3c%d" % i, hk], w=["bank%d" % (2 * ft + 1)])
                g.act(sil[ft][:, 0:n], h1[:, 0:n], AF.Silu, r=["bank%d" % (2 * ft)], w=["sil%d" % ft])
                g.tt("dve", gt[:, ft, 0:n], sil[ft][:, 0:n], h3[:, 0:n], ALU.mult,
                     r=["sil%d" % ft, "bank%d" % (2 * ft + 1)], w=[gk_])
            for dc in range(8):
                pb = g.bank[4 + dc % 4]
                bk = "bank%d" % (4 + dc % 4)
                g.mmk(pb[:, 0:n], [w2c[i][:, ft, dc * 128:(dc + 1) * 128] for ft in range(FC // 128)],
                      [gt[:, ft, 0:n] for ft in range(FC // 128)], r=["w2c%d" % i, gk_], w=[bk])
                res = g.res_tile(dc, t0, n)
                g.stt("dve", res, pb[:, 0:n], M["gate2"][:, dc, v:v + 1], res, ALU.mult, ALU.add,
                      r=[bk, "xT.g%d" % gi] + M["key"], w=["xT.g%d" % gi])
    S.barrier()
    ar.base = 0
    ar.reset()


def _out_layer0(self):
    g = self
    xo = g.outp("xT_out", [128, KT, g.TL])
    co = g.outp("cT_out", [128, KT, LC])
    for k in range(KT):
        g.dma("sp", xo[:, k, :], g.xT[:, k, :], r=["xT.g%d" % i for i in range(len(g.groups))])
    g.dma("sp", co, g.cT[:], r=["xT.g%d" % (len(g.groups) - 1)])


Gen.layer0_main = _layer0_main
Gen.out_layer0 = _out_layer0


def _to_fm(a, n):
    return np.ascontiguousarray(a.reshape(n, KT, 128).transpose(2, 1, 0))


def _from_fm(a):
    n = a.shape[2]
    return np.ascontiguousarray(a.transpose(2, 1, 0).reshape(n, D))


def core_inputs(inputs, TL, core, xT=None, cT=None):
    m = {}
    for k, v in host_consts(core, TL).items():
        m["c_" + k] = v
    x = inputs["x"][0]
    if xT is None:
        m["xT_in"] = _to_fm(x[core * TL:(core + 1) * TL], TL)
        m["cT_in"] = _to_fm(inputs["ctx"][0], LC)
    else:
        m["xT_in"], m["cT_in"] = xT, cT
    cv = np.stack([inputs["c"][0], inputs["c_ctx"]], axis=-1)
    m["cvec"] = np.ascontiguousarray(cv.reshape(KT, 128, 2).transpose(1, 0, 2))
    return m


def run_prog(g, in_maps):
    names = set(g.din.keys())
    maps = [{k: v for k, v in m.items() if k in names} for m in in_maps]
    for m in maps:
        missing = names - set(m.keys())
        assert not missing, missing
    res = run_bass_kernel_spmd(g.nc, maps, core_ids=list(range(NCORES)))
    return res.results


def build_l0(TL, mode, stop=None):
    g = Gen(TL, mode)
    g.setup_common()
    g.layer0_pre()
    if mode == "main0":
        g.layer0_main(stop)
        g.out_layer0()
    g.finish()
    return g


def run_layer0(inputs, TL, stop=None):
    L0 = [k for k in inputs if k.startswith("l0_")]
    base = []
    for c in range(NCORES):
        m = core_inputs(inputs, TL, c)
        for k in L0:
            m[k] = inputs[k]
        base.append(m)
    g1 = build_l0(TL, "pre0")
    r1 = run_prog(g1, base)
    K0all = np.ascontiguousarray(np.stack([r["K0own"] for r in r1], axis=1))
    V0all = np.ascontiguousarray(np.stack([r["V0own"] for r in r1], axis=1))
    Uall = np.ascontiguousarray(np.stack([r["Uown"] for r in r1], axis=0))
    for m in base:
        m["K0all"], m["V0all"], m["Uall"] = K0all, V0all, Uall
    g2 = build_l0(TL, "main0", stop)
    r2 = run_prog(g2, base)
    return [r["xT_out"] for r in r2], [r["cT_out"] for r in r2]


def _layer1_pre(self):
    g = self
    S = g.S
    TL, T, NCH = g.TL, g.T, g.NCH
    ar = g.ar
    W = {}
    for nm, shape in [("l1_ada_w", [D, 6 * D]), ("l1_ada_b", [6 * D]), ("l1_norm1_g", [D]), ("l1_norm2_g", [D]),
                      ("l1_w_in", [D, L1_IN]), ("l1_q_lora_g", [384]), ("l1_kv_lora_g", [256]),
                      ("l1_w_uq", [384, 768]), ("l1_w_ukv", [256, 1024]), ("l1_q_nope_g", [64]),
                      ("l1_q_rope_g", [32]), ("l1_k_nope_g", [64]), ("l1_k_rope_g", [32]),
                      ("l1_w_out", [512, D]), ("l1_router", [D, NE]), ("l1_exp_w1", [NE, D, EDIM]),
                      ("l1_exp_w3", [NE, D, EDIM]), ("l1_exp_w2", [NE, EDIM, D])]:
        if g.mode == "pre1" and nm in ("l1_w_out", "l1_router", "l1_exp_w1", "l1_exp_w3", "l1_exp_w2"):
            continue
        W[nm] = g.inp(nm, shape)
    g.W1 = W
    modT = g.ada(1, W["l1_ada_w"], W["l1_ada_b"], g.scvec)
    n1g = g.gain_cols("n1g1", W["l1_norm1_g"], 8)
    n2g = g.gain_cols("n2g1", W["l1_norm2_g"], 8)
    M = g.mod_derived(1, modT, n1g, n2g)
    g.M1 = M
    qlg = g.gain_cols("qlg", W["l1_q_lora_g"], 3)
    kvlg = g.gain_cols("kvlg", W["l1_kv_lora_g"], 2)
    gq1 = g.gain_cols("gq1", None, 1, pieces=[(0, 64, W["l1_q_nope_g"]), (64, 96, W["l1_q_rope_g"])])
    gk1 = g.gain_cols("gk1", None, 1, pieces=[(0, 64, W["l1_k_nope_g"]), (64, 96, W["l1_k_rope_g"])])

    ar.reset()
    sq = ar.alloc([128, KT, 512], BF16)
    rstd = ar.alloc([128, 512], F32)
    tmpf = [ar.alloc([128, 512], F32) for _ in range(2)]
    src_fn, src_keys = g.res_src()
    g.norm_mod(src_fn, src_keys, g.hT, "hT", M["gs1"], M["sh1"], M["key"], (sq, rstd, tmpf))
    S.barrier()

    ar.reset()
    wi1 = ar.alloc([128, KT, L1_IN], BF16)
    g.dma("pool", wi1, W["l1_w_in"].rearrange("(k p) c -> p k c", p=128), w=["wi1"])
    wuq = ar.alloc([128, 3, 768], BF16)
    g.dma("pool", wuq, W["l1_w_uq"].rearrange("(k p) c -> p k c", p=128), w=["wuq"])
    wukp = ar.alloc([128, 2, 8, 96], BF16)
    wuv = ar.alloc([128, 2, 8, 64], BF16)
    wkrp = ar.alloc([128, KT, 96], BF16)
    g.memset("pool", wukp, 0.0, w=["wukp"])
    g.memset("pool", wkrp, 0.0, w=["wkrp"])
    ukv = W["l1_w_ukv"].rearrange("(ct p) (h c) -> p ct h c", p=128, c=128)
    for ct in range(2):
        g.dma("pool", wukp[:, ct, :, 0:64], ukv[:, ct, :, 0:64], r=["wukp"], w=["wukp_%d" % ct])
        g.dma("pool", wuv[:, ct, :, :], ukv[:, ct, :, 64:128], w=["wuv_%d" % ct])
    g.dma("pool", wkrp[:, :, 64:96], W["l1_w_in"].rearrange("(k p) c -> p k c", p=128)[:, :, 640:672], r=["wkrp"], w=["wkrp_d"])
    wkeys = ["wukp", "wukp_0", "wukp_1", "wkrp", "wkrp_d"]
    cosb = ar.alloc([128, 512], F32)
    sinb = ar.alloc([128, 512], F32)
    hn_tmp = (ar.alloc([128, 512], BF16), ar.alloc([128, 512], F32), ar.alloc([128, 512], F32),
              ar.alloc([128, 512], BF16), ar.alloc([128, 512], F32))
    obuf = [ar.alloc([128, 512], BF16) for _ in range(2)]
    vaug = [ar.alloc([128, 8, 65], BF16) for _ in range(2)]
    for i in range(2):
        g.memset("pool", vaug[i], 1.0, w=["vaug%d" % i])
    cf = ar.alloc([128, 3, 512], F32)
    csq = ar.alloc([128, 3, 512], BF16)
    crs = ar.alloc([128, 512], F32)
    cqn = ar.alloc([128, 3, 512], BF16)
    ckvn = ar.alloc([128, 2, 512], BF16)
    g.Q1 = g.scratch("Q1", [8, 96, TL], BF16)
    mk = g.outp if g.mode == "pre1" else g.scratch
    g.K1own = mk("K1own", [8, 96, TL], BF16)
    g.V1own = mk("V1own", [8, 128, NCH, 65], BF16)
    g.K1ctx = mk("K1ctx", [8, 96, LC], BF16)
    g.V1ctx = mk("V1ctx", [8, 128, LC // 128, 65], BF16)

    def lora_norm(gi, t0, n, col0, ntile, gains, gkey, out, okey, divisor):
        for ct in range(ntile):
            pb = g.bank[ct]
            g.mmk(pb[:, 0:n], [wi1[:, k, col0 + ct * 128:col0 + (ct + 1) * 128] for k in range(KT)],
                  [g.hT[:, k, t0:t0 + n] for k in range(KT)], r=["wi1", "hT.g%d" % gi], w=["bank%d" % ct])
            g.cp("act", cf[:, ct, 0:n], pb[:, 0:n], r=["bank%d" % ct], w=["cf%d" % ct])
            g.tt("pool", csq[:, ct, 0:n], cf[:, ct, 0:n], cf[:, ct, 0:n], ALU.mult, r=["cf%d" % ct], w=["csq%d" % ct])
        pb = g.bank[3]
        g.mmk(pb[:, 0:n], [g.C["ones_bf"][:, :]] * ntile, [csq[:, ct, 0:n] for ct in range(ntile)],
              r=["csq%d" % ct for ct in range(ntile)] + ["C_ones_bf"], w=["bank3"])
        g.act(crs[:, 0:n], pb[:, 0:n], AF.Sqrt, bias=g.epsc[:, 0:1], scale=1.0 / divisor, r=["bank3", "epsc"], w=["crs"])
        g.recip(crs[:, 0:n], crs[:, 0:n], r=["crs"], w=["crs"])
        for ct in range(ntile):
            g.stt("dve", out[:, ct, 0:n], cf[:, ct, 0:n], gains[:, ct:ct + 1], crs[:, 0:n], ALU.mult, ALU.mult,
                  r=["cf%d" % ct, "crs", gkey], w=[okey])

    cnt = 0
    for gi, (t0, n, v) in enumerate(g.groups):
        rope = None
        if v == 0:
            g.dma("sp", cosb[:, 0:n], g.C_dram["cos1"][:, t0:t0 + n], w=["cosb"])
            g.dma("sp", sinb[:, 0:n], g.C_dram["sin1"][:, t0:t0 + n], w=["sinb"])
            rope = (cosb[0:96, 0:n], sinb[0:96, 0:n], g.C["rsw96"], ["cosb", "sinb", "C_rsw96"])
            lora_norm(gi, t0, n, 0, 3, qlg, "qlg", cqn, "cqn", 384.0)
        lora_norm(gi, t0, n, 384, 2, kvlg, "kvlg", ckvn, "ckvn", 256.0)
        for hh in range(16 if v == 0 else 8):
            h = hh % 8
            pb = g.bank[4 + hh % 2]
            bk = "bank%d" % (4 + hh % 2)
            if hh < 8:
                g.mmk(pb[0:96, 0:n], [wukp[:, ct, h, :] for ct in range(2)] + [wkrp[:, k, :] for k in range(KT)],
                      [ckvn[:, ct, 0:n] for ct in range(2)] + [g.hT[:, k, t0:t0 + n] for k in range(KT)],
                      r=wkeys + ["ckvn", "hT.g%d" % gi], w=[bk])
                gain, gkey = gk1, "gk1"
            else:
                g.mmk(pb[0:96, 0:n], [wuq[:, ct, h * 96:(h + 1) * 96] for ct in range(3)],
                      [cqn[:, ct, 0:n] for ct in range(3)], r=["wuq", "cqn"], w=[bk])
                gain, gkey = gq1, "gq1"
            ob = obuf[cnt % 2]
            okey = "obuf%d" % (cnt % 2)
            cnt += 1
            g.headnorm(96, n, pb[0:96, 0:n], bk, g.C["bm96"], "C_bm96", gain[0:96, 0:1], gkey, ob[0:96, 0:n], okey,
                       rope=rope, pbank=6, tmp=hn_tmp)
            if hh < 8:
                dst = g.K1own[h, :, t0:t0 + n] if v == 0 else g.K1ctx[h, :, :]
                g.dma("sp", dst, ob[0:96, 0:n], r=[okey], w=["K1own" if v == 0 else "K1ctx"])
            else:
                g.dma("sp", g.Q1[h, :, t0:t0 + n], ob[0:96, 0:n], r=[okey], w=["Q1"])
        for tt_ in range(n // 128):
            c0 = tt_ * 128
            pb = g.bank[7]
            g.mmk(pb[:, 0:512], [ckvn[:, ct, c0:c0 + 128] for ct in range(2)],
                  [wuv[:, ct, :, :].rearrange("p h c -> p (h c)") for ct in range(2)],
                  r=["wuv_0", "wuv_1", "ckvn"], w=["bank7"])
            va = vaug[tt_ % 2]
            g.cp("act", va[:, :, 0:64], pb[:, 0:512].rearrange("p (a b) -> p a b", b=64), r=["bank7"], w=["vaug%d" % (tt_ % 2)])
            blk = (t0 + c0) // 128 if v == 0 else (t0 + c0 - TL) // 128
            dst = (g.V1own if v == 0 else g.V1ctx)[:, :, blk, :].rearrange("h p c -> p h c")
            g.dma("sp", dst, va[:, :, :], r=["vaug%d" % (tt_ % 2)], w=["V1own" if v == 0 else "V1ctx"])
    S.barrier()


def _layer1_main(self, stop=None):
    g = self
    S = g.S
    TL, T, NCH = g.TL, g.T, g.NCH
    ar = g.ar
    M, W = g.M1, g.W1
    if g.mode == "fused":
        S.barrier()
        rg = [list(range(NCORES))]
        nk, nv = 8 * 96 * TL, 8 * 128 * NCH * 65
        k2 = g.scratch("K1all2d", [NCORES * nk // 512, 512], BF16)
        v2 = g.scratch("V1all2d", [NCORES * nv // 512, 512], BF16)
        for (src, dst, key) in ((g.K1own.rearrange("h d (a b) -> (h d a) b", b=512), k2, "K1all"),
                                (g.V1own.rearrange("h p j c -> (h p j c)").rearrange("(a b) -> a b", b=512), v2, "V1all")):
            S.add("pool", lambda e, s_=src, d_=dst: e.collective_compute(
                "AllGather", ALU.bypass, replica_groups=rg, ins=[s_.opt()], outs=[d_.opt()]),
                ["K1own", "V1own"], [key], dma=True, tag="cc")
        g.K1all = k2.rearrange("(r h d a) b -> h r d (a b)", r=NCORES, h=8, d=96)
        g.V1all = v2.rearrange("a b -> (a b)").rearrange("(r h p j c) -> h r p j c", r=NCORES, h=8, p=128, c=65)
    else:
        g.K1all = g.inp("K1all", [8, NCORES, 96, TL], BF16)
        g.V1all = g.inp("V1all", [8, NCORES, 128, NCH, 65], BF16)
    S.barrier()
    ar.base = 0
    ar.reset()
    g.atT = ar.alloc([128, 4, T], BF16)
    ar.base = ar.off
    CK = min(1024, TL)
    tmp = ([ar.alloc([128, T], BF16) for _ in range(2)], [ar.alloc([128, CK], BF16) for _ in range(3)],
           [ar.alloc([128, CK // 128, 65], BF16) for _ in range(3)], [ar.alloc([128, 512], BF16) for _ in range(3)],
           ar.alloc([128, 512], F32), ar.alloc([128, 512], F32), ar.alloc([128, 512], BF16),
           ar.alloc([128, 512], BF16), ar.alloc([128, 512], BF16))

    def ksegs(h):
        segs = []
        for r_ in range(NCORES):
            for c in range(TL // CK):
                segs.append((g.K1all[h, r_, :, c * CK:(c + 1) * CK],
                             g.V1all[h, r_, :, c * CK // 128:(c + 1) * CK // 128, :], "K1all", "V1all"))
        segs.append((g.K1ctx[h], g.V1ctx[h], "K1ctx", "V1ctx"))
        return segs

    g.attention("a1", 96, 96.0 ** -0.5, 8, lambda h: g.Q1[h, :, 0:TL], ksegs, TL, g.atT, "atT", 0, tmp)
    S.barrier()
    if stop == "attn":
        return
    ar.reset()
    lat_groups = [gr for gr in g.groups if gr[2] == 0]
    wo = ar.alloc([128, 4, D], BF16)
    g.dma("pool", wo, W["l1_w_out"].rearrange("(k p) c -> p k c", p=128), w=["wo1"])
    for gi, (t0, n, v) in enumerate(lat_groups):
        cat = [g.atT[:, kt, t0:t0 + n] for kt in range(4)]
        for dc in range(8):
            pb = g.bank[dc % 4]
            bk = "bank%d" % (dc % 4)
            g.mmk(pb[:, 0:n], [wo[:, kt, dc * 128:(dc + 1) * 128] for kt in range(4)], cat, r=["wo1", "atT"], w=[bk])
            res = g.res_tile(dc, t0, n)
            g.stt("dve", res, pb[:, 0:n], M["gate1"][:, dc, v:v + 1], res, ALU.mult, ALU.add,
                  r=[bk, "xT.g%d" % gi] + M["key"], w=["xT.g%d" % gi])
    S.barrier()
    if stop == "wout":
        return
    ar.base = 0
    ar.reset()
    NG = len(lat_groups)
    gT_sb = ar.alloc([8, TL], F32)
    esel_d = g.inp("c_esel", [8, NE * 128], F32)
    esel = ar.alloc([8, NE * 128], F32)
    g.dma("sp", esel, esel_d, w=["esel"])
    gbc = ar.alloc([128, NG, 512], F32)
    moe_mark = ar.off
    sq = ar.alloc([128, KT, 512], BF16)
    rstd = ar.alloc([128, 512], F32)
    tmpf = [ar.alloc([128, 512], F32) for _ in range(2)]
    h2f = ar.alloc([128, KT, 512], F32)
    rt = ar.alloc([128, KT, NE], F32)
    g.dma("sp", rt, W["l1_router"].rearrange("(k p) e -> p k e", p=128), w=["rt"])
    lg = ar.alloc([128, NCH, NE], F32)
    gates = ar.alloc([128, NCH, NE], F32)
    sm = [ar.alloc([128, 8], F32) for _ in range(4)]
    col = [ar.alloc([128, 1], F32) for _ in range(5)]
    for gi, (t0, n, v) in enumerate(lat_groups):
        pb = g.bank[6]
        for k in range(KT):
            eng = ("act", "pool", "dve")[k % 3]
            src = g.xT[:, k, t0:t0 + n]
            if eng == "act":
                g.act(sq[:, k, 0:n], src, AF.Square, r=["xT.g%d" % gi], w=["nm_sq%d" % k])
            else:
                g.tt(eng, sq[:, k, 0:n], src, src, ALU.mult, r=["xT.g%d" % gi], w=["nm_sq%d" % k])
        g.mmk(pb[:, 0:n], [g.C["ones_bf"][:, :]] * KT, [sq[:, k, 0:n] for k in range(KT)],
              r=["nm_sq%d" % k for k in range(KT)] + ["C_ones_bf"], w=["bank6"])
        g.act(rstd[:, 0:n], pb[:, 0:n], AF.Sqrt, bias=g.epsc[:, 0:1], scale=1.0 / D, r=["bank6", "epsc"], w=["nm_rstd"])
        g.recip(rstd[:, 0:n], rstd[:, 0:n], r=["nm_rstd"], w=["nm_rstd"])
        for k in range(KT):
            tf = tmpf[k % 2]
            g.tt("dve" if k % 2 == 0 else "pool", tf[:, 0:n], g.xT[:, k, t0:t0 + n], rstd[:, 0:n], ALU.mult,
                 r=["xT.g%d" % gi, "nm_rstd"], w=["nm_tmp%d" % (k % 2)])
            g.act(h2f[:, k, 0:n], tf[:, 0:n], AF.Identity, bias=M["sh2"][:, k, 0:1], scale=M["gs2"][:, k, 0:1],
                  r=["nm_tmp%d" % (k % 2)] + M["key"], w=["h2f%d" % k])
            g.cp("pool", g.hT[:, k, t0:t0 + n], h2f[:, k, 0:n], r=["h2f%d" % k], w=["hT.g%d" % gi])
        for tt_ in range(n // 128):
            tile_i = (t0 + tt_ * 128) // 128
            pb2 = g.bank[7]
            g.mmk(pb2[:, 0:NE], [h2f[:, k, tt_ * 128:(tt_ + 1) * 128] for k in range(KT)], [rt[:, k, :] for k in range(KT)],
                  r=["h2f%d" % k for k in range(KT)] + ["rt"], w=["bank7"])
            g.cp("dve", lg[:, tile_i, :], pb2[:, 0:NE], r=["bank7"], w=["lg"])
    for ti in range(NCH):
        lt = lg[:, ti, :]
        m1, m2, nm1, den, rden = [c[:, 0:1] for c in col]
        eq, lg2, sel, ex = [s_[:, :] for s_ in sm]
        g.op("dve", lambda e, o=m1, i=lt: e.tensor_reduce(o, i, AX.X, ALU.max), r=["lg"], w=["c_m1"])
        g.ts("dve", eq, lt, m1, None, ALU.is_equal, r=["lg", "c_m1"], w=["s_eq"])
        g.stt("dve", lg2, eq, -1e30, lt, ALU.mult, ALU.add, r=["s_eq", "lg"], w=["s_lg2"])
        g.op("dve", lambda e, o=m2, i=lg2: e.tensor_reduce(o, i, AX.X, ALU.max), r=["s_lg2"], w=["c_m2"])
        g.ts("dve", sel, lt, m2, None, ALU.is_ge, r=["lg", "c_m2"], w=["s_sel"])
        g.ts("dve", nm1, m1, -1.0, None, ALU.mult, r=["c_m1"], w=["c_nm1"])
        g.act(ex, lt, AF.Exp, bias=nm1, scale=1.0, r=["lg", "c_nm1"], w=["s_ex"])
        g.tt("dve", ex, ex, sel, ALU.mult, r=["s_ex", "s_sel"], w=["s_ex"])
        g.op("dve", lambda e, o=den, i=ex: e.tensor_reduce(o, i, AX.X, ALU.add), r=["s_ex"], w=["c_den"])
        g.recip(rden, den, r=["c_den"], w=["c_rden"])
        g.ts("dve", gates[:, ti, :], ex, rden, None, ALU.mult, r=["s_ex", "c_rden"], w=["gates"])
        pb = g.bank[6]
        g.mm(pb[0:8, 0:128], gates[:, ti, :], g.C["ident_f"][:, :], True, True, r=["gates", "C_ident_f"], w=["bank6"])
        g.cp("act", gT_sb[0:8, ti * 128:(ti + 1) * 128], pb[0:8, 0:128], r=["bank6"], w=["gT_sb"])
    if stop == "gates":
        go = g.outp("gates_out", [8, TL])
        g.dma("sp", go, gT_sb, r=["gT_sb"])
        return
    S.barrier()
    ar.off = moe_mark
    FC = 256
    NFC = EDIM // FC
    w1c = [ar.alloc([128, KT, FC], BF16) for _ in range(2)]
    w3c = [ar.alloc([128, KT, FC], BF16) for _ in range(2)]
    w2c = [ar.alloc([128, FC // 128, D], BF16) for _ in range(2)]
    gTt = [ar.alloc([128, FC // 128, 512], BF16) for _ in range(2)]
    sil = [ar.alloc([128, 512], F32) for _ in range(2)]
    sig = [ar.alloc([128, 512], F32) for _ in range(2)]

    def load_w(idx):
        e, fc = idx // NFC, idx % NFC
        i = idx % 2
        g.dma("pool", w1c[i], W["l1_exp_w1"][e].rearrange("(k p) f -> p k f", p=128)[:, :, fc * FC:(fc + 1) * FC], w=["w1c%d" % i])
        g.dma("pool", w3c[i], W["l1_exp_w3"][e].rearrange("(k p) f -> p k f", p=128)[:, :, fc * FC:(fc + 1) * FC], w=["w3c%d" % i])
        g.dma("pool", w2c[i], W["l1_exp_w2"][e].rearrange("(ft p) d -> p ft d", p=128)[:, fc * (FC // 128):(fc + 1) * (FC // 128), :], w=["w2c%d" % i])
    load_w(0)
    units = [(e, fc, gi) for e in range(NE) for fc in range(NFC) for gi in range(NG)]
    state = {"c2": 0}

    def gate_bc(e):
        for gi, (t0, n, v) in enumerate(lat_groups):
            pb = g.bank[gi % 4]
            g.mm(pb[:, 0:n], esel[0:8, e * 128:(e + 1) * 128], gT_sb[0:8, t0:t0 + n], True, True,
                 r=["esel", "gT_sb"], w=["bank%d" % (gi % 4)])
            g.cp("act", gbc[:, gi, 0:n], pb[:, 0:n], r=["bank%d" % (gi % 4)], w=["gbc"])

    def h_stage(t):
        e, fc, gi = units[t]
        i = (e * NFC + fc) % 2
        t0, n, v = lat_groups[gi]
        hk = "hT.g%d" % gi
        gt = gTt[t % 2]
        gk_ = "gTt%d" % (t % 2)
        hsl = [g.hT[:, k, t0:t0 + n] for k in range(KT)]
        for ft in range(FC // 128):
            j = state["c2"] % 2
            state["c2"] += 1
            h1, h3 = g.bank[2 * j], g.bank[2 * j + 1]
            g.mmk(h1[:, 0:n], [w1c[i][:, k, ft * 128:(ft + 1) * 128] for k in range(KT)], hsl,
                  r=["w1c%d" % i, hk], w=["bank%d" % (2 * j)])
            g.mmk(h3[:, 0:n], [w3c[i][:, k, ft * 128:(ft + 1) * 128] for k in range(KT)], hsl,
                  r=["w3c%d" % i, hk], w=["bank%d" % (2 * j + 1)])
            g.act(sil[j][:, 0:n], h1[:, 0:n], AF.Silu, r=["bank%d" % (2 * j)], w=["sil%d" % j])
            g.tt("pool", sig[j][:, 0:n], sil[j][:, 0:n], gbc[:, gi, 0:n], ALU.mult, r=["sil%d" % j, "gbc"], w=["sig%d" % j])
            g.tt("dve", gt[:, ft, 0:n], sig[j][:, 0:n], h3[:, 0:n], ALU.mult,
                 r=["sig%d" % j, "bank%d" % (2 * j + 1)], w=[gk_])

    def y_stage(t):
        e, fc, gi = units[t]
        i = (e * NFC + fc) % 2
        t0, n, v = lat_groups[gi]
        gt = gTt[t % 2]
        gk_ = "gTt%d" % (t % 2)
        for dc in range(8):
            pb = g.bank[4 + dc % 4]
            bk = "bank%d" % (4 + dc % 4)
            g.mmk(pb[:, 0:n], [w2c[i][:, ft, dc * 128:(dc + 1) * 128] for ft in range(FC // 128)],
                  [gt[:, ft, 0:n] for ft in range(FC // 128)], r=["w2c%d" % i, gk_], w=[bk])
            res = g.xT[:, dc, t0:t0 + n]
            g.stt("dve", res, pb[:, 0:n], M["gate2"][:, dc, 0:1], res, ALU.mult, ALU.add,
                  r=[bk, "xT.g%d" % gi] + M["key"], w=["xT.g%d" % gi])

    for t in range(len(units)):
        e, fc, gi = units[t]
        if fc == 0 and gi == 0:
            gate_bc(e)
        h_stage(t)
        if t > 0:
            y_stage(t - 1)
        if gi == 0:
            idx = e * NFC + fc
            if idx + 1 < NE * NFC:
                load_w(idx + 1)
    y_stage(len(units) - 1)
    S.barrier()


def _out_final(self):
    g = self
    xo = g.outp("xT_out", [128, KT, g.TL])
    for k in range(KT):
        g.dma("sp", xo[:, k, :], g.xT[:, k, :], r=["xT.g%d" % i for i in range(len(g.groups))])


Gen.layer1_pre = _layer1_pre
Gen.layer1_main = _layer1_main
Gen.out_final = _out_final


def build_l1(TL, mode, stop=None):
    g = Gen(TL, mode)
    g.setup_common()
    g.layer1_pre()
    if mode == "main1":
        g.layer1_main(stop)
        g.out_final()
    g.finish()
    return g


def _esel():
    e = np.zeros((8, NE * 128), np.float32)
    for i in range(NE):
        e[i, i * 128:(i + 1) * 128] = 1.0
    return e


def run_layer1(inputs, TL, xTs, cTs, stop=None):
    L1 = [k for k in inputs if k.startswith("l1_")]
    base = []
    for c in range(NCORES):
        m = core_inputs(inputs, TL, c, xTs[c], cTs[c])
        for k in L1:
            m[k] = inputs[k]
        m["c_esel"] = _esel()
        base.append(m)
    g1 = build_l1(TL, "pre1")
    r1 = run_prog(g1, base)
    K1all = np.ascontiguousarray(np.stack([r["K1own"] for r in r1], axis=1))
    V1all = np.ascontiguousarray(np.stack([r["V1own"] for r in r1], axis=1))
    for m in base:
        m["K1all"], m["V1all"] = K1all, V1all
    g2 = build_l1(TL, "main1", stop)
    r2 = run_prog(g2, base)
    return r2


def build_fused(TL, upto=None, stop=None):
    g = Gen(TL, "fused")
    g.setup_common()
    g.layer0_pre()
    g.layer0_main(stop)
    if upto == "l0":
        g.out_layer0()
        g.finish()
        return g
    g.layer1_pre()
    g.layer1_main()
    g.out_final()
    g.finish()
    return g


def run_fused(inputs, TL):
    base = []
    esel = _esel()
    for c in range(NCORES):
        m = core_inputs(inputs, TL, c)
        for k in inputs:
            if k.startswith("l0_") or k.startswith("l1_"):
                m[k] = inputs[k]
        m["c_esel"] = esel
        base.append(m)
    g = build_fused(TL)
    return run_prog(g, base)


def kernel_unfused(**inputs):
    inputs = {k: np.asarray(v) for k, v in inputs.items()}
    SEQ = inputs["x"].shape[1]
    TL = SEQ // NCORES
    xTs, cTs = run_layer0(inputs, TL)
    r2 = run_layer1(inputs, TL, xTs, cTs)
    out = np.concatenate([_from_fm(r["xT_out"]) for r in r2], axis=0)
    return out[None].astype(np.float32)


FUSED = False


def kernel(**inputs):
    if not FUSED:
        return kernel_unfused(**inputs)
    inputs = {k: np.asarray(v) for k, v in inputs.items()}
    SEQ = inputs["x"].shape[1]
    TL = SEQ // NCORES
    r2 = run_fused(inputs, TL)
    out = np.concatenate([_from_fm(r["xT_out"]) for r in r2], axis=0)
    return out[None].astype(np.float32)
```

```python
import numpy as np
import ml_dtypes
import concourse.bass as bass
import concourse.mybir as mybir
from concourse.bass_utils import run_bass_kernel_spmd

F32 = mybir.dt.float32
BF16 = mybir.dt.bfloat16
AF = mybir.ActivationFunctionType
ALU = mybir.AluOpType
AX = mybir.AxisListType

NCORES = 8
D = 1024
KT = 8
GRID_W = 64
LC = 256
EPS = 1e-6
ROPE_THETA = 10000.0
FFN = 2816
NE = 8
EDIM = 3584
L0_IN = 2816
L1_IN = 672


class Op:
    __slots__ = ("eng", "fn", "deps", "dma", "sig", "ms", "dsem", "dval", "ringprev", "tag")


class Sched:
    ENGS = ("pe", "act", "dve", "pool", "sp")
    RING = 8

    def __init__(self):
        self.ops = []
        self.lastw = {}
        self.rds = {}
        self.pend_barrier = {e: [] for e in self.ENGS}
        self.last_op = {e: None for e in self.ENGS}
        self.all_dma = []

    def add(self, eng, fn, reads=(), writes=(), dma=False, tag=None):
        op = Op()
        op.eng, op.fn, op.dma, op.sig, op.ms = eng, fn, dma, False, 0
        op.dsem = op.dval = op.ringprev = None
        op.tag = tag
        deps = set()
        for k in reads:
            w = self.lastw.get(k)
            if w is not None:
                deps.add(w)
            if k.startswith("bank"):
                for r in self.rds.get(k, ()):
                    if r.eng != eng:
                        deps.add(r)
        for k in writes:
            w = self.lastw.get(k)
            if w is not None:
                deps.add(w)
            for r in self.rds.get(k, ()):
                deps.add(r)
        for d in self.pend_barrier[eng]:
            deps.add(d)
        self.pend_barrier[eng] = []
        op.deps = deps
        for k in reads:
            lst = self.rds.setdefault(k, [])
            if not dma:
                lst[:] = [r for r in lst if r.dma or r.eng != eng]
            lst.append(op)
        for k in writes:
            self.lastw[k] = op
            self.rds[k] = []
        self.ops.append(op)
        self.last_op[eng] = op
        if dma:
            self.all_dma.append(op)
        return op

    def barrier(self):
        lasts = [o for o in self.last_op.values() if o is not None]
        dm = list(self.all_dma)
        self.all_dma = []
        for e in self.ENGS:
            self.pend_barrier[e] = self.pend_barrier[e] + lasts + dm

    def finalize(self, nc):
        for op in self.ops:
            for d in op.deps:
                if d.dma:
                    continue
                if d.eng == "pe" and op.eng == "pe" and not op.dma:
                    continue
                d.sig = True
        self.sem = {e: nc.alloc_semaphore("sem_" + e) for e in self.ENGS}
        self.ring = {e: [nc.alloc_semaphore("dma_%s_%d" % (e, i)) for i in range(self.RING)]
                     for e in ("sp", "pool", "act")}
        cnt = {e: 0 for e in self.ENGS}
        dcnt = {e: 0 for e in self.ENGS}
        ringlast = {e: [None] * self.RING for e in self.ENGS}
        for op in self.ops:
            if op.dma and op.tag == "cc":
                op.dsem = nc.alloc_semaphore("cc_%d" % len(self.ops) + "_%d" % id(op))
                op.dval = 1
            elif op.dma:
                i = dcnt[op.eng]
                dcnt[op.eng] += 1
                slot = i % self.RING
                op.dsem = self.ring[op.eng][slot]
                op.dval = 16 * (i // self.RING + 1)
                op.ringprev = ringlast[op.eng][slot]
                ringlast[op.eng][slot] = op
            elif op.sig:
                cnt[op.eng] += 1
                op.ms = cnt[op.eng]
        self.by_eng = {e: [o for o in self.ops if o.eng == e] for e in self.ENGS}
        self.counts = cnt

    def emit(self, ename, eng):
        waited = {}
        for op in self.by_eng[ename]:
            waits = {}
            for d in op.deps:
                if d.dma:
                    key, sem, val = ("d", d.eng, id(d.dsem)), d.dsem, d.dval
                else:
                    if d.eng == "pe" and ename == "pe" and not op.dma:
                        continue
                    key, sem, val = ("c", d.eng, 0), self.sem[d.eng], d.ms
                if key not in waits or waits[key][1] < val:
                    waits[key] = (sem, val)
            if op.dma and op.ringprev is not None:
                d = op.ringprev
                key = ("d", d.eng, id(d.dsem))
                if key not in waits or waits[key][1] < d.dval:
                    waits[key] = (d.dsem, d.dval)
            for key, (sem, val) in waits.items():
                if waited.get(key, 0) < val:
                    eng.wait_ge(sem, val)
                    waited[key] = val
            ins = op.fn(eng)
            if ins is None:
                continue
            if op.dma and op.tag == "cc":
                ins.then_inc(op.dsem, 1)
            elif op.dma:
                ins.then_inc(op.dsem, 16)
            elif op.sig:
                ins.then_inc(self.sem[ename], 1)


def _rope_tables(pos_rows, pos_cols, rot_dim):
    axis_dim = rot_dim // 2
    inv_freq = (ROPE_THETA ** (-np.arange(0, axis_dim, 2, dtype=np.float32) / axis_dim)).astype(np.float32)
    ang = np.concatenate([pos_rows[:, None].astype(np.float32) * inv_freq,
                          pos_cols[:, None].astype(np.float32) * inv_freq], axis=-1)
    return np.cos(ang).astype(np.float32), np.sin(ang).astype(np.float32)


def host_consts(core, TL):
    bf = ml_dtypes.bfloat16
    c = {}
    c["ident_bf"] = np.eye(128, dtype=np.float32).astype(bf)
    c["ident_f"] = np.eye(128, dtype=np.float32)
    c["ones_bf"] = np.ones((128, 128), np.float32).astype(bf)
    c["ones_f"] = np.ones((128, 128), np.float32)
    bo = np.zeros((128, 128), np.float32)
    bo[:64, :64] = 1.0 / 64
    bo[64:, 64:] = 1.0 / 64
    c["bo64"] = bo.astype(bf)
    bm = np.zeros((128, 128), np.float32)
    bm[:64, :64] = 1.0 / 64
    bm[64:96, 64:96] = 1.0 / 32
    c["bm96"] = bm.astype(bf)
    bd = np.zeros((128, 128), np.float32)
    bd[:64, :64] = 1.0
    bd[64:, 64:] = 1.0
    c["bdmask"] = bd
    r = np.zeros((128, 128), np.float32)
    for i in range(64):
        r[2 * i + 1, 2 * i] = -1.0
        r[2 * i, 2 * i + 1] = 1.0
    c["rsw"] = r.astype(bf)
    r96 = r.copy()
    r96[:64, :] = 0.0
    r96[:, :64] = 0.0
    r96[96:, :] = 0.0
    r96[:, 96:] = 0.0
    c["rsw96"] = r96.astype(bf)
    t = core * TL + np.arange(TL)
    rows, cols = t // GRID_W, t % GRID_W
    cs, sn = _rope_tables(rows, cols, 64)
    c["cos0"] = np.ascontiguousarray(np.repeat(cs, 2, axis=1).T)
    c["sin0"] = np.ascontiguousarray(np.repeat(sn, 2, axis=1).T)
    c["cos0"] = np.concatenate([c["cos0"], c["cos0"]], axis=0)
    c["sin0"] = np.concatenate([c["sin0"], c["sin0"]], axis=0)
    cs, sn = _rope_tables(rows, cols, 32)
    c1 = np.ones((128, TL), np.float32)
    s1 = np.zeros((128, TL), np.float32)
    c1[64:96] = np.repeat(cs, 2, axis=1).T
    s1[64:96] = np.repeat(sn, 2, axis=1).T
    c["cos1"], c["sin1"] = c1, s1
    m = np.arange(128, dtype=np.float32)
    cc = np.arange(128, dtype=np.float32)
    relf = np.maximum(cc[None, :] - m[:, None], 0.0)
    relb = np.maximum(m[:, None] - cc[None, :], 0.0)
    c["relf"] = relf.astype(np.float32)
    c["relb"] = relb.astype(np.float32)
    c["indf"] = (cc[None, :] >= m[:, None]).astype(np.float32)
    c["indb"] = (m[:, None] >= cc[None, :]).astype(np.float32)
    c["posf"] = np.repeat((127.0 - m)[:, None], 64, axis=1).astype(np.float32)
    c["posb"] = np.repeat(m[:, None], 64, axis=1).astype(np.float32)
    c["rampf"] = np.repeat((cc + 1.0)[None, :], 128, axis=0).astype(np.float32)
    c["rampb"] = np.repeat((128.0 - cc)[None, :], 128, axis=0).astype(np.float32)
    nch = TL // 128
    cp = np.zeros((128, 40), np.float32)
    for r_ in range(NCORES):
        if r_ < core:
            cp[:, r_] = 128.0 * nch * (core - 1 - r_)
            cp[:, 8 + r_] = 1.0
        if r_ > core:
            cp[:, 16 + r_] = 128.0 * nch * (r_ - core - 1)
            cp[:, 24 + r_] = 1.0
    cp[:, 32] = 128.0 * nch * core
    cp[:, 33] = 128.0 * nch * (NCORES - 1 - core)
    c["cpos"] = cp
    return c


class Gen:
    def __init__(self, TL, mode):
        self.TL = TL
        self.T = TL + LC
        self.NCH = TL // 128
        self.S_ALL = NCORES * TL + LC
        self.NKB = self.S_ALL // 128
        self.mode = mode
        self.nc = bass.Bass("TRN2", target_bir_lowering=False)
        self.S = Sched()
        self.din = {}
        self.dout = {}
        self.groups = [(g * 512, 512, 0) for g in range(TL // 512)] + [(TL, LC, 1)]
        self.uid = 0

    def inp(self, name, shape, dt=F32):
        t = self.nc.dram_tensor(name, list(shape), dt, kind="ExternalInput")
        self.din[name] = t
        return t.ap()

    def outp(self, name, shape, dt=F32):
        t = self.nc.dram_tensor(name, list(shape), dt, kind="ExternalOutput")
        self.dout[name] = t
        return t.ap()

    def scratch(self, name, shape, dt):
        return self.nc.dram_tensor(name, list(shape), dt).ap()

    def sb(self, name, shape, dt):
        return self.nc.alloc_sbuf_tensor(name, list(shape), dt)

    def op(self, eng, fn, r=(), w=(), dma=False):
        return self.S.add(eng, fn, r, w, dma)

    def dma(self, q, out, in_, r=(), w=()):
        return self.S.add(q, lambda e, o=out, i=in_: e.dma_start(out=o, in_=i), r, w, dma=True)

    def mm(self, out, lhsT, rhs, start, stop, r=(), w=()):
        return self.S.add("pe", lambda e, o=out, l=lhsT, rr=rhs, s=start, t=stop:
                          e.matmul(o, l, rr, start=s, stop=t), r, w)

    def mmk(self, out, lhs_list, rhs_list, r=(), w=()):
        n = len(lhs_list)

        def fn(e, o=out, ll=lhs_list, rl=rhs_list):
            ins = None
            for k in range(n):
                ins = e.matmul(o, ll[k], rl[k], start=(k == 0), stop=(k == n - 1))
            return ins
        return self.S.add("pe", fn, r, w)

    def act(self, out, in_, func, bias=0.0, scale=1.0, r=(), w=()):
        return self.S.add("act", lambda e, o=out, i=in_, f=func, b=bias, s=scale:
                          e.activation(out=o, in_=i, func=f, bias=b, scale=s), r, w)

    def tt(self, eng, out, in0, in1, op, r=(), w=()):
        return self.S.add(eng, lambda e, o=out, a=in0, b=in1, p=op: e.tensor_tensor(o, a, b, p), r, w)

    def ts(self, eng, out, in0, s1, s2, op0, op1=None, r=(), w=()):
        if op1 is None:
            return self.S.add(eng, lambda e, o=out, a=in0, x=s1, p=op0:
                              e.tensor_scalar(o, a, x, None, p), r, w)
        return self.S.add(eng, lambda e, o=out, a=in0, x=s1, y=s2, p=op0, q=op1:
                          e.tensor_scalar(o, a, x, y, p, q), r, w)

    def stt(self, eng, out, in0, scalar, in1, op0, op1, r=(), w=()):
        return self.S.add(eng, lambda e, o=out, a=in0, s=scalar, b=in1, p=op0, q=op1:
                          e.scalar_tensor_tensor(o, a, s, b, p, q), r, w)

    def cp(self, eng, out, in_, r=(), w=()):
        if eng == "act":
            return self.S.add("act", lambda e, o=out, i=in_: e.copy(o, i), r, w)
        return self.S.add(eng, lambda e, o=out, i=in_: e.tensor_copy(o, i), r, w)

    def recip(self, out, in_, r=(), w=()):
        return self.S.add("dve", lambda e, o=out, i=in_: e.reciprocal(o, i), r, w)

    def memset(self, eng, ap, val, w=()):
        return self.S.add(eng, lambda e, a=ap, v=val: e.memset(a, v), (), w)

    def load_consts(self):
        TL = self.TL
        spec = [("ident_bf", [128, 128], BF16), ("ident_f", [128, 128], F32),
                ("ones_bf", [128, 128], BF16), ("ones_f", [128, 128], F32),
                ("bo64", [128, 128], BF16), ("bm96", [128, 128], BF16),
                ("bdmask", [128, 128], F32), ("rsw", [128, 128], BF16), ("rsw96", [128, 128], BF16),
                ("cpos", [128, 40], F32)]
        self.C = {}
        for name, shape, dt in spec:
            d = self.inp("c_" + name, shape, dt)
            s = self.sb("C_" + name, shape, dt)
            self.dma("sp", s[:], d, w=["C_" + name])
            self.C[name] = s
        self.C_dram = {}
        for name, shape in [("cos0", [128, TL]), ("sin0", [128, TL]), ("cos1", [128, TL]), ("sin1", [128, TL]),
                            ("relf", [128, 128]), ("relb", [128, 128]), ("indf", [128, 128]),
                            ("indb", [128, 128]), ("posf", [128, 64]), ("posb", [128, 64]),
                            ("rampf", [128, 128]), ("rampb", [128, 128])]:
            self.C_dram[name] = self.inp("c_" + name, shape, F32)
        self.bank = [self.nc.alloc_psum_tensor("bank%d" % i, [128, 512], F32) for i in range(8)]

    def ada(self, L, ada_w, ada_b, cvec_sb):
        modT = self.sb("modT%d" % L, [128, 48, 2], F32)
        abT = self.gain_cols("abT%d" % L, ada_b, 48)
        NCHK = 12
        CW = 6 * D // NCHK
        self.ar.reset()
        wbuf = [self.ar.alloc([128, KT, CW], F32) for i in range(2)]
        aw = ada_w.rearrange("(k p) c -> p k c", p=128)
        pb = self.bank[7]
        for ch in range(NCHK):
            wb = wbuf[ch % 2]
            self.dma("sp", wb, aw[:, :, ch * CW:(ch + 1) * CW], w=["adaw%d" % (ch % 2)])
            for jj in range(CW // 128):
                j = ch * (CW // 128) + jj
                self.mmk(pb[:, 2 * j:2 * j + 2],
                         [wb[:, k, jj * 128:(jj + 1) * 128] for k in range(KT)],
                         [cvec_sb[:, k, :] for k in range(KT)],
                         r=["adaw%d" % (ch % 2), "scvec"], w=["bank7"])
        pv = pb[:, 0:96].rearrange("p (j v) -> p j v", v=2)
        for v in range(2):
            self.tt("dve", modT[:, :, v], pv[:, :, v], abT[:], ALU.add, r=["bank7", "abT%d" % L], w=["modT%d" % L])
        self.S.barrier()
        return modT

    def gain_cols(self, name, g_ap, n, pieces=None):
        t = self.sb(name, [128, n], F32)
        rows = self.sb(name + "_rows", [n, 128], F32)
        if pieces is None:
            self.dma("sp", rows[:], g_ap.rearrange("(k p) -> k p", p=128), w=[name + "_rows"])
        else:
            self.memset("pool", rows[:], 0.0, w=[name + "_rows"])
            for (c0, c1, src) in pieces:
                self.dma("sp", rows[0:1, c0:c1], src.rearrange("(o p) -> o p", o=1), r=[name + "_rows"], w=[name + "_rows_%d" % c0])
        pb = self.bank[7]
        rk = [name + "_rows"] + ([name + "_rows_%d" % c0 for (c0, c1, src) in pieces] if pieces else [])
        self.mm(pb[:, 0:n], rows[0:n, :], self.C["ident_f"][0:n, 0:n], True, True, r=rk + ["C_ident_f"], w=["bank7"])
        self.cp("dve", t[:], pb[:, 0:n], r=["bank7"], w=[name])
        return t

    def mod_derived(self, L, modT, n1g, n2g):
        o = {}
        for nm, piece_scale, g in (("gs1", 1, n1g), ("gs2", 4, n2g)):
            t = self.sb("%s_%d" % (nm, L), [128, 8, 2], F32)
            for v in range(2):
                self.stt("dve", t[:, :, v], modT[:, piece_scale * 8:(piece_scale + 1) * 8, v], 1.0, g[:],
                         ALU.add, ALU.mult, r=["modT%d" % L, g.name if hasattr(g, "name") else "g"],
                         w=["%s_%d" % (nm, L)])
            o[nm] = t
        o["sh1"] = modT[:, 0:8, :]
        o["gate1"] = modT[:, 16:24, :]
        o["sh2"] = modT[:, 24:32, :]
        o["gate2"] = modT[:, 40:48, :]
        o["key"] = ["modT%d" % L, "gs1_%d" % L, "gs2_%d" % L]
        return o

    def norm_mod(self, src_fn, src_keys, dst, dst_key, gs, sh, modkeys, tmp):
        sq, rstd, tmpf = tmp
        for gi, (t0, n, v) in enumerate(self.groups):
            pb = self.bank[6]
            for k in range(KT):
                eng = ("act", "pool", "dve")[k % 3]
                src = src_fn(k, t0, n)
                if eng == "act":
                    self.act(sq[:, k, 0:n], src, AF.Square, r=src_keys(gi), w=["nm_sq%d" % k])
                else:
                    self.tt(eng, sq[:, k, 0:n], src, src, ALU.mult, r=src_keys(gi), w=["nm_sq%d" % k])
            self.mmk(pb[:, 0:n], [self.C["ones_bf"][:, :] for k in range(KT)],
                     [sq[:, k, 0:n] for k in range(KT)],
                     r=["nm_sq%d" % k for k in range(KT)] + ["C_ones_bf"], w=["bank6"])
            self.act(rstd[:, 0:n], pb[:, 0:n], AF.Sqrt, bias=self.epsc[:, 0:1], scale=1.0 / D, r=["bank6", "epsc"], w=["nm_rstd"])
            self.recip(rstd[:, 0:n], rstd[:, 0:n], r=["nm_rstd"], w=["nm_rstd"])
            for k in range(KT):
                src = src_fn(k, t0, n)
                tf = tmpf[k % 2]
                self.tt("dve" if k % 2 == 0 else "pool", tf[:, 0:n], src, rstd[:, 0:n], ALU.mult,
                        r=src_keys(gi) + ["nm_rstd"], w=["nm_tmp%d" % (k % 2)])
                self.act(dst[:, k, t0:t0 + n], tf[:, 0:n], AF.Identity, bias=sh[:, k, v:v + 1], scale=gs[:, k, v:v + 1],
                         r=["nm_tmp%d" % (k % 2)] + modkeys, w=["%s.g%d" % (dst_key, gi)])

    def headnorm(self, P, n, src_psum, src_key, blk, blk_key, gain_col, gain_key, out_bf, out_key,
                 rope=None, pbank=5, tmp=None):
        sqb, rs, qn, qnb, t1 = tmp
        pb = self.bank[pbank]
        bk = "bank%d" % pbank
        self.act(sqb[0:P, 0:n], src_psum, AF.Square, r=[src_key], w=["hn_sq"])
        self.mm(pb[0:P, 0:n], blk[0:P, 0:P], sqb[0:P, 0:n], True, True, r=["hn_sq", blk_key], w=[bk])
        self.act(rs[0:P, 0:n], pb[0:P, 0:n], AF.Sqrt, bias=self.epsc[0:P, 0:1], scale=1.0, r=[bk, "epsc"], w=["hn_rs"])
        self.recip(rs[0:P, 0:n], rs[0:P, 0:n], r=["hn_rs"], w=["hn_rs"])
        if rope is None:
            self.stt("dve", out_bf, src_psum, gain_col, rs[0:P, 0:n], ALU.mult, ALU.mult,
                     r=[src_key, gain_key, "hn_rs"], w=[out_key])
            return
        cos_ap, sin_ap, rsw, rkeys = rope
        self.stt("dve", qn[0:P, 0:n], src_psum, gain_col, rs[0:P, 0:n], ALU.mult, ALU.mult,
                 r=[src_key, gain_key, "hn_rs"], w=["hn_qn"])
        self.cp("act", qnb[0:P, 0:n], qn[0:P, 0:n], r=["hn_qn"], w=["hn_qnb"])
        self.mm(pb[0:P, 0:n], rsw[0:P, 0:P], qnb[0:P, 0:n], True, True, r=["hn_qnb"] + rkeys, w=[bk])
        self.tt("pool", t1[0:P, 0:n], qn[0:P, 0:n], cos_ap, ALU.mult, r=["hn_qn"] + rkeys, w=["hn_t1"])
        self.tt("dve", qn[0:P, 0:n], pb[0:P, 0:n], sin_ap, ALU.mult, r=[bk] + rkeys, w=["hn_qn"])
        self.tt("dve", out_bf, qn[0:P, 0:n], t1[0:P, 0:n], ALU.add, r=["hn_qn", "hn_t1"], w=[out_key])

    def attention(self, tagp, dq, scale, nheads, qsrc, ksegs_of_head, nq, out_tile, out_key, out_off, tmp):
        qbuf, kbuf, vbuf, pbuf, osb, rl, rlh, rll, atmp = tmp
        NQG = (nq + 511) // 512
        LA = 2
        its = []
        for h in range(nheads):
            segs = ksegs_of_head(h)
            nseg = len(segs)
            for si, (K_ap, V_ap, kkey, vkey) in enumerate(segs):
                nkb = K_ap.shape[1] // 128
                for qg in range(NQG):
                    for kb in range(nkb):
                        its.append((h, si, qg, kb, si == 0 and kb == 0, si == nseg - 1 and kb == nkb - 1))
        segctr = {}
        chunk_id = {}
        ctr = 0
        for (h, si, qg, kb, first, last) in its:
            if (h, si) not in chunk_id:
                chunk_id[(h, si)] = ctr
                ctr += 1
        loaded = set()
        qloaded = set()

        def load_q(h):
            if h in qloaded or h >= nheads:
                return
            qloaded.add(h)
            self.dma("sp", qbuf[h % 2][0:dq, 0:nq], qsrc(h), r=[tagp + "qsrc"], w=["%s_q%d" % (tagp, h % 2)])

        def load_chunk(h, si):
            if (h, si) in loaded:
                return
            loaded.add((h, si))
            segs = ksegs_of_head(h)
            K_ap, V_ap, kkey, vkey = segs[si]
            c = chunk_id[(h, si)]
            nk = K_ap.shape[1]
            self.dma("sp", kbuf[c % 3][0:dq, 0:nk], K_ap, r=[kkey], w=["%s_k%d" % (tagp, c % 3)])
            self.dma("sp", vbuf[c % 3][:, 0:nk // 128, :], V_ap, r=[vkey], w=["%s_v%d" % (tagp, c % 3)])

        order = sorted(chunk_id.items(), key=lambda kv: kv[1])
        nxt = {}
        for i in range(len(order) - 1):
            nxt[order[i][0]] = order[i + 1][0]
        n = len(its)
        for i in range(n + LA):
            if i < n:
                h, si, qg, kb, first, last = its[i]
                load_q(h)
                load_chunk(h, si)
                if (h, si) in nxt and qg == 0 and kb == 0:
                    nh, nsi = nxt[(h, si)]
                    load_q(nh)
                    load_chunk(nh, nsi)
                c = chunk_id[(h, si)]
                nqq = min(512, nq - qg * 512)
                sb_ = self.bank[i % 4]
                self.mm(sb_[:, 0:nqq], kbuf[c % 3][0:dq, kb * 128:(kb + 1) * 128],
                        qbuf[h % 2][0:dq, qg * 512:qg * 512 + nqq], True, True,
                        r=["%s_k%d" % (tagp, c % 3), "%s_q%d" % (tagp, h % 2)], w=["bank%d" % (i % 4)])
            j = i - LA
            if j >= 0:
                h, si, qg, kb, first, last = its[j]
                c = chunk_id[(h, si)]
                nqq = min(512, nq - qg * 512)
                self.act(pbuf[j % 3][:, 0:nqq], self.bank[j % 4][:, 0:nqq], AF.Exp, scale=scale,
                         r=["bank%d" % (j % 4)], w=["%s_p%d" % (tagp, j % 3)])
                ob = self.bank[4 + qg]
                self.mm(ob[0:65, 0:nqq], vbuf[c % 3][:, kb, :], pbuf[j % 3][:, 0:nqq], first, last,
                        r=["%s_v%d" % (tagp, c % 3), "%s_p%d" % (tagp, j % 3)], w=["bank%d" % (4 + qg)])
                if last:
                    bk = "bank%d" % (4 + qg)
                    self.cp("act", osb[0:65, 0:nqq], ob[0:65, 0:nqq], r=[bk], w=[tagp + "_osb"])
                    self.recip(rl[64:65, 0:nqq], osb[64:65, 0:nqq], r=[tagp + "_osb"], w=[tagp + "_rl"])
                    self.cp("dve", rlh[64:65, 0:nqq], rl[64:65, 0:nqq], r=[tagp + "_rl"], w=[tagp + "_rlh"])
                    self.tt("dve", rl[64:65, 0:nqq], rl[64:65, 0:nqq], rlh[64:65, 0:nqq], ALU.subtract,
                            r=[tagp + "_rl", tagp + "_rlh"], w=[tagp + "_rl"])
                    self.cp("dve", rll[64:65, 0:nqq], rl[64:65, 0:nqq], r=[tagp + "_rl"], w=[tagp + "_rll"])
                    self.mmk(ob[0:64, 0:nqq], [self.C["ones_bf"][64:65, 0:64], self.C["ones_bf"][64:65, 0:64]],
                             [rlh[64:65, 0:nqq], rll[64:65, 0:nqq]],
                             r=[tagp + "_rlh", tagp + "_rll", tagp + "_osb", "C_ones_bf"], w=[bk])
                    q0 = out_off + qg * 512
                    if h % 2 == 0:
                        self.tt("dve", out_tile[0:64, h // 2, q0:q0 + nqq], osb[0:64, 0:nqq], ob[0:64, 0:nqq], ALU.mult,
                                r=[bk, tagp + "_osb"], w=[out_key])
                    else:
                        self.tt("dve", atmp[0:64, 0:nqq], osb[0:64, 0:nqq], ob[0:64, 0:nqq], ALU.mult,
                                r=[bk, tagp + "_osb"], w=[tagp + "_atmp"])
                        self.cp("dve", out_tile[64:128, h // 2, q0:q0 + nqq], atmp[0:64, 0:nqq],
                                r=[tagp + "_atmp"], w=[out_key])


class Arena:
    def __init__(self, gen, nbytes):
        self.t = gen.sb("arena", [128, nbytes // 4], F32)
        self.n = nbytes
        self.off = 0
        self.peak = 0
        self.base = 0

    def reset(self):
        self.off = self.base

    def alloc(self, shape, dt):
        free = 1
        for s in shape[1:]:
            free *= s
        nb = free * (2 if dt == BF16 else 4)
        nb_al = (nb + 63) // 64 * 64
        assert self.off + nb_al <= self.n, ("arena overflow", self.off, nb_al, self.n)
        a = self.off // 4
        ap = self.t[:, a:a + nb_al // 4]
        self.off += nb_al
        self.peak = max(self.peak, self.off)
        if dt == BF16:
            ap = ap.bitcast(BF16)
        ap = ap[0:shape[0], 0:free]
        if len(shape) == 3:
            ap = ap.rearrange("p (a b) -> p a b", b=shape[2])
        elif len(shape) == 4:
            ap = ap.rearrange("p (a b c) -> p a b c", b=shape[2], c=shape[3])
        return ap


def _gen_finish(self):
    S = self.S
    outs = [o for o in S.ops if o.dma]
    S.pend_barrier["sp"] = S.pend_barrier["sp"] + outs + [o for o in S.last_op.values() if o is not None]
    S.add("sp", lambda e: None)
    S.finalize(self.nc)
    nc = self.nc
    with nc.Block() as block:
        @block.tensor
        def _(e):
            S.emit("pe", e)

        @block.scalar
        def _(e):
            S.emit("act", e)

        @block.vector
        def _(e):
            S.emit("dve", e)

        @block.gpsimd
        def _(e):
            S.emit("pool", e)

        @block.sync
        def _(e):
            S.emit("sp", e)


Gen.finish = _gen_finish


def _setup_common(self):
    g = self
    TL, T = g.TL, g.T
    g.load_consts()
    g.epsc = g.sb("epsc", [128, 1], F32)
    g.memset("pool", g.epsc[:], EPS, w=["epsc"])
    g.xT = g.sb("xT", [128, KT, TL], F32)
    g.cT = g.sb("cT", [128, KT, LC], F32)
    g.hT = g.sb("hT", [128, KT, T], BF16)
    g.cvec = g.sb("cvec_sb", [128, KT, 2], F32)
    g.scvec = g.sb("scvec_sb", [128, KT, 2], F32)
    xin = g.inp("xT_in", [128, KT, TL])
    cin = g.inp("cT_in", [128, KT, LC])
    cv = g.inp("cvec", [128, KT, 2])
    for k in range(KT):
        g.dma("sp", g.xT[:, k, :], xin[:, k, :], w=["xT.g%d" % i for i in range(len(g.groups) - 1)])
    g.dma("sp", g.cT[:], cin, w=["xT.g%d" % (len(g.groups) - 1)])
    g.dma("sp", g.cvec[:], cv, w=["cvec"])
    g.act(g.scvec[:], g.cvec[:], AF.Silu, r=["cvec"], w=["scvec"])
    rem = g.nc.sbuf_bytes_remaining
    g.ar = Arena(g, (rem - 14336) // 64 * 64)
    g.S.barrier()


def _res_src(self):
    g = self

    def src_fn(k, t0, n):
        if t0 >= g.TL:
            return g.cT[:, k, t0 - g.TL:t0 - g.TL + n]
        return g.xT[:, k, t0:t0 + n]
    return src_fn, (lambda gi: ["xT.g%d" % gi])


def _res_tile(self, k, t0, n):
    if t0 >= self.TL:
        return self.cT[:, k, t0 - self.TL:t0 - self.TL + n]
    return self.xT[:, k, t0:t0 + n]


def _layer0(self):
    g = self
    S = g.S
    TL, T, NCH = g.TL, g.T, g.NCH
    ar = g.ar
    W = {}
    for nm, shape in [("l0_ada_w", [D, 6 * D]), ("l0_ada_b", [6 * D]), ("l0_norm1_g", [D]), ("l0_norm2_g", [D]),
                      ("l0_w_in", [D, L0_IN]), ("l0_ret_log_decay", [2, 8]), ("l0_q_norm_g", [64]),
                      ("l0_k_norm_g", [64]), ("l0_w_out", [D, D]), ("l0_ffn_w1", [D, FFN]),
                      ("l0_ffn_w3", [D, FFN]), ("l0_ffn_w2", [FFN, D])]:
        W[nm] = g.inp(nm, shape)
    modT = g.ada(0, W["l0_ada_w"], W["l0_ada_b"], g.scvec)
    n1g = g.gain_cols("n1g0", W["l0_norm1_g"], 8)
    n2g = g.gain_cols("n2g0", W["l0_norm2_g"], 8)
    M = g.mod_derived(0, modT, n1g, n2g)
    gq = g.gain_cols("gq0", None, 1, pieces=[(0, 64, W["l0_q_norm_g"]), (64, 128, W["l0_q_norm_g"])])
    gk = g.gain_cols("gk0", None, 1, pieces=[(0, 64, W["l0_k_norm_g"]), (64, 128, W["l0_k_norm_g"])])
    lgb = g.sb("lgb", [128, 16], F32)
    g.dma("sp", lgb[:], W["l0_ret_log_decay"].rearrange("a b -> (a b)").partition_broadcast(128), w=["lgb"])
    lgp = g.sb("lgp", [128, 2, 4], F32)
    for dr in range(2):
        for half in range(2):
            src = lgb[half * 64:(half + 1) * 64, dr * 8 + half:dr * 8 + 8:2]
            g.cp("dve", lgp[half * 64:(half + 1) * 64, dr, :], src, r=["lgb"], w=["lgp"])
    GP = g.sb("GP", [128, 2, 4], F32)
    g.act(GP[:], lgp[:], AF.Exp, scale=128.0, r=["lgp"], w=["GP"])

    ar.reset()
    sq = ar.alloc([128, KT, 512], BF16)
    rstd = ar.alloc([128, 512], F32)
    tmpf = [ar.alloc([128, 512], F32) for _ in range(2)]
    src_fn, src_keys = g.res_src()
    g.norm_mod(src_fn, src_keys, g.hT, "hT", M["gs1"], M["sh1"], M["key"], (sq, rstd, tmpf))
    S.barrier()

    ar.reset()
    hkeys = ["hT.g%d" % i for i in range(len(g.groups))]
    wq = ar.alloc([128, KT, 512], BF16)
    wkv = ar.alloc([128, KT, 256], BF16)
    win = W["l0_w_in"].rearrange("(k p) c -> p k c", p=128)
    g.dma("pool", wq, win[:, :, 2048:2560], w=["wq"])
    g.dma("pool", wkv, win[:, :, 2560:2816], w=["wkv"])
    cosb = ar.alloc([128, 512], F32)
    sinb = ar.alloc([128, 512], F32)
    hn_tmp = (ar.alloc([128, 512], BF16), ar.alloc([128, 512], F32), ar.alloc([128, 512], F32),
              ar.alloc([128, 512], BF16), ar.alloc([128, 512], F32))
    obuf = [ar.alloc([128, 512], BF16) for _ in range(2)]
    vaug = [ar.alloc([128, 2, 65], BF16) for _ in range(2)]
    for i in range(2):
        g.memset("pool", vaug[i], 1.0, w=["vaug%d" % i])
    g.Q0 = g.scratch("Q0", [8, 64, T], BF16)
    if g.mode == "pre0":
        g.K0own = g.outp("K0own", [2, 64, TL], BF16)
        g.V0own = g.outp("V0own", [2, 128, NCH, 65], BF16)
        g.K0ctx = g.outp("K0ctx", [2, 64, LC], BF16)
        g.V0ctx = g.outp("V0ctx", [2, 128, LC // 128, 65], BF16)
    else:
        g.K0own = g.scratch("K0own", [2, 64, TL], BF16)
        g.V0own = g.scratch("V0own", [2, 128, NCH, 65], BF16)
        g.K0ctx = g.scratch("K0ctx", [2, 64, LC], BF16)
        g.V0ctx = g.scratch("V0ctx", [2, 128, LC // 128, 65], BF16)
    cnt = 0
    for gi, (t0, n, v) in enumerate(g.groups):
        rope = None
        if v == 0:
            g.dma("sp", cosb[:, 0:n], g.C_dram["cos0"][:, t0:t0 + n], w=["cosb"])
            g.dma("sp", sinb[:, 0:n], g.C_dram["sin0"][:, t0:t0 + n], w=["sinb"])
            rope = (cosb[0:64, 0:n], sinb[0:64, 0:n], g.C["rsw"], ["cosb", "sinb", "C_rsw"])
        for hh in range(10):
            pb = g.bank[hh % 2]
            bk = "bank%d" % (hh % 2)
            if hh < 2:
                wsl = [wkv[:, k, hh * 64:(hh + 1) * 64] for k in range(KT)]
                wk_ = "wkv"
                gain, gkey = gk, "gk0"
            else:
                wsl = [wq[:, k, (hh - 2) * 64:(hh - 1) * 64] for k in range(KT)]
                wk_ = "wq"
                gain, gkey = gq, "gq0"
            g.mmk(pb[0:64, 0:n], wsl, [g.hT[:, k, t0:t0 + n] for k in range(KT)], r=[wk_, "hT.g%d" % gi], w=[bk])
            ob = obuf[cnt % 2]
            okey = "obuf%d" % (cnt % 2)
            cnt += 1
            g.headnorm(64, n, pb[0:64, 0:n], bk, g.C["bo64"], "C_bo64", gain[0:64, 0:1], gkey, ob[0:64, 0:n], okey,
                       rope=rope, pbank=2, tmp=hn_tmp)
            if hh < 2:
                dst = g.K0own[hh, :, t0:t0 + n] if v == 0 else g.K0ctx[hh, :, :]
                g.dma("sp", dst, ob[0:64, 0:n], r=[okey], w=["K0own" if v == 0 else "K0ctx"])
            else:
                g.dma("sp", g.Q0[hh - 2, :, t0:t0 + n], ob[0:64, 0:n], r=[okey], w=["Q0"])
        for tt_ in range(n // 128):
            c0 = t0 + tt_ * 128
            pb = g.bank[3]
            g.mmk(pb[:, 0:128], [g.hT[:, k, c0:c0 + 128] for k in range(KT)], [wkv[:, k, 128:256] for k in range(KT)],
                  r=["wkv", "hT.g%d" % gi], w=["bank3"])
            va = vaug[tt_ % 2]
            g.cp("act", va[:, :, 0:64], pb[:, 0:128].rearrange("p (a b) -> p a b", b=64), r=["bank3"], w=["vaug%d" % (tt_ % 2)])
            for kvh in range(2):
                if v == 0:
                    dst = g.V0own[kvh, :, c0 // 128, :]
                else:
                    dst = g.V0ctx[kvh, :, (c0 - TL) // 128, :]
                g.dma("sp", dst, va[:, kvh, :], r=["vaug%d" % (tt_ % 2)], w=["V0own" if v == 0 else "V0ctx"])
    S.barrier()

    ar.base = 0
    ar.reset()
    g.raT = ar.alloc([128, 4, T], BF16)
    ar.base = ar.off
    ub_store = ar.alloc([128, 4, NCH + 2, 128], BF16)
    wrp = [ar.alloc([128, KT, 512], BF16) for _ in range(2)]
    ZF = ar.alloc([128, 2, 128], F32)
    kz = [ar.alloc([128, 128], BF16) for _ in range(2)]
    vb = ar.alloc([128, 128], BF16)
    Uacc = ar.alloc([128, 2, 4, 128], F32)
    sctx = ar.alloc([128, 2, 4, 128], F32)
    ut = ar.alloc([128, 2, 128], F32)
    posf = ar.alloc([128, 64], F32)
    posb = ar.alloc([128, 64], F32)
    g.dma("sp", posf, g.C_dram["posf"], w=["posf"])
    g.dma("sp", posb, g.C_dram["posb"], w=["posb"])
    g.memset("pool", Uacc, 0.0, w=["Uacc"])
    g.memset("pool", sctx, 0.0, w=["sctx"])
    if g.mode == "pre0":
        g.Uown = g.outp("Uown", [128, 2, 4, 128], F32)
    else:
        g.Uown = g.scratch("Uown", [128, 2, 4, 128], F32)

    def load_wrp(p):
        wb = wrp[p % 2]
        for qi in range(4):
            g.dma("pool", wb[:, :, qi * 128:(qi + 1) * 128], win[:, :, qi * 512 + p * 128:qi * 512 + (p + 1) * 128],
                  w=["wrp%d_%d" % (p % 2, qi)])
    g.load_wrp = load_wrp
    chunks = [(j * 128, j, 0) for j in range(NCH)] + [(TL + j * 128, NCH + j, 1) for j in range(2)]
    g.chunks = chunks
    load_wrp(0)
    for p in range(4):
        if p + 1 < 4:
            load_wrp(p + 1)
        wb = wrp[p % 2]
        wk_ = "wrp%d" % (p % 2)
        for dr, pos, pkey in ((0, posf, "posf"), (1, posb, "posb")):
            for half in range(2):
                g.act(ZF[:, dr, half * 64:(half + 1) * 64], pos[:, :], AF.Exp,
                      scale=lgb[:, dr * 8 + 2 * p + half:dr * 8 + 2 * p + half + 1], r=[pkey, "lgb"], w=["ZF"])
        g.ts("dve", ZF[:, :, :], ZF[:, :, :], 0.125, None, ALU.mult, r=["ZF"], w=["ZF"])
        for (c0, j, isctx) in chunks:
            gi = min(c0 // 512, len(g.groups) - 1) if not isctx else len(g.groups) - 1
            hk = "hT.g%d" % gi
            kp = g.bank[0]
            vp = g.bank[1]
            g.mmk(kp[:, 0:128], [g.hT[:, k, c0:c0 + 128] for k in range(KT)], [wb[:, k, 128:256] for k in range(KT)],
                  r=[wk_ + "_1", hk], w=["bank0"])
            g.mmk(vp[:, 0:128], [g.hT[:, k, c0:c0 + 128] for k in range(KT)], [wb[:, k, 256:384] for k in range(KT)],
                  r=[wk_ + "_2", hk], w=["bank1"])
            g.tt("dve", kz[0][:, :], kp[:, 0:128], ZF[:, 0, :], ALU.mult, r=["bank0", "ZF"], w=["kz0"])
            g.tt("dve", kz[1][:, :], kp[:, 0:128], ZF[:, 1, :], ALU.mult, r=["bank0", "ZF"], w=["kz1"])
            g.cp("act", vb[:, :], vp[:, 0:128], r=["bank1"], w=["vb"])
            up = g.bank[2]
            for dr in range(2):
                g.mm(up[:, dr * 128:(dr + 1) * 128], kz[dr][:, :], vb[:, :], True, True, r=["kz%d" % dr, "vb"], w=["bank2"])
            for dr in range(2):
                g.tt("dve", ut[:, dr, :], up[:, dr * 128:(dr + 1) * 128], g.C["bdmask"][:, :], ALU.mult,
                     r=["bank2", "C_bdmask"], w=["ut"])
            g.cp("pool", ub_store[:, p, j, :], ut[:, 1, :], r=["ut"], w=["ub_store"])
            acc = sctx if isctx else Uacc
            akey = "sctx" if isctx else "Uacc"
            g.stt("dve", acc[:, 0, p, :], acc[:, 0, p, :], GP[:, 0, p:p + 1], ut[:, 0, :], ALU.mult, ALU.add,
                  r=[akey, "GP", "ut"], w=[akey])
        for (lo, hi, acc, akey) in ((0, NCH, Uacc, "Uacc"), (NCH, NCH + 2, sctx, "sctx")):
            for j in range(hi - 1, lo - 1, -1):
                g.stt("dve", acc[:, 1, p, :], acc[:, 1, p, :], GP[:, 1, p:p + 1], ub_store[:, p, j, :], ALU.mult, ALU.add,
                      r=[akey, "GP", "ub_store"], w=[akey])
    g.dma("sp", g.Uown, Uacc, r=["Uacc"], w=["Uown"])
    g.ub_store, g.sctx, g.lgb, g.lgp, g.GP, g.M0, g.W0 = ub_store, sctx, lgb, lgp, GP, M, W
    g.Uacc = Uacc
    g.wrp = wrp
    g.ar_mark = ar.off


Gen.setup_common = _setup_common
Gen.res_src = _res_src
Gen.res_tile = _res_tile
Gen.layer0_pre = _layer0


def _layer0_main(self, stop=None):
    g = self
    S = g.S
    TL, T, NCH = g.TL, g.T, g.NCH
    ar = g.ar
    M, W = g.M0, g.W0
    lgb, lgp, GP = g.lgb, g.lgp, g.GP
    ub_store, sctx = g.ub_store, g.sctx
    wrp = g.wrp
    if g.mode == "fused":
        if stop == "4ax":
            g.stage_tst = g.scratch("stage_tst", [D, 128], F32)
            g.dma("sp", g.stage_tst, W["l0_w_in"][:, 0:128], w=["stage_tst"])
        S.barrier()
        rg = [list(range(NCORES))]
        nk, nv, nu = 2 * 64 * TL, 2 * 128 * NCH * 65, 128 * 1024
        k2 = g.scratch("K0all2d", [NCORES * nk // 512, 512], BF16)
        v2 = g.scratch("V0all2d", [NCORES * nv // 512, 512], BF16)
        u2 = g.scratch("Uall2d", [NCORES * nu // 256, 256], F32)
        for (src, dst, key) in ((g.K0own.rearrange("k d (a b) -> (k d a) b", b=512), k2, "K0all"),
                                (g.V0own.rearrange("k p j c -> (k p j c)").rearrange("(a b) -> a b", b=512), v2, "V0all"),
                                (g.Uown.rearrange("p a b c -> (p a b c)").rearrange("(x d) -> x d", d=256), u2, "Uall")):
            S.add("pool", lambda e, s_=src, d_=dst: e.collective_compute(
                "AllGather", ALU.bypass, replica_groups=rg, ins=[s_.opt()], outs=[d_.opt()]),
                ["K0own", "V0own", "Uown"], [key], dma=True, tag="cc")
        g.K0all = k2.rearrange("(r k d a) b -> k r d (a b)", r=NCORES, k=2, d=64)
        g.V0all = v2.rearrange("a b -> (a b)").rearrange("(r k p j c) -> k r p j c", r=NCORES, k=2, p=128, c=65)
        g.Uall = u2.rearrange("x d -> (x d)").rearrange("(r p a b c) -> r p a b c", r=NCORES, p=128, a=2, b=4)
    else:
        g.K0all = g.inp("K0all", [2, NCORES, 64, TL], BF16)
        g.V0all = g.inp("V0all", [2, NCORES, 128, NCH, 65], BF16)
        g.Uall = g.inp("Uall", [NCORES, 128, 2, 4, 128], F32)
    S.barrier()
    ar.off = g.ar_mark
    Sin = g.Uacc
    coef = ar.alloc([128, 2, 4, NCORES], F32)
    coefc = ar.alloc([128, 2, 4], F32)
    ex = ar.alloc([128, 4], F32)
    utmp = [ar.alloc([128, 2, 4, 128], F32) for _ in range(1)]
    cpos = g.C["cpos"]
    for dr in range(2):
        for r_ in range(NCORES):
            g.ts("dve", ex[:, :], lgp[:, dr, :], cpos[:, dr * 16 + r_:dr * 16 + r_ + 1], None, ALU.mult,
                 r=["lgp", "C_cpos"], w=["ex"])
            g.act(ex[:, :], ex[:, :], AF.Exp, r=["ex"], w=["ex"])
            g.ts("dve", coef[:, dr, :, r_], ex[:, :], cpos[:, dr * 16 + 8 + r_:dr * 16 + 9 + r_], None, ALU.mult,
                 r=["ex", "C_cpos"], w=["coef"])
        g.ts("dve", ex[:, :], lgp[:, dr, :], cpos[:, 32 + dr:33 + dr], None, ALU.mult, r=["lgp", "C_cpos"], w=["ex"])
        g.act(coefc[:, dr, :], ex[:, :], AF.Exp, r=["ex"], w=["coefc"])
        for p in range(4):
            g.ts("dve", Sin[:, dr, p, :], sctx[:, dr, p, :], coefc[:, dr, p:p + 1], None, ALU.mult,
                 r=["sctx", "coefc"], w=["Uacc"])
    for r_ in range(NCORES):
        ub = utmp[0]
        g.dma("sp", ub, g.Uall[r_], w=["utmp0"])
        for dr in range(2):
            for p in range(4):
                g.stt("dve", Sin[:, dr, p, :], ub[:, dr, p, :], coef[:, dr, p, r_:r_ + 1], Sin[:, dr, p, :],
                      ALU.mult, ALU.add, r=["utmp0", "coef", "Uacc"], w=["Uacc"])
    Rb = ar.alloc([128, 128], F32)
    tf = ar.alloc([128, 128], F32)
    for p in range(4):
        for (lo, hi, init) in ((0, NCH, True), (NCH, NCH + 2, False)):
            if init:
                g.cp("dve", Rb[:, :], Sin[:, 1, p, :], r=["Uacc"], w=["Rb"])
            else:
                g.memset("dve", Rb[:, :], 0.0, w=["Rb"])
            for j in range(hi - 1, lo - 1, -1):
                g.cp("dve", tf[:, :], ub_store[:, p, j, :], r=["ub_store"], w=["tf"])
                g.cp("dve", ub_store[:, p, j, :], Rb[:, :], r=["Rb", "tf"], w=["ub_store"])
                g.stt("dve", Rb[:, :], Rb[:, :], GP[:, 1, p:p + 1], tf[:, :], ALU.mult, ALU.add,
                      r=["Rb", "tf", "GP", "ub_store"], w=["Rb"])
    if stop == "4a":
        return
    if stop == "4ap":
        g.load_wrp(0)
        return
    if stop == "4ax":
        tst = ar.alloc([128, KT, 128], F32)
        g.dma("sp", tst, g.stage_tst.rearrange("(k p) c -> p k c", p=128), w=["tst"])
        return
    if stop == "4as":
        tst = ar.alloc([128, KT, 128], F32)
        g.dma("sp", tst, W["l0_w_in"].rearrange("(k p) c -> p k c", p=128)[:, :, 0:128], w=["tst"])
        return
    ramp = [ar.alloc([128, 128], F32) for _ in range(2)]
    rel = [ar.alloc([128, 128], F32) for _ in range(2)]
    ind = [ar.alloc([128, 128], F32) for _ in range(2)]
    for i, (a, b, c) in enumerate((("rampf", "relf", "indf"), ("rampb", "relb", "indb"))):
        g.dma("sp", ramp[i], g.C_dram[a], w=["ramp%d" % i])
        g.dma("sp", rel[i], g.C_dram[b], w=["rel%d" % i])
        g.dma("sp", ind[i], g.C_dram[c], w=["ind%d" % i])
    posf = ar.alloc([128, 64], F32)
    g.dma("sp", posf, g.C_dram["posf"], w=["posf2"])
    XI = ar.alloc([128, 2, 128], F32)
    MASK = ar.alloc([128, 2, 128], F32)
    mt = ar.alloc([128, 128], F32)
    ZFm = ar.alloc([128, 128], F32)
    qTb = ar.alloc([128, 128], BF16)
    qx = [ar.alloc([128, 128], BF16) for _ in range(2)]
    kTb = ar.alloc([128, 128], BF16)
    sg = ar.alloc([128, 128], F32)
    kzf = ar.alloc([128, 128], BF16)
    vbb = ar.alloc([128, 128], BF16)
    vpad = ar.alloc([128, 2, 128], BF16)
    ATb = ar.alloc([128, 256], BF16)
    Sf = ar.alloc([128, 128], F32)
    Sfb = [ar.alloc([128, 128], BF16) for _ in range(2)]
    of = ar.alloc([128, 128], F32)
    obf = ar.alloc([128, 128], BF16)
    cen = ar.alloc([128, 128], F32)
    sqc = ar.alloc([128, 128], BF16)
    rs = ar.alloc([128, 128], F32)
    yy = ar.alloc([128, 128], F32)
    g.memset("pool", vpad, 0.0, w=["vpad"])
    bo = g.C["bo64"]
    it = 0
    for p in range(4):
        g.load_wrp(p)
        wb = wrp[p % 2]
        wk = ["wrp%d_%d" % (p % 2, qi) for qi in range(4)]
        for dr in range(2):
            g.act(XI[:, dr, :], ramp[dr][:, :], AF.Exp, scale=lgp[:, dr, p:p + 1], r=["ramp%d" % dr, "lgp"], w=["XI"])
        for half in range(2):
            hcol = 2 * p + half
            g.act(MASK[:, half, :], rel[0][:, :], AF.Exp, scale=lgb[:, hcol:hcol + 1], r=["rel0", "lgb"], w=["MASK"])
            g.tt("dve", MASK[:, half, :], MASK[:, half, :], ind[0][:, :], ALU.mult, r=["MASK", "ind0"], w=["MASK"])
            g.act(mt[:, :], rel[1][:, :], AF.Exp, scale=lgb[:, 8 + hcol:9 + hcol], r=["rel1", "lgb"], w=["mt"])
            g.tt("dve", mt[:, :], mt[:, :], ind[1][:, :], ALU.mult, r=["mt", "ind1"], w=["mt"])
            g.tt("dve", MASK[:, half, :], MASK[:, half, :], mt[:, :], ALU.add, r=["MASK", "mt"], w=["MASK"])
            g.act(ZFm[:, half * 64:(half + 1) * 64], posf[:, :], AF.Exp, scale=lgb[:, hcol:hcol + 1],
                  r=["posf2", "lgb"], w=["ZFm"])
        g.ts("dve", ZFm[:, :], ZFm[:, :], 0.125, None, ALU.mult, r=["ZFm"], w=["ZFm"])
        for (c0, j, isctx) in g.chunks:
            gi = min(c0 // 512, len(g.groups) - 1) if not isctx else len(g.groups) - 1
            hk = "hT.g%d" % gi
            if j == 0:
                g.cp("dve", Sf[:, :], Sin[:, 0, p, :], r=["Uacc"], w=["Sf"])
            if j == NCH:
                g.memset("dve", Sf[:, :], 0.0, w=["Sf"])
            sfb = Sfb[it % 2]
            sfk = "Sfb%d" % (it % 2)
            it += 1
            g.cp("act", sfb[:, :], Sf[:, :], r=["Sf"], w=[sfk])
            hsl = [g.hT[:, k, c0:c0 + 128] for k in range(KT)]
            b0, b1, b2, b3, b4 = g.bank[0], g.bank[1], g.bank[2], g.bank[3], g.bank[4]
            b5, b6, b7 = g.bank[5], g.bank[6], g.bank[7]
            g.mmk(b0[:, 0:128], [wb[:, k, 0:128] for k in range(KT)], hsl, r=[wk[0], hk], w=["bank0"])
            g.mmk(b5[:, 0:128], [wb[:, k, 128:256] for k in range(KT)], hsl, r=[wk[1], hk], w=["bank5"])
            g.mmk(b5[:, 128:256], [wb[:, k, 384:512] for k in range(KT)], hsl, r=[wk[3], hk], w=["bank5"])
            g.mmk(b1[:, 0:128], hsl, [wb[:, k, 128:256] for k in range(KT)], r=[wk[1], hk], w=["bank1"])
            g.mmk(b6[:, 0:128], hsl, [wb[:, k, 256:384] for k in range(KT)], r=[wk[2], hk], w=["bank6"])
            g.cp("dve", qTb[:, :], b0[:, 0:128], r=["bank0"], w=["qTb"])
            for dr in range(2):
                g.tt("dve", qx[dr][:, :], b0[:, 0:128], XI[:, dr, :], ALU.mult, r=["bank0", "XI"], w=["qx%d" % dr])
            g.act(kTb[:, :], b5[:, 0:128], AF.Identity, scale=0.125, r=["bank5"], w=["kTb"])
            g.act(sg[:, :], b5[:, 128:256], AF.Silu, r=["bank5"], w=["sg"])
            g.tt("dve", kzf[:, :], b1[:, 0:128], ZFm[:, :], ALU.mult, r=["bank1", "ZFm"], w=["kzf"])
            g.cp("act", vbb[:, :], b6[:, 0:128], r=["bank6"], w=["vbb"])
            g.cp("pool", vpad[:, 0, 0:64], vbb[:, 0:64], r=["vbb"], w=["vpad"])
            g.cp("pool", vpad[:, 1, 64:128], vbb[:, 64:128], r=["vbb"], w=["vpad"])
            g.mm(b2[:, 0:128], kTb[0:64, :], qTb[0:64, :], True, True, r=["kTb", "qTb"], w=["bank2"])
            g.mm(b7[:, 0:128], kTb[64:128, :], qTb[64:128, :], True, True, r=["kTb", "qTb"], w=["bank7"])
            g.tt("dve", ATb[:, 0:128], b2[:, 0:128], MASK[:, 0, :], ALU.mult, r=["bank2", "MASK"], w=["ATb"])
            g.tt("dve", ATb[:, 128:256], b7[:, 0:128], MASK[:, 1, :], ALU.mult, r=["bank7", "MASK"], w=["ATb"])
            g.mm(b3[:, 0:128], vpad[:, 0, :], ATb[:, 0:128], True, False, r=["vpad", "ATb"], w=["bank3"])
            g.mm(b3[:, 0:128], vpad[:, 1, :], ATb[:, 128:256], False, False, r=["vpad", "ATb"], w=["bank3"])
            g.mm(b3[:, 0:128], sfb[:, :], qx[0][:, :], False, False, r=[sfk, "qx0"], w=["bank3"])
            g.mm(b3[:, 0:128], ub_store[:, p, j, :], qx[1][:, :], False, True, r=["ub_store", "qx1"], w=["bank3"])
            g.mm(b2[:, 256:384], kzf[:, :], vbb[:, :], True, True, r=["kzf", "vbb", "ATb"], w=["bank2"])
            g.tt("dve", mt[:, :], b2[:, 256:384], g.C["bdmask"][:, :], ALU.mult, r=["bank2", "C_bdmask"], w=["mt"])
            g.stt("dve", Sf[:, :], Sf[:, :], GP[:, 0, p:p + 1], mt[:, :], ALU.mult, ALU.add, r=["Sf", "mt", "GP", sfk], w=["Sf"])
            g.cp("act", of[:, :], b3[:, 0:128], r=["bank3"], w=["of"])
            g.cp("pool", obf[:, :], of[:, :], r=["of"], w=["obf"])
            g.mm(b4[:, 0:128], bo[:, :], obf[:, :], True, True, r=["obf", "C_bo64"], w=["bank4"])
            g.tt("dve", cen[:, :], of[:, :], b4[:, 0:128], ALU.subtract, r=["of", "bank4"], w=["cen"])
            g.tt("pool", sqc[:, :], cen[:, :], cen[:, :], ALU.mult, r=["cen"], w=["sqc"])
            g.mm(b4[:, 128:256], bo[:, :], sqc[:, :], True, True, r=["sqc", "C_bo64", "cen"], w=["bank4"])
            g.act(rs[:, :], b4[:, 128:256], AF.Sqrt, bias=g.epsc[:, 0:1], r=["bank4", "epsc"], w=["rs"])
            g.recip(rs[:, :], rs[:, :], r=["rs"], w=["rs"])
            g.tt("dve", yy[:, :], cen[:, :], rs[:, :], ALU.mult, r=["cen", "rs"], w=["yy"])
            g.tt("pool", g.raT[:, p, c0:c0 + 128], yy[:, :], sg[:, :], ALU.mult, r=["yy", "sg"], w=["raT"])
    S.barrier()
    if stop == "4b":
        return

    ar.reset()
    g.atT = ar.alloc([128, 4, T], BF16)
    ar.base = ar.off
    CK = min(1024, TL)
    tmp = ([ar.alloc([128, T], BF16) for _ in range(2)], [ar.alloc([128, CK], BF16) for _ in range(3)],
           [ar.alloc([128, CK // 128, 65], BF16) for _ in range(3)], [ar.alloc([128, 512], BF16) for _ in range(3)],
           ar.alloc([128, 512], F32), ar.alloc([128, 512], F32), ar.alloc([128, 512], BF16),
           ar.alloc([128, 512], BF16), ar.alloc([128, 512], BF16))

    def ksegs_lat(h):
        kvh = h // 4
        segs = []
        for r_ in range(NCORES):
            for c in range(TL // CK):
                segs.append((g.K0all[kvh, r_, :, c * CK:(c + 1) * CK],
                             g.V0all[kvh, r_, :, c * CK // 128:(c + 1) * CK // 128, :], "K0all", "V0all"))
        segs.append((g.K0ctx[kvh], g.V0ctx[kvh], "K0ctx", "V0ctx"))
        return segs

    g.attention("a0", 64, 0.125, 8, lambda h: g.Q0[h, :, 0:TL], ksegs_lat, TL, g.atT, "atT", 0, tmp)
    g.attention("a0", 64, 0.125, 8, lambda h: g.Q0[h, :, TL:T],
                lambda h: [(g.K0ctx[h // 4], g.V0ctx[h // 4], "K0ctx", "V0ctx")], LC, g.atT, "atT", TL, tmp)
    S.barrier()
    if stop == "5":
        return

    ar.reset()
    wo = ar.alloc([128, KT, D], BF16)
    g.dma("pool", wo, W["l0_w_out"].rearrange("(k p) c -> p k c", p=128), w=["wo"])
    for gi, (t0, n, v) in enumerate(g.groups):
        cat = [g.raT[:, kt, t0:t0 + n] for kt in range(4)] + [g.atT[:, kt, t0:t0 + n] for kt in range(4)]
        for dc in range(8):
            pb = g.bank[dc % 4]
            bk = "bank%d" % (dc % 4)
            g.mmk(pb[:, 0:n], [wo[:, kt, dc * 128:(dc + 1) * 128] for kt in range(8)], cat, r=["wo", "raT", "atT"], w=[bk])
            res = g.res_tile(dc, t0, n)
            g.stt("dve", res, pb[:, 0:n], M["gate1"][:, dc, v:v + 1], res, ALU.mult, ALU.add,
                  r=[bk, "xT.g%d" % gi] + M["key"], w=["xT.g%d" % gi])
    S.barrier()
    if stop == "6":
        return

    ar.reset()
    sq = ar.alloc([128, KT, 512], BF16)
    rstd = ar.alloc([128, 512], F32)
    tmpf = [ar.alloc([128, 512], F32) for _ in range(2)]
    src_fn, src_keys = g.res_src()
    g.norm_mod(src_fn, src_keys, g.hT, "hT", M["gs2"], M["sh2"], M["key"], (sq, rstd, tmpf))
    FC = 256
    NFC = FFN // FC
    w1v = W["l0_ffn_w1"].rearrange("(k p) f -> p k f", p=128)
    w3v = W["l0_ffn_w3"].rearrange("(k p) f -> p k f", p=128)
    w2v = W["l0_ffn_w2"].rearrange("(ft p) d -> p ft d", p=128)
    w1c = [ar.alloc([128, KT, FC], BF16) for _ in range(2)]
    w3c = [ar.alloc([128, KT, FC], BF16) for _ in range(2)]
    w2c = [ar.alloc([128, FC // 128, D], BF16) for _ in range(2)]
    gT = [ar.alloc([128, FC // 128, 512], BF16) for _ in range(2)]
    sil = [ar.alloc([128, 512], F32) for _ in range(2)]

    def load_fc(fc):
        i = fc % 2
        g.dma("pool", w1c[i], w1v[:, :, fc * FC:(fc + 1) * FC], w=["w1c%d" % i])
        g.dma("pool", w3c[i], w3v[:, :, fc * FC:(fc + 1) * FC], w=["w3c%d" % i])
        g.dma("pool", w2c[i], w2v[:, fc * (FC // 128):(fc + 1) * (FC // 128), :], w=["w2c%d" % i])
    load_fc(0)
    cnt = 0
    for fc in range(NFC):
        if fc + 1 < NFC:
            load_fc(fc + 1)
        i = fc % 2
        for gi, (t0, n, v) in enumerate(g.groups):
            hk = "hT.g%d" % gi
            gt = gT[cnt % 2]
            gk_ = "gT%d" % (cnt % 2)
            cnt += 1
            hsl = [g.hT[:, k, t0:t0 + n] for k in range(KT)]
            for ft in range(FC // 128):
                h1, h3 = g.bank[2 * ft], g.bank[2 * ft + 1]
                g.mmk(h1[:, 0:n], [w1c[i][:, k, ft * 128:(ft + 1) * 128] for k in range(KT)], hsl,
                      r=["w1c%d" % i, hk], w=["bank%d" % (2 * ft)])
                g.mmk(h3[:, 0:n], [w3c[i][:, k, ft * 128:(ft + 1) * 128] for k in range(KT)], hsl,
                      r=["w3c%d" % i, hk], w=["bank%d" % (2 * ft + 1)])
                g.act(sil[ft][:, 0:n], h1[:, 0:n], AF.Silu, r=["bank%d" % (2 * ft)], w=["sil%d" % ft])
                g.tt("dve", gt[:, ft, 0:n], sil[ft][:, 0:n], h3[:, 0:n], ALU.mult,
                     r=["sil%d" % ft, "bank%d" % (2 * ft + 1)], w=[gk_])
            for dc in range(8):
                pb = g.bank[4 + dc % 4]
                bk = "bank%d" % (4 + dc % 4)
                g.mmk(pb[:, 0:n], [w2c[i][:, ft, dc * 128:(dc + 1) * 128] for ft in range(FC // 128)],
                      [gt[:, ft, 0:n] for ft in range(FC // 128)], r=["w2c%d" % i, gk_], w=[bk])
                res = g.res_tile(dc, t0, n)
                g.stt("dve", res, pb[:, 0:n], M["gate2"][:, dc, v:v + 1], res, ALU.mult, ALU.add,
                      r=[bk, "xT.g%d" % gi] + M["key"], w=["xT.g%d" % gi])
    S.barrier()
    ar.base = 0
    ar.reset()


def _out_layer0(self):
    g = self
    xo = g.outp("xT_out", [128, KT, g.TL])
    co = g.outp("cT_out", [128, KT, LC])
    for k in range(KT):
        g.dma("sp", xo[:, k, :], g.xT[:, k, :], r=["xT.g%d" % i for i in range(len(g.groups))])
    g.dma("sp", co, g.cT[:], r=["xT.g%d" % (len(g.groups) - 1)])


Gen.layer0_main = _layer0_main
Gen.out_layer0 = _out_layer0


def _to_fm(a, n):
    return np.ascontiguousarray(a.reshape(n, KT, 128).transpose(2, 1, 0))


def _from_fm(a):
    n = a.shape[2]
    return np.ascontiguousarray(a.transpose(2, 1, 0).reshape(n, D))


def core_inputs(inputs, TL, core, xT=None, cT=None):
    m = {}
    for k, v in host_consts(core, TL).items():
        m["c_" + k] = v
    x = inputs["x"][0]
    if xT is None:
        m["xT_in"] = _to_fm(x[core * TL:(core + 1) * TL], TL)
        m["cT_in"] = _to_fm(inputs["ctx"][0], LC)
    else:
        m["xT_in"], m["cT_in"] = xT, cT
    cv = np.stack([inputs["c"][0], inputs["c_ctx"]], axis=-1)
    m["cvec"] = np.ascontiguousarray(cv.reshape(KT, 128, 2).transpose(1, 0, 2))
    return m


def run_prog(g, in_maps):
    names = set(g.din.keys())
    maps = [{k: v for k, v in m.items() if k in names} for m in in_maps]
    for m in maps:
        missing = names - set(m.keys())
        assert not missing, missing
    res = run_bass_kernel_spmd(g.nc, maps, core_ids=list(range(NCORES)))
    return res.results


def build_l0(TL, mode, stop=None):
    g = Gen(TL, mode)
    g.setup_common()
    g.layer0_pre()
    if mode == "main0":
        g.layer0_main(stop)
        g.out_layer0()
    g.finish()
    return g


def run_layer0(inputs, TL, stop=None):
    L0 = [k for k in inputs if k.startswith("l0_")]
    base = []
    for c in range(NCORES):
        m = core_inputs(inputs, TL, c)
        for k in L0:
            m[k] = inputs[k]
        base.append(m)
    g1 = build_l0(TL, "pre0")
    r1 = run_prog(g1, base)
    K0all = np.ascontiguousarray(np.stack([r["K0own"] for r in r1], axis=1))
    V0all = np.ascontiguousarray(np.stack([r["V0own"] for r in r1], axis=1))
    Uall = np.ascontiguousarray(np.stack([r["Uown"] for r in r1], axis=0))
    for m in base:
        m["K0all"], m["V0all"], m["Uall"] = K0all, V0all, Uall
    g2 = build_l0(TL, "main0", stop)
    r2 = run_prog(g2, base)
    return [r["xT_out"] for r in r2], [r["cT_out"] for r in r2]


def _layer1_pre(self):
    g = self
    S = g.S
    TL, T, NCH = g.TL, g.T, g.NCH
    ar = g.ar
    W = {}
    for nm, shape in [("l1_ada_w", [D, 6 * D]), ("l1_ada_b", [6 * D]), ("l1_norm1_g", [D]), ("l1_norm2_g", [D]),
                      ("l1_w_in", [D, L1_IN]), ("l1_q_lora_g", [384]), ("l1_kv_lora_g", [256]),
                      ("l1_w_uq", [384, 768]), ("l1_w_ukv", [256, 1024]), ("l1_q_nope_g", [64]),
                      ("l1_q_rope_g", [32]), ("l1_k_nope_g", [64]), ("l1_k_rope_g", [32]),
                      ("l1_w_out", [512, D]), ("l1_router", [D, NE]), ("l1_exp_w1", [NE, D, EDIM]),
                      ("l1_exp_w3", [NE, D, EDIM]), ("l1_exp_w2", [NE, EDIM, D])]:
        if g.mode == "pre1" and nm in ("l1_w_out", "l1_router", "l1_exp_w1", "l1_exp_w3", "l1_exp_w2"):
            continue
        W[nm] = g.inp(nm, shape)
    g.W1 = W
    modT = g.ada(1, W["l1_ada_w"], W["l1_ada_b"], g.scvec)
    n1g = g.gain_cols("n1g1", W["l1_norm1_g"], 8)
    n2g = g.gain_cols("n2g1", W["l1_norm2_g"], 8)
    M = g.mod_derived(1, modT, n1g, n2g)
    g.M1 = M
    qlg = g.gain_cols("qlg", W["l1_q_lora_g"], 3)
    kvlg = g.gain_cols("kvlg", W["l1_kv_lora_g"], 2)
    gq1 = g.gain_cols("gq1", None, 1, pieces=[(0, 64, W["l1_q_nope_g"]), (64, 96, W["l1_q_rope_g"])])
    gk1 = g.gain_cols("gk1", None, 1, pieces=[(0, 64, W["l1_k_nope_g"]), (64, 96, W["l1_k_rope_g"])])

    ar.reset()
    sq = ar.alloc([128, KT, 512], BF16)
    rstd = ar.alloc([128, 512], F32)
    tmpf = [ar.alloc([128, 512], F32) for _ in range(2)]
    src_fn, src_keys = g.res_src()
    g.norm_mod(src_fn, src_keys, g.hT, "hT", M["gs1"], M["sh1"], M["key"], (sq, rstd, tmpf))
    S.barrier()

    ar.reset()
    wi1 = ar.alloc([128, KT, L1_IN], BF16)
    g.dma("pool", wi1, W["l1_w_in"].rearrange("(k p) c -> p k c", p=128), w=["wi1"])
    wuq = ar.alloc([128, 3, 768], BF16)
    g.dma("pool", wuq, W["l1_w_uq"].rearrange("(k p) c -> p k c", p=128), w=["wuq"])
    wukp = ar.alloc([128, 2, 8, 96], BF16)
    wuv = ar.alloc([128, 2, 8, 64], BF16)
    wkrp = ar.alloc([128, KT, 96], BF16)
    g.memset("pool", wukp, 0.0, w=["wukp"])
    g.memset("pool", wkrp, 0.0, w=["wkrp"])
    ukv = W["l1_w_ukv"].rearrange("(ct p) (h c) -> p ct h c", p=128, c=128)
    for ct in range(2):
        g.dma("pool", wukp[:, ct, :, 0:64], ukv[:, ct, :, 0:64], r=["wukp"], w=["wukp_%d" % ct])
        g.dma("pool", wuv[:, ct, :, :], ukv[:, ct, :, 64:128], w=["wuv_%d" % ct])
    g.dma("pool", wkrp[:, :, 64:96], W["l1_w_in"].rearrange("(k p) c -> p k c", p=128)[:, :, 640:672], r=["wkrp"], w=["wkrp_d"])
    wkeys = ["wukp", "wukp_0", "wukp_1", "wkrp", "wkrp_d"]
    cosb = ar.alloc([128, 512], F32)
    sinb = ar.alloc([128, 512], F32)
    hn_tmp = (ar.alloc([128, 512], BF16), ar.alloc([128, 512], F32), ar.alloc([128, 512], F32),
              ar.alloc([128, 512], BF16), ar.alloc([128, 512], F32))
    obuf = [ar.alloc([128, 512], BF16) for _ in range(2)]
    vaug = [ar.alloc([128, 8, 65], BF16) for _ in range(2)]
    for i in range(2):
        g.memset("pool", vaug[i], 1.0, w=["vaug%d" % i])
    cf = ar.alloc([128, 3, 512], F32)
    csq = ar.alloc([128, 3, 512], BF16)
    crs = ar.alloc([128, 512], F32)
    cqn = ar.alloc([128, 3, 512], BF16)
    ckvn = ar.alloc([128, 2, 512], BF16)
    g.Q1 = g.scratch("Q1", [8, 96, TL], BF16)
    mk = g.outp if g.mode == "pre1" else g.scratch
    g.K1own = mk("K1own", [8, 96, TL], BF16)
    g.V1own = mk("V1own", [8, 128, NCH, 65], BF16)
    g.K1ctx = mk("K1ctx", [8, 96, LC], BF16)
    g.V1ctx = mk("V1ctx", [8, 128, LC // 128, 65], BF16)

    def lora_norm(gi, t0, n, col0, ntile, gains, gkey, out, okey, divisor):
        for ct in range(ntile):
            pb = g.bank[ct]
            g.mmk(pb[:, 0:n], [wi1[:, k, col0 + ct * 128:col0 + (ct + 1) * 128] for k in range(KT)],
                  [g.hT[:, k, t0:t0 + n] for k in range(KT)], r=["wi1", "hT.g%d" % gi], w=["bank%d" % ct])
            g.cp("act", cf[:, ct, 0:n], pb[:, 0:n], r=["bank%d" % ct], w=["cf%d" % ct])
            g.tt("pool", csq[:, ct, 0:n], cf[:, ct, 0:n], cf[:, ct, 0:n], ALU.mult, r=["cf%d" % ct], w=["csq%d" % ct])
        pb = g.bank[3]
        g.mmk(pb[:, 0:n], [g.C["ones_bf"][:, :]] * ntile, [csq[:, ct, 0:n] for ct in range(ntile)],
              r=["csq%d" % ct for ct in range(ntile)] + ["C_ones_bf"], w=["bank3"])
        g.act(crs[:, 0:n], pb[:, 0:n], AF.Sqrt, bias=g.epsc[:, 0:1], scale=1.0 / divisor, r=["bank3", "epsc"], w=["crs"])
        g.recip(crs[:, 0:n], crs[:, 0:n], r=["crs"], w=["crs"])
        for ct in range(ntile):
            g.stt("dve", out[:, ct, 0:n], cf[:, ct, 0:n], gains[:, ct:ct + 1], crs[:, 0:n], ALU.mult, ALU.mult,
                  r=["cf%d" % ct, "crs", gkey], w=[okey])

    cnt = 0
    for gi, (t0, n, v) in enumerate(g.groups):
        rope = None
        if v == 0:
            g.dma("sp", cosb[:, 0:n], g.C_dram["cos1"][:, t0:t0 + n], w=["cosb"])
            g.dma("sp", sinb[:, 0:n], g.C_dram["sin1"][:, t0:t0 + n], w=["sinb"])
            rope = (cosb[0:96, 0:n], sinb[0:96, 0:n], g.C["rsw96"], ["cosb", "sinb", "C_rsw96"])
            lora_norm(gi, t0, n, 0, 3, qlg, "qlg", cqn, "cqn", 384.0)
        lora_norm(gi, t0, n, 384, 2, kvlg, "kvlg", ckvn, "ckvn", 256.0)
        for hh in range(16 if v == 0 else 8):
            h = hh % 8
            pb = g.bank[4 + hh % 2]
            bk = "bank%d" % (4 + hh % 2)
            if hh < 8:
                g.mmk(pb[0:96, 0:n], [wukp[:, ct, h, :] for ct in range(2)] + [wkrp[:, k, :] for k in range(KT)],
                      [ckvn[:, ct, 0:n] for ct in range(2)] + [g.hT[:, k, t0:t0 + n] for k in range(KT)],
                      r=wkeys + ["ckvn", "hT.g%d" % gi], w=[bk])
                gain, gkey = gk1, "gk1"
            else:
                g.mmk(pb[0:96, 0:n], [wuq[:, ct, h * 96:(h + 1) * 96] for ct in range(3)],
                      [cqn[:, ct, 0:n] for ct in range(3)], r=["wuq", "cqn"], w=[bk])
                gain, gkey = gq1, "gq1"
            ob = obuf[cnt % 2]
            okey = "obuf%d" % (cnt % 2)
            cnt += 1
            g.headnorm(96, n, pb[0:96, 0:n], bk, g.C["bm96"], "C_bm96", gain[0:96, 0:1], gkey, ob[0:96, 0:n], okey,
                       rope=rope, pbank=6, tmp=hn_tmp)
            if hh < 8:
                dst = g.K1own[h, :, t0:t0 + n] if v == 0 else g.K1ctx[h, :, :]
                g.dma("sp", dst, ob[0:96, 0:n], r=[okey], w=["K1own" if v == 0 else "K1ctx"])
            else:
                g.dma("sp", g.Q1[h, :, t0:t0 + n], ob[0:96, 0:n], r=[okey], w=["Q1"])
        for tt_ in range(n // 128):
            c0 = tt_ * 128
            pb = g.bank[7]
            g.mmk(pb[:, 0:512], [ckvn[:, ct, c0:c0 + 128] for ct in range(2)],
                  [wuv[:, ct, :, :].rearrange("p h c -> p (h c)") for ct in range(2)],
                  r=["wuv_0", "wuv_1", "ckvn"], w=["bank7"])
            va = vaug[tt_ % 2]
            g.cp("act", va[:, :, 0:64], pb[:, 0:512].rearrange("p (a b) -> p a b", b=64), r=["bank7"], w=["vaug%d" % (tt_ % 2)])
            blk = (t0 + c0) // 128 if v == 0 else (t0 + c0 - TL) // 128
            dst = (g.V1own if v == 0 else g.V1ctx)[:, :, blk, :].rearrange("h p c -> p h c")
            g.dma("sp", dst, va[:, :, :], r=["vaug%d" % (tt_ % 2)], w=["V1own" if v == 0 else "V1ctx"])
    S.barrier()


def _layer1_main(self, stop=None):
    g = self
    S = g.S
    TL, T, NCH = g.TL, g.T, g.NCH
    ar = g.ar
    M, W = g.M1, g.W1
    if g.mode == "fused":
        S.barrier()
        rg = [list(range(NCORES))]
        nk, nv = 8 * 96 * TL, 8 * 128 * NCH * 65
        k2 = g.scratch("K1all2d", [NCORES * nk // 512, 512], BF16)
        v2 = g.scratch("V1all2d", [NCORES * nv // 512, 512], BF16)
        for (src, dst, key) in ((g.K1own.rearrange("h d (a b) -> (h d a) b", b=512), k2, "K1all"),
                                (g.V1own.rearrange("h p j c -> (h p j c)").rearrange("(a b) -> a b", b=512), v2, "V1all")):
            S.add("pool", lambda e, s_=src, d_=dst: e.collective_compute(
                "AllGather", ALU.bypass, replica_groups=rg, ins=[s_.opt()], outs=[d_.opt()]),
                ["K1own", "V1own"], [key], dma=True, tag="cc")
        g.K1all = k2.rearrange("(r h d a) b -> h r d (a b)", r=NCORES, h=8, d=96)
        g.V1all = v2.rearrange("a b -> (a b)").rearrange("(r h p j c) -> h r p j c", r=NCORES, h=8, p=128, c=65)
    else:
        g.K1all = g.inp("K1all", [8, NCORES, 96, TL], BF16)
        g.V1all = g.inp("V1all", [8, NCORES, 128, NCH, 65], BF16)
    S.barrier()
    ar.base = 0
    ar.reset()
    g.atT = ar.alloc([128, 4, T], BF16)
    ar.base = ar.off
    CK = min(1024, TL)
    tmp = ([ar.alloc([128, T], BF16) for _ in range(2)], [ar.alloc([128, CK], BF16) for _ in range(3)],
           [ar.alloc([128, CK // 128, 65], BF16) for _ in range(3)], [ar.alloc([128, 512], BF16) for _ in range(3)],
           ar.alloc([128, 512], F32), ar.alloc([128, 512], F32), ar.alloc([128, 512], BF16),
           ar.alloc([128, 512], BF16), ar.alloc([128, 512], BF16))

    def ksegs(h):
        segs = []
        for r_ in range(NCORES):
            for c in range(TL // CK):
                segs.append((g.K1all[h, r_, :, c * CK:(c + 1) * CK],
                             g.V1all[h, r_, :, c * CK // 128:(c + 1) * CK // 128, :], "K1all", "V1all"))
        segs.append((g.K1ctx[h], g.V1ctx[h], "K1ctx", "V1ctx"))
        return segs

    g.attention("a1", 96, 96.0 ** -0.5, 8, lambda h: g.Q1[h, :, 0:TL], ksegs, TL, g.atT, "atT", 0, tmp)
    S.barrier()
    if stop == "attn":
        return
    ar.reset()
    lat_groups = [gr for gr in g.groups if gr[2] == 0]
    wo = ar.alloc([128, 4, D], BF16)
    g.dma("pool", wo, W["l1_w_out"].rearrange("(k p) c -> p k c", p=128), w=["wo1"])
    for gi, (t0, n, v) in enumerate(lat_groups):
        cat = [g.atT[:, kt, t0:t0 + n] for kt in range(4)]
        for dc in range(8):
            pb = g.bank[dc % 4]
            bk = "bank%d" % (dc % 4)
            g.mmk(pb[:, 0:n], [wo[:, kt, dc * 128:(dc + 1) * 128] for kt in range(4)], cat, r=["wo1", "atT"], w=[bk])
            res = g.res_tile(dc, t0, n)
            g.stt("dve", res, pb[:, 0:n], M["gate1"][:, dc, v:v + 1], res, ALU.mult, ALU.add,
                  r=[bk, "xT.g%d" % gi] + M["key"], w=["xT.g%d" % gi])
    S.barrier()
    if stop == "wout":
        return
    ar.base = 0
    ar.reset()
    NG = len(lat_groups)
    gT_sb = ar.alloc([8, TL], F32)
    esel_d = g.inp("c_esel", [8, NE * 128], F32)
    esel = ar.alloc([8, NE * 128], F32)
    g.dma("sp", esel, esel_d, w=["esel"])
    gbc = ar.alloc([128, NG, 512], F32)
    moe_mark = ar.off
    sq = ar.alloc([128, KT, 512], BF16)
    rstd = ar.alloc([128, 512], F32)
    tmpf = [ar.alloc([128, 512], F32) for _ in range(2)]
    h2f = ar.alloc([128, KT, 512], F32)
    rt = ar.alloc([128, KT, NE], F32)
    g.dma("sp", rt, W["l1_router"].rearrange("(k p) e -> p k e", p=128), w=["rt"])
    lg = ar.alloc([128, NCH, NE], F32)
    gates = ar.alloc([128, NCH, NE], F32)
    sm = [ar.alloc([128, 8], F32) for _ in range(4)]
    col = [ar.alloc([128, 1], F32) for _ in range(5)]
    for gi, (t0, n, v) in enumerate(lat_groups):
        pb = g.bank[6]
        for k in range(KT):
            eng = ("act", "pool", "dve")[k % 3]
            src = g.xT[:, k, t0:t0 + n]
            if eng == "act":
                g.act(sq[:, k, 0:n], src, AF.Square, r=["xT.g%d" % gi], w=["nm_sq%d" % k])
            else:
                g.tt(eng, sq[:, k, 0:n], src, src, ALU.mult, r=["xT.g%d" % gi], w=["nm_sq%d" % k])
        g.mmk(pb[:, 0:n], [g.C["ones_bf"][:, :]] * KT, [sq[:, k, 0:n] for k in range(KT)],
              r=["nm_sq%d" % k for k in range(KT)] + ["C_ones_bf"], w=["bank6"])
        g.act(rstd[:, 0:n], pb[:, 0:n], AF.Sqrt, bias=g.epsc[:, 0:1], scale=1.0 / D, r=["bank6", "epsc"], w=["nm_rstd"])
        g.recip(rstd[:, 0:n], rstd[:, 0:n], r=["nm_rstd"], w=["nm_rstd"])
        for k in range(KT):
            tf = tmpf[k % 2]
            g.tt("dve" if k % 2 == 0 else "pool", tf[:, 0:n], g.xT[:, k, t0:t0 + n], rstd[:, 0:n], ALU.mult,
                 r=["xT.g%d" % gi, "nm_rstd"], w=["nm_tmp%d" % (k % 2)])
            g.act(h2f[:, k, 0:n], tf[:, 0:n], AF.Identity, bias=M["sh2"][:, k, 0:1], scale=M["gs2"][:, k, 0:1],
                  r=["nm_tmp%d" % (k % 2)] + M["key"], w=["h2f%d" % k])
            g.cp("pool", g.hT[:, k, t0:t0 + n], h2f[:, k, 0:n], r=["h2f%d" % k], w=["hT.g%d" % gi])
        for tt_ in range(n // 128):
            tile_i = (t0 + tt_ * 128) // 128
            pb2 = g.bank[7]
            g.mmk(pb2[:, 0:NE], [h2f[:, k, tt_ * 128:(tt_ + 1) * 128] for k in range(KT)], [rt[:, k, :] for k in range(KT)],
                  r=["h2f%d" % k for k in range(KT)] + ["rt"], w=["bank7"])
            g.cp("dve", lg[:, tile_i, :], pb2[:, 0:NE], r=["bank7"], w=["lg"])
    for ti in range(NCH):
        lt = lg[:, ti, :]
        m1, m2, nm1, den, rden = [c[:, 0:1] for c in col]
        eq, lg2, sel, ex = [s_[:, :] for s_ in sm]
        g.op("dve", lambda e, o=m1, i=lt: e.tensor_reduce(o, i, AX.X, ALU.max), r=["lg"], w=["c_m1"])
        g.ts("dve", eq, lt, m1, None, ALU.is_equal, r=["lg", "c_m1"], w=["s_eq"])
        g.stt("dve", lg2, eq, -1e30, lt, ALU.mult, ALU.add, r=["s_eq", "lg"], w=["s_lg2"])
        g.op("dve", lambda e, o=m2, i=lg2: e.tensor_reduce(o, i, AX.X, ALU.max), r=["s_lg2"], w=["c_m2"])
        g.ts("dve", sel, lt, m2, None, ALU.is_ge, r=["lg", "c_m2"], w=["s_sel"])
        g.ts("dve", nm1, m1, -1.0, None, ALU.mult, r=["c_m1"], w=["c_nm1"])
        g.act(ex, lt, AF.Exp, bias=nm1, scale=1.0, r=["lg", "c_nm1"], w=["s_ex"])
        g.tt("dve", ex, ex, sel, ALU.mult, r=["s_ex", "s_sel"], w=["s_ex"])
        g.op("dve", lambda e, o=den, i=ex: e.tensor_reduce(o, i, AX.X, ALU.add), r=["s_ex"], w=["c_den"])
        g.recip(rden, den, r=["c_den"], w=["c_rden"])
        g.ts("dve", gates[:, ti, :], ex, rden, None, ALU.mult, r=["s_ex", "c_rden"], w=["gates"])
        pb = g.bank[6]
        g.mm(pb[0:8, 0:128], gates[:, ti, :], g.C["ident_f"][:, :], True, True, r=["gates", "C_ident_f"], w=["bank6"])
        g.cp("act", gT_sb[0:8, ti * 128:(ti + 1) * 128], pb[0:8, 0:128], r=["bank6"], w=["gT_sb"])
    if stop == "gates":
        go = g.outp("gates_out", [8, TL])
        g.dma("sp", go, gT_sb, r=["gT_sb"])
        return
    S.barrier()
    ar.off = moe_mark
    FC = 256
    NFC = EDIM // FC
    w1c = [ar.alloc([128, KT, FC], BF16) for _ in range(2)]
    w3c = [ar.alloc([128, KT, FC], BF16) for _ in range(2)]
    w2c = [ar.alloc([128, FC // 128, D], BF16) for _ in range(2)]
    gTt = [ar.alloc([128, FC // 128, 512], BF16) for _ in range(2)]
    sil = [ar.alloc([128, 512], F32) for _ in range(2)]
    sig = [ar.alloc([128, 512], F32) for _ in range(2)]

    def load_w(idx):
        e, fc = idx // NFC, idx % NFC
        i = idx % 2
        g.dma("pool", w1c[i], W["l1_exp_w1"][e].rearrange("(k p) f -> p k f", p=128)[:, :, fc * FC:(fc + 1) * FC], w=["w1c%d" % i])
        g.dma("pool", w3c[i], W["l1_exp_w3"][e].rearrange("(k p) f -> p k f", p=128)[:, :, fc * FC:(fc + 1) * FC], w=["w3c%d" % i])
        g.dma("pool", w2c[i], W["l1_exp_w2"][e].rearrange("(ft p) d -> p ft d", p=128)[:, fc * (FC // 128):(fc + 1) * (FC // 128), :], w=["w2c%d" % i])
    load_w(0)
    units = [(e, fc, gi) for e in range(NE) for fc in range(NFC) for gi in range(NG)]
    state = {"c2": 0}

    def gate_bc(e):
        for gi, (t0, n, v) in enumerate(lat_groups):
            pb = g.bank[gi % 4]
            g.mm(pb[:, 0:n], esel[0:8, e * 128:(e + 1) * 128], gT_sb[0:8, t0:t0 + n], True, True,
                 r=["esel", "gT_sb"], w=["bank%d" % (gi % 4)])
            g.cp("act", gbc[:, gi, 0:n], pb[:, 0:n], r=["bank%d" % (gi % 4)], w=["gbc"])

    def h_stage(t, fts):
        e, fc, gi = units[t]
        i = (e * NFC + fc) % 2
        t0, n, v = lat_groups[gi]
        hk = "hT.g%d" % gi
        gt = gTt[t % 2]
        gk_ = "gTt%d" % (t % 2)
        hsl = [g.hT[:, k, t0:t0 + n] for k in range(KT)]
        for ft in fts:
            j = state["c2"] % 2
            state["c2"] += 1
            h1, h3 = g.bank[2 * j], g.bank[2 * j + 1]
            g.mmk(h1[:, 0:n], [w1c[i][:, k, ft * 128:(ft + 1) * 128] for k in range(KT)], hsl,
                  r=["w1c%d" % i, hk], w=["bank%d" % (2 * j)])
            g.mmk(h3[:, 0:n], [w3c[i][:, k, ft * 128:(ft + 1) * 128] for k in range(KT)], hsl,
                  r=["w3c%d" % i, hk], w=["bank%d" % (2 * j + 1)])
            g.act(sil[j][:, 0:n], h1[:, 0:n], AF.Silu, r=["bank%d" % (2 * j)], w=["sil%d" % j])
            g.tt("pool", sig[j][:, 0:n], sil[j][:, 0:n], gbc[:, gi, 0:n], ALU.mult, r=["sil%d" % j, "gbc"], w=["sig%d" % j])
            g.tt("dve", gt[:, ft, 0:n], sig[j][:, 0:n], h3[:, 0:n], ALU.mult,
                 r=["sig%d" % j, "bank%d" % (2 * j + 1)], w=[gk_])

    def y_stage(t, dcs):
        e, fc, gi = units[t]
        i = (e * NFC + fc) % 2
        t0, n, v = lat_groups[gi]
        gt = gTt[t % 2]
        gk_ = "gTt%d" % (t % 2)
        for dc in dcs:
            pb = g.bank[4 + dc % 4]
            bk = "bank%d" % (4 + dc % 4)
            g.mmk(pb[:, 0:n], [w2c[i][:, ft, dc * 128:(dc + 1) * 128] for ft in range(FC // 128)],
                  [gt[:, ft, 0:n] for ft in range(FC // 128)], r=["w2c%d" % i, gk_], w=[bk])
            res = g.xT[:, dc, t0:t0 + n]
            g.stt("dve", res, pb[:, 0:n], M["gate2"][:, dc, 0:1], res, ALU.mult, ALU.add,
                  r=[bk, "xT.g%d" % gi] + M["key"], w=["xT.g%d" % gi])

    for t in range(len(units)):
        e, fc, gi = units[t]
        if fc == 0 and gi == 0:
            gate_bc(e)
        nft = FC // 128
        for ft in range(nft):
            h_stage(t, [ft])
            if t > 0:
                y_stage(t - 1, range(ft * 8 // nft, (ft + 1) * 8 // nft))
        if gi == 0:
            idx = e * NFC + fc
            if idx + 1 < NE * NFC:
                load_w(idx + 1)
    y_stage(len(units) - 1, range(8))
    S.barrier()


def _out_final(self):
    g = self
    xo = g.outp("xT_out", [128, KT, g.TL])
    for k in range(KT):
        g.dma("sp", xo[:, k, :], g.xT[:, k, :], r=["xT.g%d" % i for i in range(len(g.groups))])


Gen.layer1_pre = _layer1_pre
Gen.layer1_main = _layer1_main
Gen.out_final = _out_final


def build_l1(TL, mode, stop=None):
    g = Gen(TL, mode)
    g.setup_common()
    g.layer1_pre()
    if mode == "main1":
        g.layer1_main(stop)
        g.out_final()
    g.finish()
    return g


def _esel():
    e = np.zeros((8, NE * 128), np.float32)
    for i in range(NE):
        e[i, i * 128:(i + 1) * 128] = 1.0
    return e


def run_layer1(inputs, TL, xTs, cTs, stop=None):
    L1 = [k for k in inputs if k.startswith("l1_")]
    base = []
    for c in range(NCORES):
        m = core_inputs(inputs, TL, c, xTs[c], cTs[c])
        for k in L1:
            m[k] = inputs[k]
        m["c_esel"] = _esel()
        base.append(m)
    g1 = build_l1(TL, "pre1")
    r1 = run_prog(g1, base)
    K1all = np.ascontiguousarray(np.stack([r["K1own"] for r in r1], axis=1))
    V1all = np.ascontiguousarray(np.stack([r["V1own"] for r in r1], axis=1))
    for m in base:
        m["K1all"], m["V1all"] = K1all, V1all
    g2 = build_l1(TL, "main1", stop)
    r2 = run_prog(g2, base)
    return r2


def build_fused(TL, upto=None, stop=None):
    g = Gen(TL, "fused")
    g.setup_common()
    g.layer0_pre()
    g.layer0_main(stop)
    if upto == "l0":
        g.out_layer0()
        g.finish()
        return g
    g.layer1_pre()
    g.layer1_main()
    g.out_final()
    g.finish()
    return g


def run_fused(inputs, TL):
    base = []
    esel = _esel()
    for c in range(NCORES):
        m = core_inputs(inputs, TL, c)
        for k in inputs:
            if k.startswith("l0_") or k.startswith("l1_"):
                m[k] = inputs[k]
        m["c_esel"] = esel
        base.append(m)
    g = build_fused(TL)
    return run_prog(g, base)


def kernel_unfused(**inputs):
    inputs = {k: np.asarray(v) for k, v in inputs.items()}
    SEQ = inputs["x"].shape[1]
    TL = SEQ // NCORES
    xTs, cTs = run_layer0(inputs, TL)
    r2 = run_layer1(inputs, TL, xTs, cTs)
    out = np.concatenate([_from_fm(r["xT_out"]) for r in r2], axis=0)
    return out[None].astype(np.float32)


FUSED = False


def kernel(**inputs):
    if not FUSED:
        return kernel_unfused(**inputs)
    inputs = {k: np.asarray(v) for k, v in inputs.items()}
    SEQ = inputs["x"].shape[1]
    TL = SEQ // NCORES
    r2 = run_fused(inputs, TL)
    out = np.concatenate([_from_fm(r["xT_out"]) for r in r2], axis=0)
    return out[None].astype(np.float32)
```
